# Optimizing a Trainium2 kernel written in Bass

```python
import math
import jax
import jax.numpy as jnp
from jax import lax
import numpy as np

D_MODEL = 2048
BATCH = 8
SEQ = 4096
DEPTH = 4

N_MIXERS = 3
PLE_DIM = 256
NORM_EPS = 1e-6
NEG_INF = -1e30
TINY = 1e-30
Q_BLOCK = 128

N_BUCKETS = 32
BUCKET_MAX_DIST = 2048
N_BIAS_HEADS = 16

LRU_WIDTH = D_MODEL
LRU_BLOCKS = 8
LRU_BLOCK_W = LRU_WIDTH // LRU_BLOCKS
CONV_W = 4
LRU_C = 8.0

DIL_PATTERNS = ((128, 1), (512, 4), (2048, 16))
N_DIL = len(DIL_PATTERNS)
DIL_HEADS = 16
DIL_HEAD_DIM = 64
DIL_WIDTH = DIL_HEADS * DIL_HEAD_DIM
DIL_IN_COLS = N_DIL * 3 * DIL_WIDTH + DIL_WIDTH

NSA_HEADS = 16
NSA_KV_GROUPS = 4
NSA_REP = NSA_HEADS // NSA_KV_GROUPS
NSA_HEAD_DIM = 128
NSA_WIDTH = NSA_HEADS * NSA_HEAD_DIM
NSA_KV_WIDTH = NSA_KV_GROUPS * NSA_HEAD_DIM
CMP_BLOCK = 32
CMP_STRIDE = 16
CMP_HIDDEN = 512
SEL_BLOCK = 64
SEL_TOPK = 16
WIN = 512
SEL_QCHUNK = 16
FORCE_BONUS = 1e4
NSA_SPLITS = (NSA_WIDTH,) + (NSA_KV_WIDTH,) * 6 + (3 * NSA_HEADS, NSA_WIDTH)
NSA_IN_COLS = sum(NSA_SPLITS)

kernel_name = "hybrid_rglru_dilated_nsa_trunk"


def _rmsnorm(x, g):
    xf = x.astype(jnp.float32)
    y = xf * lax.rsqrt(jnp.mean(xf * xf, axis=-1, keepdims=True) + NORM_EPS)
    return (y * g.astype(jnp.float32)).astype(x.dtype)


def _t5_bucket(dist):
    n = jnp.maximum(dist, 0)
    max_exact = N_BUCKETS // 2
    nf = jnp.maximum(n, max_exact).astype(jnp.float32)
    large = max_exact + (jnp.log(nf / max_exact) / math.log(BUCKET_MAX_DIST / max_exact)
                         * (N_BUCKETS - max_exact)).astype(jnp.int32)
    return jnp.where(n < max_exact, n, jnp.minimum(large, N_BUCKETS - 1))


def _masked_softmax(logits, mask):
    l = jnp.where(mask, logits, NEG_INF)
    m = jnp.max(l, axis=-1, keepdims=True)
    e = jnp.where(mask, jnp.exp(l - m), 0.0)
    s = jnp.sum(e, axis=-1, keepdims=True)
    return e / jnp.maximum(s, TINY), m[..., 0], s[..., 0]


def _unblock(t, length):
    nb, n, qb = t.shape[:3]
    t = jnp.moveaxis(t, 0, 1).reshape((n, nb * qb) + t.shape[3:])
    return t[:, :length]


def _banded_attention(q, k, v, rel_bias, lookback, max_delta, dilation):
    n, length, G, R, dh = q.shape
    nb = -(-length // Q_BLOCK)
    lp = nb * Q_BLOCK
    qp = jnp.pad(q, ((0, 0), (0, lp - length), (0, 0), (0, 0), (0, 0)))
    kp = jnp.pad(k, ((0, 0), (lookback, lp - length), (0, 0), (0, 0)))
    vp = jnp.pad(v, ((0, 0), (lookback, lp - length), (0, 0), (0, 0)))
    table = rel_bias.reshape(N_BUCKETS, G, R)
    span = Q_BLOCK + lookback
    scale = dh ** -0.5

    def block(b):
        q0 = b * Q_BLOCK
        qs = lax.dynamic_slice_in_dim(qp, q0, Q_BLOCK, axis=1)
        ks = lax.dynamic_slice_in_dim(kp, q0, span, axis=1)
        vs = lax.dynamic_slice_in_dim(vp, q0, span, axis=1)
        qpos = q0 + jnp.arange(Q_BLOCK)
        kpos = q0 - lookback + jnp.arange(span)
        delta = qpos[:, None] - kpos[None, :]
        mask = (delta >= 0) & (delta <= max_delta) & (kpos[None, :] >= 0)
        bias = jnp.transpose(table[_t5_bucket(delta * dilation)], (2, 3, 0, 1)).astype(jnp.float32)
        logits = jnp.einsum('nqgrd,nkgd->ngrqk', qs, ks).astype(jnp.float32) * scale + bias
        prob, m, s = _masked_softmax(logits, mask)
        o = jnp.einsum('ngrqk,nkgd->nqgrd', prob, vs.astype(jnp.float32))
        return o, jnp.transpose(m, (0, 3, 1, 2)), jnp.transpose(s, (0, 3, 1, 2))

    o, m, s = lax.map(block, jnp.arange(nb))
    return _unblock(o, length), _unblock(m, length), _unblock(s, length)


def _to_strided(t, d):
    bn, s = t.shape[:2]
    rest = t.shape[2:]
    t = jnp.swapaxes(t.reshape((bn, s // d, d) + rest), 1, 2)
    return t.reshape((bn * d, s // d) + rest)


def _from_strided(t, d, bn):
    n, length = t.shape[:2]
    rest = t.shape[2:]
    t = jnp.swapaxes(t.reshape((bn, d, length) + rest), 1, 2)
    return t.reshape((bn, length * d) + rest)


def _rglru_mixer(u, w_in, conv_w, conv_b, w_r, b_r, w_i, b_i, lam, w_out):
    bn, s, _ = u.shape
    proj = u @ w_in
    xb, gate = proj[..., :LRU_WIDTH], proj[..., LRU_WIDTH:]
    xc = lax.conv_general_dilated(xb, conv_w[:, None, :], window_strides=(1,),
                                  padding=((CONV_W - 1, 0),),
                                  dimension_numbers=('NWC', 'WIO', 'NWC'),
                                  feature_group_count=LRU_WIDTH) + conv_b
    xblk = xc.reshape(bn, s, LRU_BLOCKS, LRU_BLOCK_W)
    r = jax.nn.sigmoid(jnp.einsum('bsnc,ncd->bsnd', xblk, w_r) + b_r).reshape(bn, s, LRU_WIDTH)
    i = jax.nn.sigmoid(jnp.einsum('bsnc,ncd->bsnd', xblk, w_i) + b_i).reshape(bn, s, LRU_WIDTH)
    log_a = -LRU_C * r.astype(jnp.float32) * jax.nn.softplus(-lam.astype(jnp.float32))
    a = jnp.exp(log_a)
    bterm = jnp.sqrt(-jnp.expm1(2.0 * log_a)) * (i * xc).astype(jnp.float32)

    def combine(e1, e2):
        a1, b1 = e1
        a2, b2 = e2
        return a1 * a2, a2 * b1 + b2

    _, h = lax.associative_scan(combine, (a, bterm), axis=1)
    y = h.astype(u.dtype) * jax.nn.silu(gate)
    return y @ w_out


def _dilated_mixer(u, w_in, w_out, rel_bias):
    bn, s, _ = u.shape
    proj = u @ w_in
    qkv = proj[..., :N_DIL * 3 * DIL_WIDTH].reshape(bn, s, N_DIL, 3, DIL_HEADS, 1, DIL_HEAD_DIM)
    gate = proj[..., N_DIL * 3 * DIL_WIDTH:]
    outs, maxes, dens = [], [], []
    for gi, (window, dil) in enumerate(DIL_PATTERNS):
        q = _to_strided(qkv[:, :, gi, 0], dil)
        k = _to_strided(qkv[:, :, gi, 1, :, 0], dil)
        v = _to_strided(qkv[:, :, gi, 2, :, 0], dil)
        o, m, den = _banded_attention(q, k, v, rel_bias, window // dil, window // dil, dil)
        outs.append(_from_strided(o[:, :, :, 0], dil, bn))
        maxes.append(_from_strided(m[:, :, :, 0], dil, bn))
        dens.append(_from_strided(den[:, :, :, 0], dil, bn))
    o = jnp.stack(outs)
    m = jnp.stack(maxes)
    den = jnp.stack(dens)
    wts = den * jnp.exp(m - jnp.max(m, axis=0, keepdims=True))
    o = jnp.sum(wts[..., None] * o, axis=0) / jnp.sum(wts, axis=0)[..., None]
    y = o.reshape(bn, s, DIL_WIDTH).astype(u.dtype) * jax.nn.silu(gate)
    return y @ w_out


def _compress(t, cidx, pos, w1, w2):
    bn, nc = t.shape[0], cidx.shape[0]
    blk = t[:, cidx] + pos[None, None, :, None, :]
    blk = jnp.swapaxes(blk, 2, 3).reshape(bn, nc, NSA_KV_GROUPS, CMP_BLOCK * NSA_HEAD_DIM)
    return jax.nn.gelu(blk @ w1) @ w2


def _nsa_mixer(u, w_in, pos_k, w1_k, w2_k, pos_v, w1_v, w2_v, w_out, rel_bias):
    bn, s, _ = u.shape
    G, R, dh = NSA_KV_GROUPS, NSA_REP, NSA_HEAD_DIM
    scale = dh ** -0.5
    proj = u @ w_in
    offs = np.cumsum(NSA_SPLITS)[:-1].tolist()
    q, kc, vc, ksl, vsl, kw, vw, bgate, gpath = jnp.split(proj, offs, axis=-1)
    q = q.reshape(bn, s, G, R, dh)
    kc, vc, ksl, vsl, kw, vw = [t.reshape(bn, s, G, dh) for t in (kc, vc, ksl, vsl, kw, vw)]

    nc = (s - CMP_BLOCK) // CMP_STRIDE + 1
    cidx = np.arange(nc)[:, None] * CMP_STRIDE + np.arange(CMP_BLOCK)[None, :]
    cend = cidx[:, -1]
    kcmp = _compress(kc, cidx, pos_k, w1_k, w2_k)
    vcmp = _compress(vc, cidx, pos_v, w1_v, w2_v)
    nsel = s // SEL_BLOCK
    topk = min(SEL_TOPK, nsel)
    sel_start = np.arange(nsel) * SEL_BLOCK
    cover = jnp.asarray(((cidx[:, :1] < sel_start[None, :] + SEL_BLOCK)
                         & (cidx[:, -1:] >= sel_start[None, :])).astype(np.float32))
    blk_ids = jnp.arange(nsel)

    def cmp_block(b):
        q0 = b * Q_BLOCK
        qs = lax.dynamic_slice_in_dim(q, q0, Q_BLOCK, axis=1)
        t = q0 + jnp.arange(Q_BLOCK)
        mask = cend[None, :] <= t[:, None]
        logits = jnp.einsum('bqgrd,bcgd->bgrqc', qs, kcmp).astype(jnp.float32) * scale
        prob, _, _ = _masked_softmax(logits, mask)
        o = jnp.einsum('bgrqc,bcgd->bqgrd', prob, vcmp.astype(jnp.float32))
        imp = jnp.einsum('bgrqc,cn->bgqn', prob, cover)
        cur = t // SEL_BLOCK
        forced = ((blk_ids[None, :] == 0) | (blk_ids[None, :] == cur[:, None])
                  | (blk_ids[None, :] == cur[:, None] - 1)).astype(jnp.float32)
        valid = blk_ids[None, :] * SEL_BLOCK <= t[:, None]
        imp = jnp.where(valid, imp + FORCE_BONUS * forced, NEG_INF)
        _, idx = lax.top_k(imp, topk)
        return o, jnp.transpose(idx, (0, 2, 1, 3))

    o_cmp, sel_idx = lax.map(cmp_block, jnp.arange(s // Q_BLOCK))
    o_cmp = _unblock(o_cmp, s)
    sel_idx = _unblock(sel_idx, s)

    ksT = jnp.swapaxes(ksl, 1, 2)
    vsT = jnp.swapaxes(vsl, 1, 2)
    nkeys = topk * SEL_BLOCK
    tgr = jnp.transpose(rel_bias.reshape(N_BUCKETS, G, R), (1, 2, 0))
    g_ix = jnp.arange(G)[None, :, None, None, None]
    r_ix = jnp.arange(R)[None, None, :, None, None]
    gather = jax.vmap(jax.vmap(lambda a, ix: a[ix]))

    def sel_chunk(c):
        q0 = c * SEL_QCHUNK
        qs = lax.dynamic_slice_in_dim(q, q0, SEL_QCHUNK, axis=1)
        idx = lax.dynamic_slice_in_dim(sel_idx, q0, SEL_QCHUNK, axis=1)
        tok = idx[..., None] * SEL_BLOCK + jnp.arange(SEL_BLOCK)
        tok = jnp.transpose(tok.reshape(bn, SEL_QCHUNK, G, nkeys), (0, 2, 1, 3))
        flat = tok.reshape(bn, G, SEL_QCHUNK * nkeys)
        kg = gather(ksT, flat).reshape(bn, G, SEL_QCHUNK, nkeys, dh)
        vg = gather(vsT, flat).reshape(bn, G, SEL_QCHUNK, nkeys, dh)
        t = q0 + jnp.arange(SEL_QCHUNK)
        delta = t[None, None, :, None] - tok
        bias = tgr[g_ix, r_ix, _t5_bucket(delta)[:, :, None]].astype(jnp.float32)
        logits = jnp.einsum('bqgrd,bgqnd->bgrqn', qs, kg).astype(jnp.float32) * scale + bias
        prob, _, _ = _masked_softmax(logits, (delta >= 0)[:, :, None])
        return jnp.einsum('bgrqn,bgqnd->bqgrd', prob, vg.astype(jnp.float32))

    o_sel = _unblock(lax.map(sel_chunk, jnp.arange(s // SEL_QCHUNK)), s)

    o_win, _, _ = _banded_attention(q, kw, vw, rel_bias, WIN, WIN - 1, 1)

    gts = jax.nn.sigmoid(bgate.reshape(bn, s, G, R, 3).astype(jnp.float32))
    o = gts[..., 0:1] * o_cmp + gts[..., 1:2] * o_sel + gts[..., 2:3] * o_win
    y = o.reshape(bn, s, NSA_WIDTH).astype(u.dtype) * jax.nn.silu(gpath)
    return y @ w_out


def setup_inputs(seed: int = 0) -> dict:
    key = jax.random.key(seed)
    ks = iter(jax.random.split(key, 40))
    n_a = len(range(0, DEPTH, N_MIXERS))
    n_b = len(range(1, DEPTH, N_MIXERS))
    n_c = len(range(2, DEPTH, N_MIXERS))

    def nrm(shape, scale):
        return scale * jax.random.normal(next(ks), shape, jnp.float32)

    a_dec = jax.random.uniform(next(ks), (n_a, LRU_WIDTH), jnp.float32, minval=0.9, maxval=0.999)
    return {
        "x": nrm((BATCH, SEQ, D_MODEL), 1.0),
        "p": nrm((DEPTH, BATCH, SEQ, PLE_DIM), 1.0),
        "rel_bias": nrm((N_BUCKETS, N_BIAS_HEADS), 0.5),
        "norm_pre": 1.0 + nrm((DEPTH, D_MODEL), 0.05),
        "norm_post": 1.0 + nrm((DEPTH, D_MODEL), 0.05),
        "ple_w_proj": nrm((DEPTH, PLE_DIM, D_MODEL), PLE_DIM ** -0.5),
        "ple_w_gate": nrm((DEPTH, D_MODEL, D_MODEL), D_MODEL ** -0.5),
        "a_w_in": nrm((n_a, D_MODEL, 2 * LRU_WIDTH), D_MODEL ** -0.5),
        "a_conv_w": nrm((n_a, CONV_W, LRU_WIDTH), CONV_W ** -0.5),
        "a_conv_b": nrm((n_a, LRU_WIDTH), 0.01),
        "a_w_r": nrm((n_a, LRU_BLOCKS, LRU_BLOCK_W, LRU_BLOCK_W), LRU_BLOCK_W ** -0.5),
        "a_b_r": nrm((n_a, LRU_BLOCKS, LRU_BLOCK_W), 0.01),
        "a_w_i": nrm((n_a, LRU_BLOCKS, LRU_BLOCK_W, LRU_BLOCK_W), LRU_BLOCK_W ** -0.5),
        "a_b_i": nrm((n_a, LRU_BLOCKS, LRU_BLOCK_W), 0.01),
        "a_lam": jnp.log(a_dec) - jnp.log1p(-a_dec),
        "a_w_out": nrm((n_a, LRU_WIDTH, D_MODEL), LRU_WIDTH ** -0.5),
        "b_w_in": nrm((n_b, D_MODEL, DIL_IN_COLS), D_MODEL ** -0.5),
        "b_w_out": nrm((n_b, DIL_WIDTH, D_MODEL), DIL_WIDTH ** -0.5),
        "c_w_in": nrm((n_c, D_MODEL, NSA_IN_COLS), D_MODEL ** -0.5),
        "c_cmp_pos_k": nrm((n_c, CMP_BLOCK, NSA_HEAD_DIM), 0.1),
        "c_cmp_w1_k": nrm((n_c, CMP_BLOCK * NSA_HEAD_DIM, CMP_HIDDEN), (CMP_BLOCK * NSA_HEAD_DIM) ** -0.5),
        "c_cmp_w2_k": nrm((n_c, CMP_HIDDEN, NSA_HEAD_DIM), CMP_HIDDEN ** -0.5),
        "c_cmp_pos_v": nrm((n_c, CMP_BLOCK, NSA_HEAD_DIM), 0.1),
        "c_cmp_w1_v": nrm((n_c, CMP_BLOCK * NSA_HEAD_DIM, CMP_HIDDEN), (CMP_BLOCK * NSA_HEAD_DIM) ** -0.5),
        "c_cmp_w2_v": nrm((n_c, CMP_HIDDEN, NSA_HEAD_DIM), CMP_HIDDEN ** -0.5),
        "c_w_out": nrm((n_c, NSA_WIDTH, D_MODEL), NSA_WIDTH ** -0.5),
    }


def reference(x, p, rel_bias, norm_pre, norm_post, ple_w_proj, ple_w_gate,
              a_w_in, a_conv_w, a_conv_b, a_w_r, a_b_r, a_w_i, a_b_i, a_lam, a_w_out,
              b_w_in, b_w_out,
              c_w_in, c_cmp_pos_k, c_cmp_w1_k, c_cmp_w2_k, c_cmp_pos_v, c_cmp_w1_v, c_cmp_w2_v, c_w_out):
    h = x
    for i in range(DEPTH):
        kind = i % N_MIXERS
        j = i // N_MIXERS
        u = _rmsnorm(h, norm_pre[i])
        if kind == 0:
            y = _rglru_mixer(u, a_w_in[j], a_conv_w[j], a_conv_b[j], a_w_r[j], a_b_r[j],
                             a_w_i[j], a_b_i[j], a_lam[j], a_w_out[j])
        elif kind == 1:
            y = _dilated_mixer(u, b_w_in[j], b_w_out[j], rel_bias)
        else:
            y = _nsa_mixer(u, c_w_in[j], c_cmp_pos_k[j], c_cmp_w1_k[j], c_cmp_w2_k[j],
                           c_cmp_pos_v[j], c_cmp_w1_v[j], c_cmp_w2_v[j], c_w_out[j], rel_bias)
        h = h + _rmsnorm(y, norm_post[i])
        h = h + jax.nn.sigmoid(h @ ple_w_gate[i]) * (p[i] @ ple_w_proj[i])
    return h
```

```python
import math
from contextlib import ExitStack
import numpy as np
import concourse.bass as bass
import concourse.mybir as mybir
from concourse.bass_utils import run_bass_kernel_spmd

F32 = mybir.dt.float32
BF16 = mybir.dt.bfloat16
AF = mybir.ActivationFunctionType
ALU = mybir.AluOpType

S = 4096
D = 2048
NT = 512
NTI = S // NT
import os
DBG_TILES = int(os.environ.get('DBG_TILES', '8'))
DBG_HEADS = int(os.environ.get('DBG_HEADS', '16'))
DBG_DUMP = os.environ.get('DBG_DUMP', '') != ''
DEPTH = 4
NEG = -30000.0
EPS = 1e-6


class Buf:
    def __init__(self, t, name):
        self.t = t
        self.name = name
        self.writers = {}
        self.readers = {}

    def __getitem__(self, k):
        return self.t[k]


class View:
    def __init__(self, base, t):
        self.base = base
        self.t = t
        self.name = base.name + "_v"

    def __getitem__(self, k):
        return self.t[k]

    @property
    def writers(self):
        return self.base.writers

    @writers.setter
    def writers(self, v):
        self.base.writers = v

    @property
    def readers(self):
        return self.base.readers

    @readers.setter
    def readers(self, v):
        self.base.readers = v


class FW:
    ENGS = ("pe", "act", "dve", "pool", "sp")

    def __init__(self, nc, ctx):
        self.nc = nc
        self.ctx = ctx
        self.stack = [ctx]
        self.lists = {e: [] for e in self.ENGS}
        self.waited = {e: {} for e in self.ENGS}
        self.sems = {}
        self.count = {}
        for e in ("pe", "act", "dve", "pool"):
            self.getsem("E_" + e)
        self.nbuf = 0
        self.ninst = 0

    def getsem(self, key):
        if key not in self.sems:
            self.sems[key] = self.ctx.enter_context(self.nc.semaphore("s_" + key))
            self.count[key] = 0
        return key

    def push(self, sub):
        self.stack.append(sub)

    def pop(self):
        self.barrier()
        self.stack.pop()

    def sbuf(self, shape, dtype, name=None):
        self.nbuf += 1
        name = (name or "sb") + f"_{self.nbuf}"
        t = self.stack[-1].enter_context(self.nc.sbuf_tensor(name, list(shape), dtype))
        return Buf(t, name)

    def psum(self, shape, dtype, name=None):
        self.nbuf += 1
        name = (name or "ps") + f"_{self.nbuf}"
        t = self.stack[-1].enter_context(self.nc.psum_tensor(name, list(shape), dtype))
        return Buf(t, name)

    def dram(self, shape, dtype, name=None):
        self.nbuf += 1
        name = (name or "dr") + f"_{self.nbuf}"
        if DBG_DUMP:
            t = self.nc.dram_tensor(name.rsplit("_", 1)[0], list(shape), dtype, kind="ExternalOutput")
        else:
            t = self.nc.dram_tensor(name, list(shape), dtype, kind="Internal")
        return t.ap()

    def op(self, eng, fn, r=(), w=(), dsem=None):
        E = self.lists[eng]
        needs = {}

        def upd(d):
            for s, v in d.items():
                if needs.get(s, 0) < v:
                    needs[s] = v

        for b in r:
            upd(b.writers)
        for b in w:
            if eng == "pe":
                upd({s: v for s, v in b.writers.items() if s != "E_pe"})
            else:
                upd(b.writers)
            upd(b.readers)
        if dsem is not None:
            sem = self.getsem("D_" + dsem)
            inc = 16
            if self.count[sem] > 0:
                upd({sem: self.count[sem]})
        else:
            sem = "E_" + eng
            inc = 1
        wd = self.waited[eng]
        waits = []
        for s, v in needs.items():
            if wd.get(s, 0) < v:
                wd[s] = v
                waits.append((s, v))
        self.count[sem] += inc
        val = self.count[sem]
        E.append((waits, fn, sem, val, inc))
        self.ninst += 1 + len(waits)
        for b in r:
            if b.readers.get(sem, 0) < val:
                b.readers[sem] = val
        for b in w:
            b.writers = {sem: val}
            b.readers = {}

    def barrier(self):
        for e in self.ENGS:
            wd = self.waited[e]
            waits = []
            for s, v in self.count.items():
                if v > 0 and wd.get(s, 0) < v:
                    wd[s] = v
                    waits.append((s, v))
            if waits:
                self.lists[e].append((waits, None, None, 0, 0))

    def emit(self):
        nc = self.nc
        sems = self.sems
        lists = self.lists
        targets = {}
        for e in self.ENGS:
            for waits, fn, sem, val, inc in lists[e]:
                for s, v in waits:
                    targets.setdefault(s, set()).add(v)
        remap = {}
        for s, vals in targets.items():
            if s.startswith("E_"):
                remap[s] = {v: i + 1 for i, v in enumerate(sorted(vals))}
        self.nsignal = sum(len(m) for m in remap.values())

        def run(e, lst):
            for waits, fn, sem, val, inc in lst:
                for s, v in waits:
                    e.wait_ge(sems[s], remap[s][v] if s in remap else v)
                if fn is not None:
                    ins = fn(e)
                    if sem.startswith("E_"):
                        if val in remap.get(sem, ()):
                            ins.then_inc(sems[sem], 1)
                    else:
                        ins.then_inc(sems[sem], inc)

        with nc.Block() as block:
            @block.tensor
            def _(e):
                run(e, lists["pe"])

            @block.scalar
            def _(e):
                run(e, lists["act"])

            @block.vector
            def _(e):
                run(e, lists["dve"])

            @block.gpsimd
            def _(e):
                run(e, lists["pool"])

            @block.sync
            def _(e):
                run(e, lists["sp"])


class Rot:
    def __init__(self, items):
        self.items = items
        self.i = 0

    def next(self):
        b = self.items[self.i % len(self.items)]
        idx = self.i % len(self.items)
        self.i += 1
        return b, idx


def _bucket_np(n):
    n = np.maximum(n, 0)
    nf = np.maximum(n, 16).astype(np.float32)
    large = 16 + (np.log(nf / np.float32(16)) / np.float32(math.log(2048 / 16)) * np.float32(16)).astype(np.int32)
    return np.where(n < 16, n, np.minimum(large, 31))


TV_KINDS = [("dil0", 1, 128, 1152), ("dil1", 4, 128, 1152), ("dil2", 16, 128, 1152),
            ("sel", 1, 1 << 30, 2688), ("win", 1, 511, 1536)]
TV_OFF = {}
_o = 0
for _n, _d, _m, _x in TV_KINDS:
    TV_OFF[_n] = (_o, _x)
    _o += _x
TV_TOT = _o


def _host_tables():
    oh = np.zeros((33, TV_TOT), np.float32)
    for name, dil, maxd, X in TV_KINDS:
        o, _ = TV_OFF[name]
        x = np.arange(X)
        delta = x - 511
        valid = (delta >= 0) & (delta <= maxd)
        b = _bucket_np(delta * dil)
        for xi in range(X):
            if valid[xi]:
                oh[b[xi], o + xi] = 1.0
            else:
                oh[32, o + xi] = 1.0
    ncmp = 255
    cidx0 = np.arange(ncmp) * 16
    cend = cidx0 + 31
    sel_start = np.arange(64) * 64
    cover = ((cidx0[:, None] < sel_start[None, :] + 64) & (cend[:, None] >= sel_start[None, :])).astype(np.float32)
    cov = np.zeros((256, 64), np.float32)
    cov[:255] = cover
    cov = cov.reshape(2, 128, 64).transpose(1, 0, 2).copy()
    t = np.arange(S)
    cur = t // 64
    n = np.arange(64)
    forced = (n[None, :] == 0) | (n[None, :] == cur[:, None]) | (n[None, :] == cur[:, None] - 1)
    validb = n[None, :] * 64 <= t[:, None]
    addt = np.where(validb, 1e4 * forced.astype(np.float32), -1e30).astype(np.float32)
    addt = addt.reshape(32, 128, 64).transpose(1, 0, 2).copy()
    cm = np.zeros((128, 8, 2, 512), np.float32)
    for j in range(8):
        for cb in range(2):
            c = cb * 128 + np.arange(128)
            tt = j * 512 + np.arange(512)
            ok = (16 * c[:, None] + 31) <= tt[None, :]
            cm[:, j, cb, :] = np.where(ok, 0.0, NEG)
    ex = np.zeros((64, 32, 128), np.float32)
    for kb in range(32):
        for k in range(128):
            ex[2 * kb + k // 64, kb, k] = 1.0
    ident = np.eye(128, dtype=np.float32)
    return oh, cov, addt, cm, ex, ident


def _pack_vecs(inp):
    cols = []
    off = {}

    def add(name, arr):
        off[name] = sum(c.shape[1] for c in cols)
        cols.append(np.ascontiguousarray(arr, dtype=np.float32))

    def fm(v):
        return np.asarray(v, np.float32).reshape(16, 128).T

    for i in range(DEPTH):
        add(f"pre{i}", fm(inp["norm_pre"][i]))
        add(f"post{i}", fm(inp["norm_post"][i]))
    for j in range(inp["a_w_in"].shape[0]):
        for t in range(4):
            add(f"cw{j}_{t}", fm(inp["a_conv_w"][j, t]))
        add(f"cb{j}", fm(inp["a_conv_b"][j]))
        add(f"br{j}", fm(inp["a_b_r"][j].reshape(-1)))
        add(f"bi{j}", fm(inp["a_b_i"][j].reshape(-1)))
        add(f"lam{j}", fm(inp["a_lam"][j]))
    add("posk", np.asarray(inp["c_cmp_pos_k"][0], np.float32).T)
    add("posv", np.asarray(inp["c_cmp_pos_v"][0], np.float32).T)
    return np.concatenate(cols, axis=1), off


class Builder:
    def __init__(self, voff, nvec, layers=(0, 1, 2, 3)):
        self.voff = voff
        self.nvec = nvec
        self.layers = layers
        nc = bass.Bass("TRN2", target_bir_lowering=False)
        self.nc = nc

        self.input_names = []
        kinds = {l % 3 for l in layers}

        def inp(name, shape, dt=F32, need=True):
            if not need:
                return None
            self.input_names.append(name)
            return nc.dram_tensor(name, list(shape), dt, kind="ExternalInput").ap()

        att = (1 in kinds) or (2 in kinds)
        self.xT = inp("xT", [D, S])
        self.pT = inp("pT", [DEPTH, 256, S])
        self.vecs_d = inp("vecs", [128, nvec])
        self.relb = inp("relb", [33, 16], need=att)
        self.oh = inp("oh", [33, TV_TOT], need=att)
        self.cov = inp("cov", [128, 2, 64], need=2 in kinds)
        self.addt = inp("addt", [128, 32, 64], need=2 in kinds)
        self.cm = inp("cm", [128, 8, 2, 512], need=2 in kinds)
        self.ex = inp("ex", [64, 32, 128], need=2 in kinds)
        self.ident = inp("ident", [128, 128], need=2 in kinds)
        self.jrev = inp("jrev", [128, 128], need=att)
        self.ple_w_proj = inp("ple_w_proj", [DEPTH, 256, D])
        self.ple_w_gate = inp("ple_w_gate", [DEPTH, D, D])
        self.a_w_in = inp("a_w_in", [2, D, 2 * D], need=0 in kinds)
        self.a_w_r = inp("a_w_r", [2, 8, 256, 256], need=0 in kinds)
        self.a_w_i = inp("a_w_i", [2, 8, 256, 256], need=0 in kinds)
        self.a_w_out = inp("a_w_out", [2, D, D], need=0 in kinds)
        self.b_w_in = inp("b_w_in", [1, D, 10240], need=1 in kinds)
        self.b_w_out = inp("b_w_out", [1, 1024, D], need=1 in kinds)
        self.c_w_in = inp("c_w_in", [1, D, 7216], need=2 in kinds)
        self.c_w1_k = inp("c_cmp_w1_k", [1, 4096, 512], need=2 in kinds)
        self.c_w2_k = inp("c_cmp_w2_k", [1, 512, 128], need=2 in kinds)
        self.c_w1_v = inp("c_cmp_w1_v", [1, 4096, 512], need=2 in kinds)
        self.c_w2_v = inp("c_cmp_w2_v", [1, 512, 128], need=2 in kinds)
        self.c_w_out = inp("c_w_out", [1, D, D], need=2 in kinds)
        self.outT = nc.dram_tensor("outT", [D, S], F32, kind="ExternalOutput").ap()

        with ExitStack() as ctx:
            fw = FW(nc, ctx)
            self.fw = fw
            self.build()
            fw.barrier()
            fw.emit()
            print("kernel build: instr+waits", fw.ninst, "sems", len(fw.sems), "signals", fw.nsignal)

    def vcol(self, name, k=0, n=1):
        o = self.voff[name] + k
        return self.vecs[:, o:o + n]

    def build(self):
        fw = self.fw
        self.vecs = fw.sbuf([128, self.nvec], F32, "vecs")
        fw.op("sp", lambda e: e.dma_start(out=self.vecs[:, :], in_=self.vecs_d), w=[self.vecs], dsem="vecs")
        self.ones_bf = fw.sbuf([128, 128], BF16, "ones")
        fw.op("pool", lambda e: e.memset(self.ones_bf[:, :], 1.0), w=[self.ones_bf])
        self.cst = fw.sbuf([128, 4], F32, "cst")
        fw.op("pool", lambda e: e.memset(self.cst[:, 0:1], EPS), w=[self.cst])
        fw.op("pool", lambda e: e.memset(self.cst[:, 1:2], 1.0), w=[self.cst])
        fw.op("pool", lambda e: e.memset(self.cst[:, 2:3], 0.25), w=[self.cst])
        if self.jrev is not None:
            self.JREV = fw.sbuf([128, 128], F32, "JREV")
            fw.op("sp", lambda e: e.dma_start(out=self.JREV[:, :], in_=self.jrev), w=[self.JREV], dsem="JREV")
        self.ps = [fw.psum([128, 512], F32, f"psb{i}") for i in range(8)]
        self.psr = Rot(self.ps[:6])
        self.psS = self.ps[6]
        self.psX = self.ps[7]
        self.wcache = {}
        self.wq = "pool"
        self.pref = {}
        self.nwc = 0
        self.wt = Rot([fw.sbuf([128, 16, 512], BF16, f"wt{i}") for i in range(2)])
        hA = fw.dram([D, S], F32, "hA")
        hB = fw.dram([D, S], F32, "hB")
        self.hbufs = {}
        for nm, ap in (("x", self.xT), ("A", hA), ("B", hB), ("out", self.outT)):
            self.hbufs[nm] = (ap, [Buf(None, f"h{nm}{t}") for t in range(NTI)])
        self.tv = fw.dram([16, TV_TOT], F32, "tv")
        self.tvbuf = Buf(None, "tv")
        if any(l in (1, 2) for l in self.layers):
            self.build_tv()
        cur = "x"
        nl = len(self.layers)
        for idx, i in enumerate(self.layers):
            dst = "out" if idx == nl - 1 else ("A" if cur != "A" else "B")
            kind = i % 3
            j = i // 3
            if kind == 0:
                self.layer_rglru(i, j, cur, dst)
            elif kind == 1:
                self.layer_dil(i, cur, dst)
            else:
                self.layer_nsa(i, cur, dst)
            cur = dst

    def build_tv(self):
        fw = self.fw
        with ExitStack() as sub:
            fw.push(sub)
            rb = fw.sbuf([33, 16], F32, "rb")
            fw.op("sp", lambda e: e.dma_start(out=rb[:, :], in_=self.relb), w=[rb], dsem="rb")
            nchunk = (TV_TOT + 511) // 512
            ohs = Rot([fw.sbuf([33, 512], F32, f"ohs{i}") for i in range(2)])
            tvs = Rot([fw.sbuf([16, 512], F32, f"tvs{i}") for i in range(2)])
            for c in range(nchunk):
                c0 = c * 512
                n = min(512, TV_TOT - c0)
                o, oi = ohs.next()
                fw.op("sp", lambda e, o=o, c0=c0, n=n: e.dma_start(out=o[:, :n], in_=self.oh[:, c0:c0 + n]), w=[o], dsem=f"ohs{oi}")
                ps, _ = self.psr.next()
                fw.op("pe", lambda e, o=o, ps=ps, n=n: e.matmul(ps[:16, :n], lhsT=rb[:, :], rhs=o[:, :n], start=True, stop=True), r=[rb, o], w=[ps])
                t, ti = tvs.next()
                fw.op("act", lambda e, t=t, ps=ps, n=n: e.activation(out=t[:, :n], in_=ps[:16, :n], func=AF.Copy), r=[ps], w=[t])
                fw.op("sp", lambda e, t=t, c0=c0, n=n: e.dma_start(out=self.tv[:, c0:c0 + n], in_=t[:, :n]), r=[t], w=[self.tvbuf], dsem=f"tvs{ti}")
            fw.pop()

    def load_toeplitz(self, Wt, kind, h, base_delta, width, Hs, nm, rot):
        fw = self.fw
        o, X = TV_OFF[kind]
        assert base_delta + 384 >= 0 and base_delta + 384 + 127 + width - 1 < X, (kind, base_delta, width)
        start = h * TV_TOT + o + base_delta + 511 - 127
        src = bass.AP(self.tv.tensor, start, [[1, 128], [1, width]])
        fw.op("sp", lambda e: e.dma_start(out=Hs[:, :width], in_=src), w=[Hs], dsem=nm)
        for c0 in range(0, width, 512):
            n = min(512, width - c0)
            ps, _ = rot.next()
            fw.op("pe", lambda e, ps=ps, c0=c0, n=n: e.matmul(ps[:, :n], lhsT=self.JREV[:, :], rhs=Hs[:, c0:c0 + n], start=True, stop=True), r=[self.JREV, Hs], w=[ps])
            fw.op("act", lambda e, ps=ps, c0=c0, n=n: e.activation(out=Wt[:, c0:c0 + n], in_=ps[:, :n], func=AF.Copy), r=[ps], w=[Wt])

    def load_w(self, W, r0, kc, c0, ncols):
        fw = self.fw
        key = (W.tensor.name, int(W.offset), r0, kc, c0, ncols)
        if key in self.pref:
            return self.pref.pop(key)
        wt, wi = self.wt.next()
        if key in self.wcache:
            scr, sb = self.wcache[key]
            fw.op(self.wq, lambda e: e.dma_start(out=wt[:, :kc, :ncols], in_=scr), r=[sb], w=[wt], dsem=f"wt{wi}_{self.wq}")
        else:
            src = W[r0:r0 + kc * 128, c0:c0 + ncols].rearrange("(k p) n -> p k n", p=128)
            fw.op("pool", lambda e: e.dma_start(out=wt[:, :kc, :ncols], in_=src), w=[wt], dsem=f"wt{wi}_pool")
            self.nwc += 1
            t = self.nc.dram_tensor(f"wc{self.nwc}", [128, kc, ncols], BF16, kind="Internal")
            scr = t.ap()
            sb = Buf(None, f"wc{self.nwc}")
            fw.op("sp", lambda e: e.dma_start(out=scr, in_=wt[:, :kc, :ncols]), r=[wt], w=[sb], dsem=f"wts{wi}")
            self.wcache[key] = (scr, sb)
        return wt

    def prefetch_w(self, W, r0, kc, c0, ncols):
        key = (W.tensor.name, int(W.offset), r0, kc, c0, ncols)
        assert key not in self.pref
        wt = self.load_w(W, r0, kc, c0, ncols)
        self.pref[key] = wt

    def prenorm_load(self, src, t, B32, nm="B32"):
        fw = self.fw
        ap, bufs = self.hbufs[src]
        fw.op("sp", lambda e: e.dma_start(out=B32[:, :, :], in_=ap[:, t * NT:(t + 1) * NT].rearrange("(k p) n -> p k n", p=128)),
              r=[bufs[t]], w=[B32], dsem=nm)

    def prenorm(self, i, src, t, B32, U16, tmp, load=True, nm="B32"):
        fw = self.fw
        if load:
            self.prenorm_load(src, t, B32, nm)
        self.rstd_of(B32, tmp)
        rstd = tmp["rstd"]
        for k in range(16):
            g = self.vcol(f"pre{i}", k)
            fw.op("dve", lambda e, k=k, g=g: e.scalar_tensor_tensor(out=U16[:, k, :], in0=B32[:, k, :], scalar=g, in1=rstd[:, :], op0=ALU.mult, op1=ALU.mult),
                  r=[B32, rstd, self.vecs], w=[U16])

    def rstd_of(self, X32, tmp, squares_done=False):
        fw = self.fw
        psS = self.psS
        if not squares_done:
            for k in range(16):
                sq, _ = tmp["sq"].next()
                fw.op("act", lambda e, k=k, sq=sq: e.activation(out=sq[:, :], in_=X32[:, k, :], func=AF.Square), r=[X32], w=[sq])
                fw.op("pe", lambda e, k=k, sq=sq: e.matmul(psS[:, :], lhsT=self.ones_bf[:, :], rhs=sq[:, :], start=(k == 0), stop=(k == 15)),
                      r=[self.ones_bf, sq], w=[psS])
        rstd = tmp["rstd"]
        fw.op("act", lambda e: e.activation(out=rstd[:, :], in_=psS[:, :], func=AF.Sqrt, scale=1.0 / D, bias=self.cst[:, 0:1]), r=[psS, self.cst], w=[rstd])
        fw.op("dve", lambda e: e.reciprocal(out=rstd[:, :], in_=rstd[:, :]), r=[rstd], w=[rstd])

    def linear_fm(self, W, kc, c0, ncols, rhs_buf, evac):
        fw = self.fw
        mi = 0
        for g0 in range(0, ncols, 512):
            gn = min(512, ncols - g0)
            wt = self.load_w(W, 0, kc, c0 + g0, gn)
            for m0 in range(0, gn, 128):
                mw = min(128, gn - m0)
                ps, _ = self.psr.next()
                for k in range(kc):
                    fw.op("pe", lambda e, k=k, m0=m0, mw=mw, ps=ps, wt=wt: e.matmul(ps[:mw, :], lhsT=wt[:, k, m0:m0 + mw], rhs=rhs_buf[:, k, :], start=(k == 0), stop=(k == kc - 1)),
                          r=[wt, rhs_buf], w=[ps])
                evac(mi, mw, ps)
                mi += 1

    def linear_tm(self, W, kc, c0, ncols, lhs_buf, evac):
        fw = self.fw
        for g0 in range(0, ncols, 512):
            gn = min(512, ncols - g0)
            wt = self.load_w(W, 0, kc, c0 + g0, gn)
            for tb in range(NT // 128):
                ps, _ = self.psr.next()
                for k in range(kc):
                    fw.op("pe", lambda e, k=k, tb=tb, ps=ps, wt=wt, gn=gn: e.matmul(ps[:, :gn], lhsT=lhs_buf[:, k, tb * 128:(tb + 1) * 128], rhs=wt[:, k, :gn], start=(k == 0), stop=(k == kc - 1)),
                          r=[wt, lhs_buf], w=[ps])
                evac(tb, g0, gn, ps)

    def post_phase(self, i, w_out, kcy, src, dst, t, Y16, A32, B32, U16, tmp, mid_hook=None):
        fw = self.fw
        psS = self.psS

        def evac_out(mi, mw, ps):
            fw.op("act", lambda e: e.activation(out=A32[:, mi, :], in_=ps[:, :], func=AF.Copy), r=[ps], w=[A32])
            sq, _ = tmp["sq"].next()
            fw.op("dve", lambda e: e.tensor_tensor(out=sq[:, :], in0=ps[:, :], in1=A32[:, mi, :], op=ALU.mult), r=[ps, A32], w=[sq])
            fw.op("pe", lambda e: e.matmul(psS[:, :], lhsT=self.ones_bf[:, :], rhs=sq[:, :], start=(mi == 0), stop=(mi == 15)), r=[self.ones_bf, sq], w=[psS])

        P16 = tmp["P16"]
        fw.op("pool", lambda e: e.dma_start(out=P16[:, :, :], in_=self.pT[i, :, t * NT:(t + 1) * NT].rearrange("(k p) n -> p k n", p=128)), w=[P16], dsem="P16")
        WP = tmp["WP"]
        fw.op("pool", lambda e: e.dma_start(out=WP[:, :, :], in_=self.ple_w_proj[i].rearrange("(k p) n -> p k n", p=128)), w=[WP], dsem="WP")
        self.linear_fm(w_out, kcy, 0, D, Y16, evac_out)
        self.rstd_of(A32, tmp, squares_done=True)
        rstd = tmp["rstd"]
        ap, bufs = self.hbufs[src]
        fw.op("sp", lambda e: e.dma_start(out=B32[:, :, :], in_=ap[:, t * NT:(t + 1) * NT].rearrange("(k p) n -> p k n", p=128)),
              r=[bufs[t]], w=[B32], dsem="B32")
        for k in range(16):
            g = self.vcol(f"post{i}", k)
            fw.op("dve", lambda e, k=k, g=g: e.scalar_tensor_tensor(out=A32[:, k, :], in0=A32[:, k, :], scalar=g, in1=rstd[:, :], op0=ALU.mult, op1=ALU.mult),
                  r=[A32, rstd, self.vecs], w=[A32])
            fw.op("dve" if k % 3 else "pool", lambda e, k=k: e.tensor_tensor(out=B32[:, k, :], in0=B32[:, k, :], in1=A32[:, k, :], op=ALU.add), r=[A32, B32], w=[B32])
            fw.op("act", lambda e, k=k: e.activation(out=U16[:, k, :], in_=B32[:, k, :], func=AF.Copy), r=[B32], w=[U16])
        if mid_hook is not None:
            mid_hook()

        def evac_gate(mi, mw, ps):
            sg, _ = tmp["sg"].next()
            fw.op("act", lambda e: e.activation(out=sg[:, :], in_=ps[:, :], func=AF.Sigmoid), r=[ps], w=[sg])
            ps2 = self.psX
            for kk in range(2):
                fw.op("pe", lambda e, kk=kk: e.matmul(ps2[:, :], lhsT=WP[:, kk, mi * 128:(mi + 1) * 128], rhs=P16[:, kk, :], start=(kk == 0), stop=(kk == 1)), r=[WP, P16], w=[ps2])
            fw.op("dve", lambda e: e.tensor_tensor(out=sg[:, :], in0=sg[:, :], in1=ps2[:, :], op=ALU.mult), r=[sg, ps2], w=[sg])
            fw.op("pool", lambda e: e.tensor_tensor(out=B32[:, mi, :], in0=B32[:, mi, :], in1=sg[:, :], op=ALU.add), r=[B32, sg], w=[B32])

        self.linear_fm(self.ple_w_gate[i], 16, 0, D, U16, evac_gate)
        apd, bufd = self.hbufs[dst]
        fw.op("sp", lambda e: e.dma_start(out=apd[:, t * NT:(t + 1) * NT].rearrange("(k p) n -> p k n", p=128), in_=B32[:, :, :]),
              r=[B32], w=[bufd[t]], dsem="B32")

    def common_tiles(self):
        fw = self.fw
        tmp = {}
        tmp["sq"] = Rot([fw.sbuf([128, 512], BF16, f"sq{i}") for i in range(2)])
        tmp["rstd"] = fw.sbuf([128, 512], F32, "rstd")
        tmp["sg"] = Rot([fw.sbuf([128, 512], F32, f"sg{i}") for i in range(2)])
        tmp["P16"] = fw.sbuf([128, 2, 512], BF16, "P16")
        tmp["WP"] = fw.sbuf([128, 2, D], BF16, "WP")
        return tmp

    def layer_rglru(self, i, j, src, dst):
        fw = self.fw
        self.wq = "sp"
        with ExitStack() as sub:
            fw.push(sub)
            tmp = self.common_tiles()
            XB = fw.sbuf([128, 16, 3 + 512], F32, "XB")
            A32 = View(XB, XB.t[:, :, 3:515])
            B32 = fw.sbuf([128, 16, 512], F32, "B32")
            U16 = fw.sbuf([128, 16, 512], BF16, "U16")
            G16 = fw.sbuf([128, 16, 512], BF16, "G16")
            Y16 = G16
            WR = fw.sbuf([128, 8, 2, 256], BF16, "WR")
            WI = fw.sbuf([128, 8, 2, 256], BF16, "WI")
            c8 = fw.sbuf([128, 32], F32, "c8")
            carry = fw.sbuf([128, 16], F32, "carry")
            XC = Rot([fw.sbuf([128, 2, 512], F32, f"XC{q}") for q in range(2)])
            XCB = Rot([fw.sbuf([128, 2, 512], BF16, f"XCB{q}") for q in range(2)])
            small = {nm: Rot([fw.sbuf([128, 512], F32, f"{nm}{q}") for q in range(2 if nm == "HH" else 4)]) for nm in ("R", "I", "Ss", "HH")}
            fw.op("pool", lambda e: e.dma_start(out=WR[:, :, :, :], in_=self.a_w_r[j].rearrange("n (k p) m -> p n k m", p=128)), w=[WR], dsem="WR")
            fw.op("pool", lambda e: e.dma_start(out=WI[:, :, :, :], in_=self.a_w_i[j].rearrange("n (k p) m -> p n k m", p=128)), w=[WI], dsem="WI")
            lam = self.vcol(f"lam{j}", 0, 16)
            fw.op("act", lambda e: e.activation(out=c8[:, 0:16], in_=lam, func=AF.Exp, scale=-1.0), r=[self.vecs], w=[c8])
            fw.op("act", lambda e: e.activation(out=c8[:, 0:16], in_=c8[:, 0:16], func=AF.Ln, bias=self.cst[:, 1:2]), r=[c8, self.cst], w=[c8])
            fw.op("dve", lambda e: e.tensor_scalar(out=c8[:, 0:16], in0=c8[:, 0:16], scalar1=-4.0, scalar2=None, op0=ALU.mult), r=[c8], w=[c8])
            hb = fw.sbuf([128, 32], F32, "hb")
            fw.op("dve", lambda e: e.tensor_scalar(out=hb[:, 0:16], in0=self.vcol(f"br{j}", 0, 16), scalar1=0.5, scalar2=None, op0=ALU.mult), r=[self.vecs], w=[hb])
            fw.op("dve", lambda e: e.tensor_scalar(out=hb[:, 16:32], in0=self.vcol(f"bi{j}", 0, 16), scalar1=0.5, scalar2=None, op0=ALU.mult), r=[self.vecs], w=[hb])
            fw.op("pool", lambda e: e.memset(carry[:, :], 0.0), w=[carry])
            fw.op("pool", lambda e: e.memset(XB[:, :, 0:3], 0.0), w=[XB])
            W = self.a_w_in[j]
            NTL_ = min(NTI, DBG_TILES)
            self.prenorm_load(src, 0, A32, "XBh")
            for t in range(NTL_):
                self.prenorm(i, src, t, A32, U16, tmp, load=False)

                def evac_x(mi, mw, ps):
                    fw.op("act", lambda e: e.activation(out=XB[:, mi, 3:], in_=ps[:, :], func=AF.Copy), r=[ps], w=[XB])

                def evac_g(mi, mw, ps):
                    fw.op("act", lambda e: e.activation(out=G16[:, mi, :], in_=ps[:, :], func=AF.Silu), r=[ps], w=[G16])

                self.linear_fm(W, 16, 0, D, U16, evac_x)
                pending = []
                for gg in range(4):
                    self.linear_fm(W, 16, D + gg * 512, 512, U16, lambda mi, mw, ps, gg=gg: evac_g(4 * gg + mi, mw, ps))
                    if gg < 3:
                        self.prefetch_w(W, 0, 16, D + (gg + 1) * 512, 512)
                    else:
                        self.prefetch_w(self.a_w_out[j], 0, 16, 0, 512)
                    for n in (2 * gg, 2 * gg + 1):
                        xc, _ = XC.next()
                        xcb, _ = XCB.next()
                        for mm in range(2):
                            ch = 2 * n + mm
                            fw.op("dve", lambda e, ch=ch, mm=mm, xc=xc: e.tensor_scalar(out=xc[:, mm, :], in0=XB[:, ch, 3:515], scalar1=self.vcol(f"cw{j}_3", ch), scalar2=self.vcol(f"cb{j}", ch), op0=ALU.mult, op1=ALU.add),
                                  r=[XB, self.vecs], w=[xc])
                            for tap in range(3):
                                fw.op("dve", lambda e, ch=ch, mm=mm, xc=xc, tap=tap: e.scalar_tensor_tensor(out=xc[:, mm, :], in0=XB[:, ch, tap:tap + 512], scalar=self.vcol(f"cw{j}_{tap}", ch), in1=xc[:, mm, :], op0=ALU.mult, op1=ALU.add),
                                      r=[XB, self.vecs, xc], w=[xc])
                        fw.op("pool", lambda e, xc=xc, xcb=xcb: e.tensor_copy(out=xcb[:, :, :], in_=xc[:, :, :]), r=[xc], w=[xcb])
                        chunks = []
                        for mm in range(2):
                            ch = 2 * n + mm
                            psr_, _ = self.psr.next()
                            psi_, _ = self.psr.next()
                            for kk in range(2):
                                fw.op("pe", lambda e, kk=kk, mm=mm, n=n, xcb=xcb, p=psr_: e.matmul(p[:, :], lhsT=WR[:, n, kk, mm * 128:(mm + 1) * 128], rhs=xcb[:, kk, :], start=(kk == 0), stop=(kk == 1)), r=[WR, xcb], w=[psr_])
                            for kk in range(2):
                                fw.op("pe", lambda e, kk=kk, mm=mm, n=n, xcb=xcb, p=psi_: e.matmul(p[:, :], lhsT=WI[:, n, kk, mm * 128:(mm + 1) * 128], rhs=xcb[:, kk, :], start=(kk == 0), stop=(kk == 1)), r=[WI, xcb], w=[psi_])
                            cR = small["R"].next()[0]
                            cI = small["I"].next()[0]
                            chunks.append(dict(ch=ch, mm=mm, psr=psr_, psi=psi_, R=cR, I=cI, Aa=cR, Ss=small["Ss"].next()[0], BT=cI, HH=small["HH"].next()[0]))
                        for c in chunks:
                            fw.op("act", lambda e, c=c: e.activation(out=c["R"][:, :], in_=c["psr"][:, :], func=AF.Tanh, scale=0.5, bias=hb[:, c["ch"]:c["ch"] + 1]), r=[c["psr"], hb], w=[c["R"]])
                            fw.op("act", lambda e, c=c: e.activation(out=c["I"][:, :], in_=c["psi"][:, :], func=AF.Tanh, scale=0.5, bias=hb[:, 16 + c["ch"]:17 + c["ch"]]), r=[c["psi"], hb], w=[c["I"]])
                        for c in chunks:
                            fw.op("act", lambda e, c=c: e.activation(out=c["Aa"][:, :], in_=c["R"][:, :], func=AF.Exp, scale=c8[:, c["ch"]:c["ch"] + 1], bias=c8[:, c["ch"]:c["ch"] + 1]), r=[c["R"], c8], w=[c["Aa"]])
                        for c in chunks:
                            fw.op("dve", lambda e, c=c: e.tensor_tensor(out=c["Ss"][:, :], in0=c["Aa"][:, :], in1=c["Aa"][:, :], op=ALU.mult), r=[c["Aa"]], w=[c["Ss"]])
                            fw.op("dve", lambda e, c=c, xc=xc: e.scalar_tensor_tensor(out=c["BT"][:, :], in0=c["I"][:, :], scalar=1.0, in1=xc[:, c["mm"], :], op0=ALU.add, op1=ALU.mult), r=[c["I"], xc], w=[c["BT"]])

                        def stage_b(chunks=chunks):
                            for c in chunks:
                                fw.op("act", lambda e, c=c: e.activation(out=c["Ss"][:, :], in_=c["Ss"][:, :], func=AF.Sqrt, scale=-0.25, bias=self.cst[:, 2:3]), r=[c["Ss"], self.cst], w=[c["Ss"]])
                            for c in chunks:
                                fw.op("pool", lambda e, c=c: e.tensor_tensor(out=c["BT"][:, :], in0=c["BT"][:, :], in1=c["Ss"][:, :], op=ALU.mult), r=[c["Ss"], c["BT"]], w=[c["BT"]])
                                fw.op("dve", lambda e, c=c: e.tensor_tensor_scan(out=c["HH"][:, :], data0=c["Aa"][:, :], data1=c["BT"][:, :], initial=carry[:, c["ch"]:c["ch"] + 1], op0=ALU.mult, op1=ALU.add), r=[c["Aa"], c["BT"], carry], w=[c["HH"]])
                                fw.op("dve", lambda e, c=c: e.tensor_copy(out=carry[:, c["ch"]:c["ch"] + 1], in_=c["HH"][:, 511:512]), r=[c["HH"]], w=[carry])
                                fw.op("pool", lambda e, c=c: e.tensor_tensor(out=Y16[:, c["ch"], :], in0=c["HH"][:, :], in1=G16[:, c["ch"], :], op=ALU.mult), r=[c["HH"], G16], w=[Y16])

                        if pending:
                            pending.pop()()
                        pending.append(stage_b)
                while pending:
                    pending.pop()()
                fw.op("dve", lambda e: e.tensor_copy(out=XB[:, :, 0:3], in_=XB[:, :, 512:515]), r=[XB], w=[XB])
                self.post_phase(i, self.a_w_out[j], 16, src, dst, t, Y16, A32, B32, U16, tmp,
                                mid_hook=(lambda t=t: self.prenorm_load(src, t + 1, A32, "XBh")) if t + 1 < NTL_ else None)
            fw.pop()
        self.wq = "pool"

    def inproj_phase(self, i, src, W, segs):
        fw = self.fw
        with ExitStack() as sub:
            fw.push(sub)
            tmp = self.common_tiles()
            B32s = [fw.sbuf([128, 16, 512], F32, f"B32{q}") for q in range(2)]
            U16 = fw.sbuf([128, 16, 512], BF16, "U16")
            stage = Rot([fw.sbuf([128, 512], BF16, f"stg{q}") for q in range(3)])
            stage32 = Rot([fw.sbuf([128, 512], F32, f"stgf{q}") for q in range(2)])
            flip = [0]
            NTL_ = min(NTI, DBG_TILES)
            self.prenorm_load(src, 0, B32s[0], "B32i0")
            for t in range(NTL_):
                if t + 1 < NTL_:
                    self.prenorm_load(src, t + 1, B32s[(t + 1) % 2], f"B32i{(t + 1) % 2}")
                self.prenorm(i, src, t, B32s[t % 2], U16, tmp, load=False)
                for mode, c0, ncols, func, dap, d0 in segs:
                    if mode == "fm":
                        def ev(mi, mw, ps, func=func, dap=dap, d0=d0, t=t):
                            if dap.dtype == F32:
                                st, si = stage32.next()
                                nm = f"stgf{si}"
                            else:
                                st, si = stage.next()
                                nm = f"stg{si}"
                            flip[0] ^= 1
                            if func == AF.Copy and flip[0]:
                                fw.op("dve", lambda e: e.tensor_copy(out=st[:mw, :], in_=ps[:mw, :]), r=[ps], w=[st])
                            else:
                                fw.op("act", lambda e: e.activation(out=st[:mw, :], in_=ps[:mw, :], func=func), r=[ps], w=[st])
                            fw.op("sp", lambda e: e.dma_start(out=dap[d0 + mi * 128:d0 + mi * 128 + mw, t * NT:(t + 1) * NT], in_=st[:mw, :]), r=[st], dsem=nm)
                        self.linear_fm(W, 16, c0, ncols, U16, ev)
                    else:
                        def ev(tb, g0, gn, ps, dap=dap, d0=d0, t=t):
                            st, si = stage.next()
                            flip[0] ^= 1
                            if flip[0]:
                                fw.op("dve", lambda e: e.tensor_copy(out=st[:, :gn], in_=ps[:, :gn]), r=[ps], w=[st])
                            else:
                                fw.op("act", lambda e: e.activation(out=st[:, :gn], in_=ps[:, :gn], func=AF.Copy), r=[ps], w=[st])
                            fw.op("sp", lambda e: e.dma_start(out=dap[t * NT + tb * 128:t * NT + (tb + 1) * 128, d0 + g0:d0 + g0 + gn], in_=st[:, :gn]), r=[st], dsem=f"stg{si}")
                        self.linear_tm(W, 16, c0, ncols, U16, ev)
            fw.pop()

    def post_all(self, i, w_out, kcy, yT, src, dst):
        fw = self.fw
        with ExitStack() as sub:
            fw.push(sub)
            tmp = self.common_tiles()
            A32 = fw.sbuf([128, 16, 512], F32, "A32")
            B32 = fw.sbuf([128, 16, 512], F32, "B32")
            U16 = fw.sbuf([128, 16, 512], BF16, "U16")
            Y16s = [fw.sbuf([128, kcy, 512], BF16, f"Y16{q}") for q in range(2)]
            NTL_ = min(NTI, DBG_TILES)

            def ld(t):
                Y16 = Y16s[t % 2]
                fw.op("sp", lambda e: e.dma_start(out=Y16[:, :, :], in_=yT[:, t * NT:(t + 1) * NT].rearrange("(k p) n -> p k n", p=128)), w=[Y16], dsem=f"Y16{t % 2}")

            ld(0)
            for t in range(NTL_):
                if t + 1 < NTL_:
                    ld(t + 1)
                self.post_phase(i, w_out, kcy, src, dst, t, Y16s[t % 2], A32, B32, U16, tmp)
            fw.pop()

    def layer_dil(self, i, src, dst):
        fw = self.fw
        W = self.b_w_in[0]
        qkT = fw.dram([6144, S], BF16, "qkT")
        Vd = fw.dram([S, 3072], BF16, "Vd")
        sgT = fw.dram([1024, S], BF16, "sgTd")
        yT = fw.dram([1024, S], BF16, "yTd")
        segs = []
        for g in range(3):
            segs.append(("fm", g * 3072, 1024, AF.Copy, qkT, g * 2048))
            segs.append(("fm", g * 3072 + 1024, 1024, AF.Copy, qkT, g * 2048 + 1024))
            segs.append(("tm", g * 3072 + 2048, 1024, None, Vd, g * 1024))
        segs.append(("fm", 9216, 1024, AF.Silu, sgT, 0))
        self.inproj_phase(i, src, W, segs)
        with ExitStack() as sub:
            fw.push(sub)
            QT2 = [fw.sbuf([64, S], BF16, f"QT{q}") for q in range(2)]
            KT2 = [fw.sbuf([64, S], BF16, f"KT{q}") for q in range(2)]
            VT2 = [fw.sbuf([128, 32, 64], BF16, f"VT{q}") for q in range(2)]
            SG = fw.sbuf([64, S], BF16, "SG")
            YO = fw.sbuf([64, S], BF16, "YO")
            UA = fw.sbuf([64, S], F32, "UA")
            DA = fw.sbuf([64, S], F32, "DA")
            BTz2 = [fw.sbuf([128, 1024], F32, f"BTz{g}") for g in range(2)]
            HS = fw.sbuf([128, 1024], F32, "HSd")
            TMP = Rot([fw.sbuf([128, 512], F32, f"TMP{q}") for q in range(2)])
            PT = Rot([fw.sbuf([128, 512], BF16, f"PT{q}") for q in range(6)])
            rotS = Rot(self.ps[0:4])
            rotU = Rot(self.ps[4:6])
            rotD = Rot(self.ps[6:8])
            nheads = DBG_HEADS
            LA = 3
            dils = (1, 4, 16)

            def load_g(h, g, slot):
                d = dils[g]
                L = S // d
                nblk = L // 128
                self.load_toeplitz(BTz2[slot], f"dil{g}", h, -384, 1024, HS, "HSd", rotS)
                fw.op("sp", lambda e: e.dma_start(out=QT2[slot][:, :], in_=qkT[g * 2048 + h * 64:g * 2048 + (h + 1) * 64, :]), w=[QT2[slot]], dsem=f"QT{slot}")
                fw.op("sp", lambda e: e.dma_start(out=KT2[slot][:, :], in_=qkT[g * 2048 + 1024 + h * 64:g * 2048 + 1024 + (h + 1) * 64, :]), w=[KT2[slot]], dsem=f"KT{slot}")
                vsrc = Vd[:, g * 1024 + h * 64:g * 1024 + (h + 1) * 64].rearrange("(jb p dd) c -> dd p jb c", p=128, dd=d)
                for r in range(d):
                    fw.op("sp", lambda e, r=r: e.dma_start(out=VT2[slot][:, r * nblk:(r + 1) * nblk, :], in_=vsrc[r]), w=[VT2[slot]], dsem=f"VT{slot}")

            hg = [(h, g) for h in range(nheads) for g in range(3)]
            load_g(0, 0, 0)
            for hgi, (h, g) in enumerate(hg):
                slot = hgi % 2
                if g == 0:
                    fw.op("sp", lambda e, h=h: e.dma_start(out=SG[:, :], in_=sgT[h * 64:(h + 1) * 64, :]), w=[SG], dsem="SG")
                if hgi + 1 < len(hg):
                    load_g(hg[hgi + 1][0], hg[hgi + 1][1], 1 - slot)
                d = dils[g]
                L = S // d
                nblk = L // 128
                Nq = min(256, L)
                QT, KT, VT, BT = QT2[slot], KT2[slot], VT2[slot], BTz2[slot]
                items = []
                for r in range(d):
                    for q0 in range(0, L, Nq):
                        kbs = [kb for kb in range(q0 // 128 - 1, (q0 + Nq) // 128) if kb >= 0]
                        grp = {"qcols": slice(r + d * q0, r + d * (q0 + Nq - 1) + 1, d)}
                        for ix, kb in enumerate(kbs):
                            items.append((grp, r, q0, kb, ix == 0, ix == len(kbs) - 1))

                def s_stage(it, d=d, Nq=Nq, KT=KT, QT=QT, BT=BT):
                    grp, r, q0, kb, first, last = it
                    qcols = grp["qcols"]
                    psS, _ = rotS.next()
                    kcols = slice(r + d * kb * 128, r + d * (kb * 128 + 127) + 1, d)
                    boff = q0 - kb * 128 + 384
                    tm, _ = TMP.next()
                    pt, _ = PT.next()
                    fw.op("pe", lambda e: e.matmul(psS[:, :Nq], lhsT=KT[:, kcols], rhs=QT[:, qcols], start=True, stop=True), r=[KT, QT], w=[psS])
                    fw.op("dve", lambda e: e.scalar_tensor_tensor(out=tm[:, :Nq], in0=psS[:, :Nq], scalar=0.125, in1=BT[:, boff:boff + Nq], op0=ALU.mult, op1=ALU.add), r=[psS, BT], w=[tm])
                    fw.op("act", lambda e: e.activation(out=pt[:, :Nq], in_=tm[:, :Nq], func=AF.Exp), r=[tm], w=[pt])
                    return pt

                def pv_stage(it, pt, Nq=Nq, VT=VT, g=g, nblk=nblk):
                    grp, r, q0, kb, first, last = it
                    qcols = grp["qcols"]
                    if first:
                        grp["psU"], _ = rotU.next()
                        grp["psD"], _ = rotD.next()
                    psU, psD = grp["psU"], grp["psD"]
                    blk = r * nblk + kb
                    fw.op("pe", lambda e: e.matmul(psU[:64, :Nq], lhsT=VT[:, blk, :], rhs=pt[:, :Nq], start=first, stop=last), r=[VT, pt], w=[psU])
                    fw.op("pe", lambda e: e.matmul(psD[:64, :Nq], lhsT=self.ones_bf[:, :64], rhs=pt[:, :Nq], start=first, stop=last), r=[self.ones_bf, pt], w=[psD])
                    if last:
                        if g == 0:
                            fw.op("act", lambda e: e.activation(out=UA[:, qcols], in_=psU[:64, :Nq], func=AF.Copy), r=[psU], w=[UA])
                            fw.op("dve", lambda e: e.tensor_copy(out=DA[:, qcols], in_=psD[:64, :Nq]), r=[psD], w=[DA])
                        else:
                            fw.op("dve", lambda e: e.tensor_tensor(out=UA[:, qcols], in0=UA[:, qcols], in1=psU[:64, :Nq], op=ALU.add), r=[psU, UA], w=[UA])
                            fw.op("dve", lambda e: e.tensor_tensor(out=DA[:, qcols], in0=DA[:, qcols], in1=psD[:64, :Nq], op=ALU.add), r=[psD, DA], w=[DA])

                pts = {}
                n = len(items)
                for ii in range(n + LA):
                    if ii < n:
                        pts[ii] = s_stage(items[ii])
                    if ii >= LA:
                        pv_stage(items[ii - LA], pts.pop(ii - LA))
                if g < 2:
                    continue
                fw.op("dve", lambda e: e.reciprocal(out=DA[:, :], in_=DA[:, :]), r=[DA], w=[DA])
                fw.op("pool", lambda e: e.tensor_tensor(out=UA[:, :], in0=UA[:, :], in1=DA[:, :], op=ALU.mult), r=[UA, DA], w=[UA])
                fw.op("pool", lambda e: e.tensor_tensor(out=YO[:, :], in0=UA[:, :], in1=SG[:, :], op=ALU.mult), r=[UA, SG], w=[YO])
                fw.op("sp", lambda e, h=h: e.dma_start(out=yT[h * 64:(h + 1) * 64, :], in_=YO[:, :]), r=[YO], dsem="YO")
            fw.pop()
        self.post_all(i, self.b_w_out[0], 8, yT, src, dst)

    def layer_nsa(self, i, src, dst):
        fw = self.fw
        W = self.c_w_in[0]
        SC = 128.0 ** -0.5
        BIGS = 30000.0 / SC
        qT = fw.dram([2048, S], BF16, "qTn")
        kvcT = fw.dram([1024, S], BF16, "kvcT")
        ksT = fw.dram([512, S], BF16, "ksT")
        kwT = fw.dram([512, S], BF16, "kwT")
        vsD = fw.dram([S, 512], BF16, "vsD")
        vwD = fw.dram([S, 512], BF16, "vwD")
        bgT = fw.dram([48, S], F32, "bgT")
        sgT = fw.dram([2048, S], BF16, "sgTn")
        yT = fw.dram([2048, S], BF16, "yTn")
        ocT = fw.dram([2048, S], F32, "ocT")
        negT = fw.dram([4, 64, S], BF16, "negT")
        segs = [("fm", 0, 2048, AF.Copy, qT, 0), ("fm", 2048, 1024, AF.Copy, kvcT, 0), ("fm", 3072, 512, AF.Copy, ksT, 0),
                ("tm", 3584, 512, None, vsD, 0), ("fm", 4096, 512, AF.Copy, kwT, 0), ("tm", 4608, 512, None, vwD, 0),
                ("fm", 5120, 48, AF.Sigmoid, bgT, 0), ("fm", 5168, 2048, AF.Silu, sgT, 0)]
        self.inproj_phase(i, src, W, segs)
        NTL = min(NTI, DBG_TILES)
        with ExitStack() as outer:
            fw.push(outer)
            KCMP = fw.sbuf([128, 4, 256], BF16, "KCMP")
            VCMP = fw.sbuf([128, 4, 2, 128], BF16, "VCMP")
            with ExitStack() as sub:
                fw.push(sub)
                KC = fw.sbuf([128, 4, S], BF16, "KC")
                W1 = fw.sbuf([128, 32, 512], BF16, "W1")
                W2 = fw.sbuf([128, 4, 128], BF16, "W2")
                KCP = Rot([fw.sbuf([128, 1020], BF16, f"KCP{q}") for q in range(3)])
                HG = fw.sbuf([128, 4, 1020], BF16, "HG")
                tA = fw.sbuf([128, 1020], F32, "tA")
                tB = fw.sbuf([128, 1020], F32, "tB")
                for which in range(2):
                    w1 = (self.c_w1_k, self.c_w1_v)[which][0]
                    w2 = (self.c_w2_k, self.c_w2_v)[which][0]
                    posn = ("posk", "posv")[which]
                    fw.op("sp", lambda e, which=which: e.dma_start(out=KC[:, :, :], in_=kvcT[which * 512:(which + 1) * 512, :].rearrange("(g p) n -> p g n", p=128)), w=[KC], dsem="KC")
                    fw.op("pool", lambda e, w1=w1: e.dma_start(out=W1[:, :, :], in_=w1.rearrange("(l p) n -> p l n", p=128)), w=[W1], dsem="W1")
                    fw.op("pool", lambda e, w2=w2: e.dma_start(out=W2[:, :, :], in_=w2.rearrange("(m p) n -> p m n", p=128)), w=[W2], dsem="W2")
                    for m in range(4):
                        psA, psB = self.ps[0], self.ps[1]
                        for l in range(32):
                            kcp, _ = KCP.next()
                            fw.op("dve", lambda e, kcp=kcp, l=l, posn=posn: e.tensor_scalar(out=kcp.t[:, :].rearrange("p (g c) -> p g c", g=4), in0=KC[:, :, l:l + 16 * 254 + 1:16], scalar1=self.vcol(posn, l), scalar2=None, op0=ALU.add),
                                  r=[KC, self.vecs], w=[kcp])
                            fw.op("pe", lambda e, kcp=kcp, l=l, m=m: e.matmul(psA[:, :510], lhsT=W1[:, l, m * 128:(m + 1) * 128], rhs=kcp[:, 0:510], start=(l == 0), stop=(l == 31)), r=[W1, kcp], w=[psA])
                            fw.op("pe", lambda e, kcp=kcp, l=l, m=m: e.matmul(psB[:, :510], lhsT=W1[:, l, m * 128:(m + 1) * 128], rhs=kcp[:, 510:1020], start=(l == 0), stop=(l == 31)), r=[W1, kcp], w=[psB])
                        for half, ps in enumerate((psA, psB)):
                            hs = slice(half * 510, (half + 1) * 510)
                            fw.op("act", lambda e, ps=ps, hs=hs: e.activation(out=tA[:, hs], in_=ps[:, :510], func=AF.Square), r=[ps], w=[tA])
                            fw.op("dve", lambda e, hs=hs: e.tensor_scalar(out=tA[:, hs], in0=tA[:, hs], scalar1=0.044715, scalar2=1.0, op0=ALU.mult, op1=ALU.add), r=[tA], w=[tA])
                            fw.op("dve", lambda e, ps=ps, hs=hs: e.tensor_tensor(out=tA[:, hs], in0=tA[:, hs], in1=ps[:, :510], op=ALU.mult), r=[tA, ps], w=[tA])
                            fw.op("act", lambda e, hs=hs: e.activation(out=tB[:, hs], in_=tA[:, hs], func=AF.Sigmoid, scale=1.5957691216057308), r=[tA], w=[tB])
                            fw.op("dve", lambda e, ps=ps, hs=hs, m=m: e.tensor_tensor(out=HG[:, m, hs], in0=tB[:, hs], in1=ps[:, :510], op=ALU.mult), r=[tB, ps], w=[HG])
                    if which == 0:
                        for half in range(2):
                            ps = self.ps[2 + half]
                            for m in range(4):
                                fw.op("pe", lambda e, ps=ps, m=m, half=half: e.matmul(ps[:, :510], lhsT=W2[:, m, :], rhs=HG[:, m, half * 510:(half + 1) * 510], start=(m == 0), stop=(m == 3)), r=[W2, HG], w=[ps])
                            for gg in range(2):
                                fw.op("act", lambda e, ps=ps, half=half, gg=gg: e.activation(out=KCMP[:, 2 * half + gg, 0:255], in_=ps[:, gg * 255:(gg + 1) * 255], func=AF.Copy), r=[ps], w=[KCMP])
                    else:
                        for g in range(4):
                            for cb in range(2):
                                csz = 128 if cb == 0 else 127
                                ps, _ = self.psr.next()
                                for m in range(4):
                                    fw.op("pe", lambda e, ps=ps, m=m, g=g, cb=cb, csz=csz: e.matmul(ps[:csz, :128], lhsT=HG[:, m, g * 255 + cb * 128:g * 255 + cb * 128 + csz], rhs=W2[:, m, :], start=(m == 0), stop=(m == 3)), r=[W2, HG], w=[ps])
                                fw.op("act", lambda e, ps=ps, g=g, cb=cb, csz=csz: e.activation(out=VCMP[:csz, g, cb, :], in_=ps[:csz, :128], func=AF.Copy), r=[ps], w=[VCMP])
                fw.pop()
            with ExitStack() as sub:
                fw.push(sub)
                QG = fw.sbuf([128, 4, S], BF16, "QG")
                CM = fw.sbuf([128, 8, 2, 512], F32, "CM")
                COV = fw.sbuf([128, 2, 64], BF16, "COV")
                ADDT = fw.sbuf([128, 32, 64], F32, "ADDT")
                IDENT = fw.sbuf([128, 128], BF16, "IDENT")
                PN = fw.sbuf([128, 4, 2, 512], BF16, "PN")
                TMP = Rot([fw.sbuf([128, 512], F32, f"TMPc{q}") for q in range(2)])
                PT = Rot([fw.sbuf([128, 512], BF16, f"PTc{q}") for q in range(6)])
                RD = Rot([fw.sbuf([128, 512], F32, f"RDc{q}") for q in range(2)])
                GT = Rot([fw.sbuf([128, 512], F32, f"GTc{q}") for q in range(2)])
                OST = Rot([fw.sbuf([128, 512], F32, f"OST{q}") for q in range(2)])
                NEGS = Rot([fw.sbuf([64, 512], BF16, f"NEGS{q}") for q in range(2)])
                IMP = fw.sbuf([128, 64], F32, "IMP")
                WK = fw.sbuf([128, 64], F32, "WK")
                M8 = fw.sbuf([128, 16], F32, "M8")
                SEL = fw.sbuf([128, 64], F32, "SEL")
                NS = Rot([fw.sbuf([128, 64], BF16, f"NS{q}") for q in range(2)])
                fw.op("sp", lambda e: e.dma_start(out=CM[:, :, :, :], in_=self.cm), w=[CM], dsem="CM")
                fw.op("sp", lambda e: e.dma_start(out=ADDT[:, :, :], in_=self.addt), w=[ADDT], dsem="ADDT")
                fw.op("pool", lambda e: e.dma_start(out=COV[:, :, :], in_=self.cov), w=[COV], dsem="COV")
                fw.op("pool", lambda e: e.dma_start(out=IDENT[:, :], in_=self.ident), w=[IDENT], dsem="IDENT")
                rotS = Rot(self.ps[0:2])
                rotU = Rot(self.ps[2:4])
                rotD = Rot(self.ps[4:6])
                psI, psT = self.ps[6], self.ps[7]
                for g in range(4):
                    fw.op("sp", lambda e, g=g: e.dma_start(out=QG[:, :, :], in_=qT[g * 512:(g + 1) * 512, :].rearrange("(r p) n -> p r n", p=128)), w=[QG], dsem="QG")
                    for j in range(NTL):
                        tc = slice(j * NT, (j + 1) * NT)
                        cbs = [0] if j < 4 else [0, 1]
                        items = []
                        for r in range(4):
                            grp = {"r": r, "pts": []}
                            for ix, cb in enumerate(cbs):
                                items.append((grp, cb, ix == 0, ix == len(cbs) - 1))

                        def s_stage(it, g=g, j=j, tc=tc):
                            grp, cb, first, last = it
                            r = grp["r"]
                            csz = 128 if cb == 0 else 127
                            psS, _ = rotS.next()
                            tm, _ = TMP.next()
                            pt, _ = PT.next()
                            grp["pts"].append(pt)
                            fw.op("pe", lambda e: e.matmul(psS[:csz, :], lhsT=KCMP[:, g, cb * 128:cb * 128 + csz], rhs=QG[:, r, tc], start=True, stop=True), r=[KCMP, QG], w=[psS])
                            fw.op("dve", lambda e: e.scalar_tensor_tensor(out=tm[:csz, :], in0=psS[:csz, :], scalar=SC, in1=CM[:csz, j, cb, :], op0=ALU.mult, op1=ALU.add), r=[psS, CM], w=[tm])
                            fw.op("act", lambda e: e.activation(out=pt[:csz, :], in_=tm[:csz, :], func=AF.Exp), r=[tm], w=[pt])
                            return pt

                        def pv_stage(it, pt, g=g, j=j, tc=tc, cbs=cbs):
                            grp, cb, first, last = it
                            r = grp["r"]
                            h = 4 * g + r
                            csz = 128 if cb == 0 else 127
                            if first:
                                grp["psU"], _ = rotU.next()
                                grp["psD"], _ = rotD.next()
                            psU_, psD_ = grp["psU"], grp["psD"]
                            fw.op("pe", lambda e: e.matmul(psU_[:, :], lhsT=VCMP[:csz, g, cb, :], rhs=pt[:csz, :], start=first, stop=last), r=[VCMP, pt], w=[psU_])
                            fw.op("pe", lambda e: e.matmul(psD_[:, :], lhsT=self.ones_bf[:csz, :], rhs=pt[:csz, :], start=first, stop=last), r=[self.ones_bf, pt], w=[psD_])
                            if not last:
                                return
                            rd, _ = RD.next()
                            fw.op("dve", lambda e: e.tensor_scalar(out=rd[:, :], in0=psD_[:, :], scalar1=1e-30, scalar2=None, op0=ALU.max), r=[psD_], w=[rd])
                            fw.op("dve", lambda e: e.reciprocal(out=rd[:, :], in_=rd[:, :]), r=[rd], w=[rd])
                            for ix, cb2 in enumerate(cbs):
                                csz2 = 128 if cb2 == 0 else 127
                                p2 = grp["pts"][ix]
                                fw.op("pool", lambda e, p2=p2, cb2=cb2, csz2=csz2: e.tensor_tensor(out=PN[:csz2, r, cb2, :], in0=p2[:csz2, :], in1=rd[:csz2, :], op=ALU.mult), r=[p2, rd], w=[PN])
                            gt, gi = GT.next()
                            fw.op("sp", lambda e: e.dma_start(out=gt[:, :], in_=bass.AP(bgT.tensor, (h * 3 + 0) * S + j * NT, [[0, 128], [1, NT]])), w=[gt], dsem=f"GTc{gi}")
                            ost, oi = OST.next()
                            fw.op("pool", lambda e: e.tensor_tensor(out=gt[:, :], in0=gt[:, :], in1=rd[:, :], op=ALU.mult), r=[gt, rd], w=[gt])
                            fw.op("dve", lambda e: e.tensor_tensor(out=ost[:, :], in0=gt[:, :], in1=psU_[:, :], op=ALU.mult), r=[gt, psU_], w=[ost])
                            fw.op("sp", lambda e: e.dma_start(out=ocT[h * 128:(h + 1) * 128, tc], in_=ost[:, :]), r=[ost], dsem=f"OST{oi}")

                        LAc = 1
                        ptd = {}
                        for ii in range(len(items) + LAc):
                            if ii < len(items):
                                ptd[ii] = s_stage(items[ii])
                            if ii >= LAc:
                                pv_stage(items[ii - LAc], ptd.pop(ii - LAc))
                        for qb in range(4):
                            n_mm = 4 * len(cbs)
                            ix = 0
                            for r in range(4):
                                for cb in cbs:
                                    csz = 128 if cb == 0 else 127
                                    fw.op("pe", lambda e, r=r, cb=cb, csz=csz, qb=qb, ix=ix, n_mm=n_mm: e.matmul(psI[:, :64], lhsT=PN[:csz, r, cb, qb * 128:(qb + 1) * 128], rhs=COV[:csz, cb, :], start=(ix == 0), stop=(ix == n_mm - 1)), r=[PN, COV], w=[psI])
                                    ix += 1
                            fw.op("dve", lambda e, j=j, qb=qb: e.tensor_tensor(out=IMP[:, :], in0=psI[:, :64], in1=ADDT[:, j * 4 + qb, :], op=ALU.add), r=[psI, ADDT], w=[IMP])
                            fw.op("dve", lambda e: e.max(out=M8[:, 0:8], in_=IMP[:, :]), r=[IMP], w=[M8])
                            fw.op("dve", lambda e: e.match_replace(out=WK[:, :], in_to_replace=M8[:, 0:8], in_values=IMP[:, :], imm_value=-3.0e38), r=[IMP, M8], w=[WK])
                            fw.op("dve", lambda e: e.max(out=M8[:, 8:16], in_=WK[:, :]), r=[WK], w=[M8])
                            fw.op("dve", lambda e: e.tensor_scalar(out=SEL[:, :], in0=IMP[:, :], scalar1=M8[:, 15:16], scalar2=None, op0=ALU.is_ge), r=[IMP, M8], w=[SEL])
                            ns, _ = NS.next()
                            fw.op("dve", lambda e, ns=ns: e.tensor_scalar(out=ns[:, :], in0=SEL[:, :], scalar1=-1.0, scalar2=BIGS, op0=ALU.add, op1=ALU.mult), r=[SEL], w=[ns])
                            fw.op("pe", lambda e, ns=ns, qb=qb: e.matmul(psT[:64, qb * 128:(qb + 1) * 128], lhsT=ns[:, :], rhs=IDENT[:, :], start=True, stop=True), r=[ns, IDENT], w=[psT])
                        negs, ni = NEGS.next()
                        fw.op("act", lambda e, negs=negs: e.activation(out=negs[:, :], in_=psT[:64, :], func=AF.Copy), r=[psT], w=[negs])
                        fw.op("sp", lambda e, negs=negs, g=g, tc=tc: e.dma_start(out=negT[g, :, tc], in_=negs[:, :]), r=[negs], dsem=f"NEGS{ni}")
                fw.pop()
            with ExitStack() as sub:
                fw.push(sub)
                Q1 = fw.sbuf([128, S], BF16, "Q1")
                KS = fw.sbuf([128, S], BF16, "KS")
                KW = fw.sbuf([128, S], BF16, "KW")
                VS = fw.sbuf([128, 32, 128], BF16, "VS")
                VW = fw.sbuf([128, 32, 128], BF16, "VW")
                NEGT = fw.sbuf([64, S], BF16, "NEGT")
                EX = fw.sbuf([64, 32, 128], BF16, "EX")
                WSEL = fw.sbuf([128, 2432], F32, "WSEL")
                WWIN = fw.sbuf([128, 1408], F32, "WWIN")
                HS = fw.sbuf([128, 2432], F32, "HSn")
                OACC = fw.sbuf([128, S], F32, "OACC")
                SGP = fw.sbuf([128, S], BF16, "SGP")
                YO = fw.sbuf([128, S], BF16, "YOn")
                RB31 = fw.sbuf([128, 16], F32, "RB31")
                GT = Rot([fw.sbuf([128, 512], F32, f"GTs{q}") for q in range(3)])
                TMP = Rot([fw.sbuf([128, 512], F32, f"TMPs{q}") for q in range(2)])
                PT = Rot([fw.sbuf([128, 512], BF16, f"PTs{q}") for q in range(6)])
                RD = Rot([fw.sbuf([128, 512], F32, f"RDs{q}") for q in range(2)])
                fw.op("pool", lambda e: e.dma_start(out=EX[:, :, :], in_=self.ex), w=[EX], dsem="EX")
                fw.op("sp", lambda e: e.dma_start(out=RB31[:, :], in_=bass.AP(self.relb.tensor, 31 * 16, [[0, 128], [1, 16]])), w=[RB31], dsem="RB31")
                rotS = Rot(self.ps[0:4])
                rotU = Rot(self.ps[4:6])
                rotD = Rot(self.ps[6:8])
                ngr = (DBG_HEADS + 3) // 4
                for g in range(ngr):
                    fw.op("sp", lambda e, g=g: e.dma_start(out=KS[:, :], in_=ksT[g * 128:(g + 1) * 128, :]), w=[KS], dsem="KS")
                    fw.op("sp", lambda e, g=g: e.dma_start(out=KW[:, :], in_=kwT[g * 128:(g + 1) * 128, :]), w=[KW], dsem="KW")
                    fw.op("sp", lambda e, g=g: e.dma_start(out=VS[:, :, :], in_=vsD[:, g * 128:(g + 1) * 128].rearrange("(jb p) c -> p jb c", p=128)), w=[VS], dsem="VS")
                    fw.op("sp", lambda e, g=g: e.dma_start(out=VW[:, :, :], in_=vwD[:, g * 128:(g + 1) * 128].rearrange("(jb p) c -> p jb c", p=128)), w=[VW], dsem="VW")
                    fw.op("sp", lambda e, g=g: e.dma_start(out=NEGT[:, :], in_=negT[g]), w=[NEGT], dsem="NEGT")
                    for r in range(min(4, DBG_HEADS)):
                        h = 4 * g + r
                        fw.op("sp", lambda e, h=h: e.dma_start(out=Q1[:, :], in_=qT[h * 128:(h + 1) * 128, :]), w=[Q1], dsem="Q1")
                        fw.op("sp", lambda e, h=h: e.dma_start(out=OACC[:, :], in_=ocT[h * 128:(h + 1) * 128, :]), w=[OACC], dsem="OACC")
                        fw.op("sp", lambda e, h=h: e.dma_start(out=SGP[:, :], in_=sgT[h * 128:(h + 1) * 128, :]), w=[SGP], dsem="SGP")
                        self.load_toeplitz(WSEL, "sel", h, -384, 2432, HS, "HSn", rotS)
                        self.load_toeplitz(WWIN, "win", h, -384, 1408, HS, "HSn", rotS)
                        items = []
                        for j in range(NTL):
                            for br in (1, 2):
                                if br == 1:
                                    kbs = list(range(0, 4 * j + 4))
                                else:
                                    kbs = list(range(max(0, 4 * j - 4), 4 * j + 4))
                                grp = {"j": j, "br": br}
                                for ix, kb in enumerate(kbs):
                                    items.append((grp, kb, ix == 0, ix == len(kbs) - 1))

                        def s_stage(it, h=h):
                            grp, kb, first, last = it
                            j, br = grp["j"], grp["br"]
                            t0 = j * NT
                            tc = slice(t0, t0 + NT)
                            KK, WW = (KS, WSEL) if br == 1 else (KW, WWIN)
                            if first:
                                gt, gi = GT.next()
                                grp["gt"] = gt
                                fw.op("sp", lambda e: e.dma_start(out=gt[:, :], in_=bass.AP(bgT.tensor, (h * 3 + br) * S + t0, [[0, 128], [1, NT]])), w=[gt], dsem=f"GTs{gi}")
                            d0 = t0 - kb * 128
                            psS, _ = rotS.next()
                            pt, _ = PT.next()
                            kc_ = slice(kb * 128, (kb + 1) * 128)
                            if br == 1:
                                fw.op("pe", lambda e: e.matmul(psS[:, :], lhsT=KK[:, kc_], rhs=Q1[:, tc], start=True, stop=False), r=[KK, Q1], w=[psS])
                                fw.op("pe", lambda e: e.matmul(psS[:, :], lhsT=EX[:, kb, :], rhs=NEGT[:, tc], start=False, stop=True), r=[EX, NEGT], w=[psS])
                            else:
                                fw.op("pe", lambda e: e.matmul(psS[:, :], lhsT=KK[:, kc_], rhs=Q1[:, tc], start=True, stop=True), r=[KK, Q1], w=[psS])
                            if br == 1 and d0 >= 1664:
                                fw.op("act", lambda e: e.activation(out=pt[:, :], in_=psS[:, :], func=AF.Exp, scale=SC, bias=RB31[:, h:h + 1]), r=[psS, RB31], w=[pt])
                            else:
                                tm, _ = TMP.next()
                                fw.op("dve", lambda e: e.scalar_tensor_tensor(out=tm[:, :], in0=psS[:, :], scalar=SC, in1=WW[:, d0 + 384:d0 + 384 + NT], op0=ALU.mult, op1=ALU.add), r=[psS, WW], w=[tm])
                                fw.op("act", lambda e: e.activation(out=pt[:, :], in_=tm[:, :], func=AF.Exp), r=[tm], w=[pt])
                            return pt

                        def pv_stage(it, pt):
                            grp, kb, first, last = it
                            j, br = grp["j"], grp["br"]
                            tc = slice(j * NT, (j + 1) * NT)
                            VV = VS if br == 1 else VW
                            if first:
                                grp["psU"], _ = rotU.next()
                                grp["psD"], _ = rotD.next()
                            psU, psD, gt = grp["psU"], grp["psD"], grp["gt"]
                            fw.op("pe", lambda e: e.matmul(psU[:, :], lhsT=VV[:, kb, :], rhs=pt[:, :], start=first, stop=last), r=[VV, pt], w=[psU])
                            fw.op("pe", lambda e: e.matmul(psD[:, :], lhsT=self.ones_bf[:, :], rhs=pt[:, :], start=first, stop=last), r=[self.ones_bf, pt], w=[psD])
                            if last:
                                rd, _ = RD.next()
                                fw.op("dve", lambda e: e.reciprocal(out=rd[:, :], in_=psD[:, :]), r=[psD], w=[rd])
                                fw.op("pool", lambda e: e.tensor_tensor(out=rd[:, :], in0=rd[:, :], in1=gt[:, :], op=ALU.mult), r=[rd, gt], w=[rd])
                                fw.op("dve", lambda e: e.tensor_tensor(out=rd[:, :], in0=rd[:, :], in1=psU[:, :], op=ALU.mult), r=[rd, psU], w=[rd])
                                fw.op("pool", lambda e: e.tensor_tensor(out=OACC[:, tc], in0=OACC[:, tc], in1=rd[:, :], op=ALU.add), r=[rd, OACC], w=[OACC])

                        LA = 3
                        pts = {}
                        n = len(items)
                        for ii in range(n + LA):
                            if ii < n:
                                pts[ii] = s_stage(items[ii])
                            if ii >= LA:
                                pv_stage(items[ii - LA], pts.pop(ii - LA))
                        fw.op("pool", lambda e: e.tensor_tensor(out=YO[:, :], in0=OACC[:, :], in1=SGP[:, :], op=ALU.mult), r=[OACC, SGP], w=[YO])
                        fw.op("sp", lambda e, h=h: e.dma_start(out=yT[h * 128:(h + 1) * 128, :], in_=YO[:, :]), r=[YO], dsem="YOn")
                fw.pop()
            fw.pop()
        self.post_all(i, self.c_w_out[0], 16, yT, src, dst)


_CACHE = {}


def _prep(inputs, layers=(0, 1, 2, 3), ncores=8):
    vecs, voff = _pack_vecs(inputs)
    key = (tuple(layers), vecs.shape[1])
    if key not in _CACHE:
        _CACHE[key] = Builder(voff, vecs.shape[1], layers)
    b = _CACHE[key]
    oh, cov, addt, cm, ex, ident = _host_tables()
    relb = np.concatenate([np.asarray(inputs["rel_bias"], np.float32), np.full((1, 16), NEG, np.float32)], axis=0)
    shared = {"vecs": vecs, "relb": relb, "oh": oh, "cov": cov, "addt": addt, "cm": cm, "ex": ex, "ident": ident,
              "jrev": np.ascontiguousarray(ident[::-1])}
    for k in ("ple_w_proj", "ple_w_gate", "a_w_in", "a_w_r", "a_w_i", "a_w_out", "b_w_in", "b_w_out", "c_w_in",
              "c_cmp_w1_k", "c_cmp_w2_k", "c_cmp_w1_v", "c_cmp_w2_v", "c_w_out"):
        shared[k] = np.ascontiguousarray(inputs[k], dtype=np.float32)
    x = np.asarray(inputs["x"], np.float32)
    p = np.asarray(inputs["p"], np.float32)
    in_maps = []
    for c in range(ncores):
        m = dict(shared)
        m["xT"] = np.ascontiguousarray(x[c].T)
        m["pT"] = np.ascontiguousarray(p[:, c].transpose(0, 2, 1))
        in_maps.append({k: v for k, v in m.items() if k in b.input_names})
    return b, in_maps


def kernel(**inputs):
    b, in_maps = _prep(inputs)
    res = run_bass_kernel_spmd(b.nc, in_maps, core_ids=list(range(8)))
    out = np.stack([np.ascontiguousarray(r["outT"].T) for r in res.results], axis=0)
    return out.astype(np.float32)
```

```python
import math
from contextlib import ExitStack
import numpy as np
import concourse.bass as bass
import concourse.mybir as mybir
from concourse.bass_utils import run_bass_kernel_spmd

F32 = mybir.dt.float32
BF16 = mybir.dt.bfloat16
AF = mybir.ActivationFunctionType
ALU = mybir.AluOpType

S = 4096
D = 2048
NT = 512
NTI = S // NT
import os
DBG_TILES = int(os.environ.get('DBG_TILES', '8'))
DBG_HEADS = int(os.environ.get('DBG_HEADS', '16'))
DBG_DUMP = os.environ.get('DBG_DUMP', '') != ''
DEPTH = 4
NEG = -30000.0
EPS = 1e-6


class Buf:
    def __init__(self, t, name):
        self.t = t
        self.name = name
        self.writers = {}
        self.readers = {}

    def __getitem__(self, k):
        return self.t[k]


class View:
    def __init__(self, base, t):
        self.base = base
        self.t = t
        self.name = base.name + "_v"

    def __getitem__(self, k):
        return self.t[k]

    @property
    def writers(self):
        return self.base.writers

    @writers.setter
    def writers(self, v):
        self.base.writers = v

    @property
    def readers(self):
        return self.base.readers

    @readers.setter
    def readers(self, v):
        self.base.readers = v


class FW:
    ENGS = ("pe", "act", "dve", "pool", "sp")

    def __init__(self, nc, ctx):
        self.nc = nc
        self.ctx = ctx
        self.stack = [ctx]
        self.lists = {e: [] for e in self.ENGS}
        self.waited = {e: {} for e in self.ENGS}
        self.sems = {}
        self.count = {}
        for e in ("pe", "act", "dve", "pool"):
            self.getsem("E_" + e)
        self.nbuf = 0
        self.ninst = 0

    def getsem(self, key):
        if key not in self.sems:
            self.sems[key] = self.ctx.enter_context(self.nc.semaphore("s_" + key))
            self.count[key] = 0
        return key

    def push(self, sub):
        self.stack.append(sub)

    def pop(self):
        self.barrier()
        self.stack.pop()

    def sbuf(self, shape, dtype, name=None):
        self.nbuf += 1
        name = (name or "sb") + f"_{self.nbuf}"
        t = self.stack[-1].enter_context(self.nc.sbuf_tensor(name, list(shape), dtype))
        return Buf(t, name)

    def psum(self, shape, dtype, name=None):
        self.nbuf += 1
        name = (name or "ps") + f"_{self.nbuf}"
        t = self.stack[-1].enter_context(self.nc.psum_tensor(name, list(shape), dtype))
        return Buf(t, name)

    def dram(self, shape, dtype, name=None):
        self.nbuf += 1
        name = (name or "dr") + f"_{self.nbuf}"
        if DBG_DUMP:
            t = self.nc.dram_tensor(name.rsplit("_", 1)[0], list(shape), dtype, kind="ExternalOutput")
        else:
            t = self.nc.dram_tensor(name, list(shape), dtype, kind="Internal")
        return t.ap()

    def op(self, eng, fn, r=(), w=(), dsem=None):
        E = self.lists[eng]
        needs = {}

        def upd(d):
            for s, v in d.items():
                if needs.get(s, 0) < v:
                    needs[s] = v

        for b in r:
            upd(b.writers)
        for b in w:
            if eng == "pe":
                upd({s: v for s, v in b.writers.items() if s != "E_pe"})
            else:
                upd(b.writers)
            upd(b.readers)
        if dsem is not None:
            sem = self.getsem("D_" + dsem)
            inc = 16
            if self.count[sem] > 0:
                upd({sem: self.count[sem]})
        else:
            sem = "E_" + eng
            inc = 1
        wd = self.waited[eng]
        waits = []
        for s, v in needs.items():
            if wd.get(s, 0) < v:
                wd[s] = v
                waits.append((s, v))
        self.count[sem] += inc
        val = self.count[sem]
        E.append((waits, fn, sem, val, inc))
        self.ninst += 1 + len(waits)
        for b in r:
            if b.readers.get(sem, 0) < val:
                b.readers[sem] = val
        for b in w:
            b.writers = {sem: val}
            b.readers = {}

    def barrier(self):
        for e in self.ENGS:
            wd = self.waited[e]
            waits = []
            for s, v in self.count.items():
                if v > 0 and wd.get(s, 0) < v:
                    wd[s] = v
                    waits.append((s, v))
            if waits:
                self.lists[e].append((waits, None, None, 0, 0))

    def emit(self):
        nc = self.nc
        sems = self.sems
        lists = self.lists
        targets = {}
        for e in self.ENGS:
            for waits, fn, sem, val, inc in lists[e]:
                for s, v in waits:
                    targets.setdefault(s, set()).add(v)
        remap = {}
        for s, vals in targets.items():
            if s.startswith("E_"):
                remap[s] = {v: i + 1 for i, v in enumerate(sorted(vals))}
        self.nsignal = sum(len(m) for m in remap.values())

        def run(e, lst):
            for waits, fn, sem, val, inc in lst:
                for s, v in waits:
                    e.wait_ge(sems[s], remap[s][v] if s in remap else v)
                if fn is not None:
                    ins = fn(e)
                    if sem.startswith("E_"):
                        if val in remap.get(sem, ()):
                            ins.then_inc(sems[sem], 1)
                    else:
                        ins.then_inc(sems[sem], inc)

        with nc.Block() as block:
            @block.tensor
            def _(e):
                run(e, lists["pe"])

            @block.scalar
            def _(e):
                run(e, lists["act"])

            @block.vector
            def _(e):
                run(e, lists["dve"])

            @block.gpsimd
            def _(e):
                run(e, lists["pool"])

            @block.sync
            def _(e):
                run(e, lists["sp"])


class Rot:
    def __init__(self, items):
        self.items = items
        self.i = 0

    def next(self):
        b = self.items[self.i % len(self.items)]
        idx = self.i % len(self.items)
        self.i += 1
        return b, idx


def _bucket_np(n):
    n = np.maximum(n, 0)
    nf = np.maximum(n, 16).astype(np.float32)
    large = 16 + (np.log(nf / np.float32(16)) / np.float32(math.log(2048 / 16)) * np.float32(16)).astype(np.int32)
    return np.where(n < 16, n, np.minimum(large, 31))


TV_KINDS = [("dil0", 1, 128, 1152), ("dil1", 4, 128, 1152), ("dil2", 16, 128, 1152),
            ("sel", 1, 1 << 30, 2688), ("win", 1, 511, 1536)]
TV_OFF = {}
_o = 0
for _n, _d, _m, _x in TV_KINDS:
    TV_OFF[_n] = (_o, _x)
    _o += _x
TV_TOT = _o


def _host_tables():
    oh = np.zeros((33, TV_TOT), np.float32)
    for name, dil, maxd, X in TV_KINDS:
        o, _ = TV_OFF[name]
        x = np.arange(X)
        delta = x - 511
        valid = (delta >= 0) & (delta <= maxd)
        b = _bucket_np(delta * dil)
        for xi in range(X):
            if valid[xi]:
                oh[b[xi], o + xi] = 1.0
            else:
                oh[32, o + xi] = 1.0
    ncmp = 255
    cidx0 = np.arange(ncmp) * 16
    cend = cidx0 + 31
    sel_start = np.arange(64) * 64
    cover = ((cidx0[:, None] < sel_start[None, :] + 64) & (cend[:, None] >= sel_start[None, :])).astype(np.float32)
    cov = np.zeros((256, 64), np.float32)
    cov[:255] = cover
    cov = cov.reshape(2, 128, 64).transpose(1, 0, 2).copy()
    t = np.arange(S)
    cur = t // 64
    n = np.arange(64)
    forced = (n[None, :] == 0) | (n[None, :] == cur[:, None]) | (n[None, :] == cur[:, None] - 1)
    validb = n[None, :] * 64 <= t[:, None]
    addt = np.where(validb, 1e4 * forced.astype(np.float32), -1e30).astype(np.float32)
    addt = addt.reshape(32, 128, 64).transpose(1, 0, 2).copy()
    cm = np.zeros((128, 8, 2, 512), np.float32)
    for j in range(8):
        for cb in range(2):
            c = cb * 128 + np.arange(128)
            tt = j * 512 + np.arange(512)
            ok = (16 * c[:, None] + 31) <= tt[None, :]
            cm[:, j, cb, :] = np.where(ok, 0.0, NEG)
    ex = np.zeros((64, 32, 128), np.float32)
    for kb in range(32):
        for k in range(128):
            ex[2 * kb + k // 64, kb, k] = 1.0
    ident = np.eye(128, dtype=np.float32)
    return oh, cov, addt, cm, ex, ident


def _pack_vecs(inp):
    cols = []
    off = {}

    def add(name, arr):
        off[name] = sum(c.shape[1] for c in cols)
        cols.append(np.ascontiguousarray(arr, dtype=np.float32))

    def fm(v):
        return np.asarray(v, np.float32).reshape(16, 128).T

    for i in range(DEPTH):
        add(f"pre{i}", fm(inp["norm_pre"][i]))
        add(f"post{i}", fm(inp["norm_post"][i]))
    for j in range(inp["a_w_in"].shape[0]):
        for t in range(4):
            add(f"cw{j}_{t}", fm(inp["a_conv_w"][j, t]))
        add(f"cb{j}", fm(inp["a_conv_b"][j]))
        add(f"br{j}", fm(inp["a_b_r"][j].reshape(-1)))
        add(f"bi{j}", fm(inp["a_b_i"][j].reshape(-1)))
        add(f"lam{j}", fm(inp["a_lam"][j]))
    add("posk", np.asarray(inp["c_cmp_pos_k"][0], np.float32).T)
    add("posv", np.asarray(inp["c_cmp_pos_v"][0], np.float32).T)
    return np.concatenate(cols, axis=1), off


class Builder:
    def __init__(self, voff, nvec, layers=(0, 1, 2, 3)):
        self.voff = voff
        self.nvec = nvec
        self.layers = layers
        nc = bass.Bass("TRN2", target_bir_lowering=False)
        self.nc = nc

        self.input_names = []
        kinds = {l % 3 for l in layers}

        def inp(name, shape, dt=F32, need=True):
            if not need:
                return None
            self.input_names.append(name)
            return nc.dram_tensor(name, list(shape), dt, kind="ExternalInput").ap()

        att = (1 in kinds) or (2 in kinds)
        self.xT = inp("xT", [D, S])
        self.pT = inp("pT", [DEPTH, 256, S])
        self.vecs_d = inp("vecs", [128, nvec])
        self.relb = inp("relb", [33, 16], need=att)
        self.oh = inp("oh", [33, TV_TOT], need=att)
        self.cov = inp("cov", [128, 2, 64], need=2 in kinds)
        self.addt = inp("addt", [128, 32, 64], need=2 in kinds)
        self.cm = inp("cm", [128, 8, 2, 512], need=2 in kinds)
        self.ex = inp("ex", [64, 32, 128], need=2 in kinds)
        self.ident = inp("ident", [128, 128], need=2 in kinds)
        self.jrev = inp("jrev", [128, 128], need=att)
        self.ple_w_proj = inp("ple_w_proj", [DEPTH, 256, D])
        self.ple_w_gate = inp("ple_w_gate", [DEPTH, D, D])
        self.a_w_in = inp("a_w_in", [2, D, 2 * D], need=0 in kinds)
        self.a_w_r = inp("a_w_r", [2, 8, 256, 256], need=0 in kinds)
        self.a_w_i = inp("a_w_i", [2, 8, 256, 256], need=0 in kinds)
        self.a_w_out = inp("a_w_out", [2, D, D], need=0 in kinds)
        self.b_w_in = inp("b_w_in", [1, D, 10240], need=1 in kinds)
        self.b_w_out = inp("b_w_out", [1, 1024, D], need=1 in kinds)
        self.c_w_in = inp("c_w_in", [1, D, 7216], need=2 in kinds)
        self.c_w1_k = inp("c_cmp_w1_k", [1, 4096, 512], need=2 in kinds)
        self.c_w2_k = inp("c_cmp_w2_k", [1, 512, 128], need=2 in kinds)
        self.c_w1_v = inp("c_cmp_w1_v", [1, 4096, 512], need=2 in kinds)
        self.c_w2_v = inp("c_cmp_w2_v", [1, 512, 128], need=2 in kinds)
        self.c_w_out = inp("c_w_out", [1, D, D], need=2 in kinds)
        self.outT = nc.dram_tensor("outT", [D, S], F32, kind="ExternalOutput").ap()

        with ExitStack() as ctx:
            fw = FW(nc, ctx)
            self.fw = fw
            self.build()
            fw.barrier()
            fw.emit()
            print("kernel build: instr+waits", fw.ninst, "sems", len(fw.sems), "signals", fw.nsignal)

    def vcol(self, name, k=0, n=1):
        o = self.voff[name] + k
        return self.vecs[:, o:o + n]

    def build(self):
        fw = self.fw
        self.vecs = fw.sbuf([128, self.nvec], F32, "vecs")
        fw.op("sp", lambda e: e.dma_start(out=self.vecs[:, :], in_=self.vecs_d), w=[self.vecs], dsem="vecs")
        self.ones_bf = fw.sbuf([128, 128], BF16, "ones")
        fw.op("pool", lambda e: e.memset(self.ones_bf[:, :], 1.0), w=[self.ones_bf])
        self.cst = fw.sbuf([128, 4], F32, "cst")
        fw.op("pool", lambda e: e.memset(self.cst[:, 0:1], EPS), w=[self.cst])
        fw.op("pool", lambda e: e.memset(self.cst[:, 1:2], 1.0), w=[self.cst])
        fw.op("pool", lambda e: e.memset(self.cst[:, 2:3], 0.25), w=[self.cst])
        if self.jrev is not None:
            self.JREV = fw.sbuf([128, 128], F32, "JREV")
            fw.op("sp", lambda e: e.dma_start(out=self.JREV[:, :], in_=self.jrev), w=[self.JREV], dsem="JREV")
        self.ps = [fw.psum([128, 512], F32, f"psb{i}") for i in range(8)]
        self.psr = Rot(self.ps[:6])
        self.psS = self.ps[6]
        self.psX = self.ps[7]
        self.wcache = {}
        self.wq = "pool"
        self.pref = {}
        self.nwc = 0
        self.wt = Rot([fw.sbuf([128, 16, 512], BF16, f"wt{i}") for i in range(2)])
        hA = fw.dram([D, S], F32, "hA")
        hB = fw.dram([D, S], F32, "hB")
        self.hbufs = {}
        for nm, ap in (("x", self.xT), ("A", hA), ("B", hB), ("out", self.outT)):
            self.hbufs[nm] = (ap, [Buf(None, f"h{nm}{t}") for t in range(NTI)])
        self.tv = fw.dram([16, TV_TOT], F32, "tv")
        self.tvbuf = Buf(None, "tv")
        if any(l in (1, 2) for l in self.layers):
            self.build_tv()
        cur = "x"
        nl = len(self.layers)
        for idx, i in enumerate(self.layers):
            dst = "out" if idx == nl - 1 else ("A" if cur != "A" else "B")
            kind = i % 3
            j = i // 3
            if kind == 0:
                self.layer_rglru(i, j, cur, dst)
            elif kind == 1:
                self.layer_dil(i, cur, dst)
            else:
                self.layer_nsa(i, cur, dst)
            cur = dst

    def build_tv(self):
        fw = self.fw
        with ExitStack() as sub:
            fw.push(sub)
            rb = fw.sbuf([33, 16], F32, "rb")
            fw.op("sp", lambda e: e.dma_start(out=rb[:, :], in_=self.relb), w=[rb], dsem="rb")
            nchunk = (TV_TOT + 511) // 512
            ohs = Rot([fw.sbuf([33, 512], F32, f"ohs{i}") for i in range(2)])
            tvs = Rot([fw.sbuf([16, 512], F32, f"tvs{i}") for i in range(2)])
            for c in range(nchunk):
                c0 = c * 512
                n = min(512, TV_TOT - c0)
                o, oi = ohs.next()
                fw.op("sp", lambda e, o=o, c0=c0, n=n: e.dma_start(out=o[:, :n], in_=self.oh[:, c0:c0 + n]), w=[o], dsem=f"ohs{oi}")
                ps, _ = self.psr.next()
                fw.op("pe", lambda e, o=o, ps=ps, n=n: e.matmul(ps[:16, :n], lhsT=rb[:, :], rhs=o[:, :n], start=True, stop=True), r=[rb, o], w=[ps])
                t, ti = tvs.next()
                fw.op("act", lambda e, t=t, ps=ps, n=n: e.activation(out=t[:, :n], in_=ps[:16, :n], func=AF.Copy), r=[ps], w=[t])
                fw.op("sp", lambda e, t=t, c0=c0, n=n: e.dma_start(out=self.tv[:, c0:c0 + n], in_=t[:, :n]), r=[t], w=[self.tvbuf], dsem=f"tvs{ti}")
            fw.pop()

    def load_toeplitz(self, Wt, kind, h, base_delta, width, Hs, nm, rot):
        fw = self.fw
        o, X = TV_OFF[kind]
        assert base_delta + 384 >= 0 and base_delta + 384 + 127 + width - 1 < X, (kind, base_delta, width)
        start = h * TV_TOT + o + base_delta + 511 - 127
        src = bass.AP(self.tv.tensor, start, [[1, 128], [1, width]])
        fw.op("sp", lambda e: e.dma_start(out=Hs[:, :width], in_=src), w=[Hs], dsem=nm)
        for c0 in range(0, width, 512):
            n = min(512, width - c0)
            ps, _ = rot.next()
            fw.op("pe", lambda e, ps=ps, c0=c0, n=n: e.matmul(ps[:, :n], lhsT=self.JREV[:, :], rhs=Hs[:, c0:c0 + n], start=True, stop=True), r=[self.JREV, Hs], w=[ps])
            fw.op("act", lambda e, ps=ps, c0=c0, n=n: e.activation(out=Wt[:, c0:c0 + n], in_=ps[:, :n], func=AF.Copy), r=[ps], w=[Wt])

    def load_w(self, W, r0, kc, c0, ncols):
        fw = self.fw
        key = (W.tensor.name, int(W.offset), r0, kc, c0, ncols)
        if key in self.pref:
            return self.pref.pop(key)
        wt, wi = self.wt.next()
        if key in self.wcache:
            scr, sb = self.wcache[key]
            fw.op(self.wq, lambda e: e.dma_start(out=wt[:, :kc, :ncols], in_=scr), r=[sb], w=[wt], dsem=f"wt{wi}_{self.wq}")
        else:
            src = W[r0:r0 + kc * 128, c0:c0 + ncols].rearrange("(k p) n -> p k n", p=128)
            fw.op("pool", lambda e: e.dma_start(out=wt[:, :kc, :ncols], in_=src), w=[wt], dsem=f"wt{wi}_pool")
            self.nwc += 1
            t = self.nc.dram_tensor(f"wc{self.nwc}", [128, kc, ncols], BF16, kind="Internal")
            scr = t.ap()
            sb = Buf(None, f"wc{self.nwc}")
            fw.op("sp", lambda e: e.dma_start(out=scr, in_=wt[:, :kc, :ncols]), r=[wt], w=[sb], dsem=f"wts{wi}")
            self.wcache[key] = (scr, sb)
        return wt

    def prefetch_w(self, W, r0, kc, c0, ncols):
        key = (W.tensor.name, int(W.offset), r0, kc, c0, ncols)
        assert key not in self.pref
        wt = self.load_w(W, r0, kc, c0, ncols)
        self.pref[key] = wt

    def prenorm_load(self, src, t, B32, nm="B32"):
        fw = self.fw
        ap, bufs = self.hbufs[src]
        fw.op("sp", lambda e: e.dma_start(out=B32[:, :, :], in_=ap[:, t * NT:(t + 1) * NT].rearrange("(k p) n -> p k n", p=128)),
              r=[bufs[t]], w=[B32], dsem=nm)

    def prenorm(self, i, src, t, B32, U16, tmp, load=True, nm="B32"):
        fw = self.fw
        if load:
            self.prenorm_load(src, t, B32, nm)
        self.rstd_of(B32, tmp)
        rstd = tmp["rstd"]
        for k in range(16):
            g = self.vcol(f"pre{i}", k)
            fw.op("dve", lambda e, k=k, g=g: e.scalar_tensor_tensor(out=U16[:, k, :], in0=B32[:, k, :], scalar=g, in1=rstd[:, :], op0=ALU.mult, op1=ALU.mult),
                  r=[B32, rstd, self.vecs], w=[U16])

    def rstd_of(self, X32, tmp, squares_done=False):
        fw = self.fw
        psS = self.psS
        if not squares_done:
            for k in range(16):
                sq, _ = tmp["sq"].next()
                fw.op("act", lambda e, k=k, sq=sq: e.activation(out=sq[:, :], in_=X32[:, k, :], func=AF.Square), r=[X32], w=[sq])
                fw.op("pe", lambda e, k=k, sq=sq: e.matmul(psS[:, :], lhsT=self.ones_bf[:, :], rhs=sq[:, :], start=(k == 0), stop=(k == 15)),
                      r=[self.ones_bf, sq], w=[psS])
        rstd = tmp["rstd"]
        fw.op("act", lambda e: e.activation(out=rstd[:, :], in_=psS[:, :], func=AF.Sqrt, scale=1.0 / D, bias=self.cst[:, 0:1]), r=[psS, self.cst], w=[rstd])
        fw.op("dve", lambda e: e.reciprocal(out=rstd[:, :], in_=rstd[:, :]), r=[rstd], w=[rstd])

    def linear_fm(self, W, kc, c0, ncols, rhs_buf, evac):
        fw = self.fw
        mi = 0
        for g0 in range(0, ncols, 512):
            gn = min(512, ncols - g0)
            wt = self.load_w(W, 0, kc, c0 + g0, gn)
            for m0 in range(0, gn, 128):
                mw = min(128, gn - m0)
                ps, _ = self.psr.next()
                for k in range(kc):
                    fw.op("pe", lambda e, k=k, m0=m0, mw=mw, ps=ps, wt=wt: e.matmul(ps[:mw, :], lhsT=wt[:, k, m0:m0 + mw], rhs=rhs_buf[:, k, :], start=(k == 0), stop=(k == kc - 1)),
                          r=[wt, rhs_buf], w=[ps])
                evac(mi, mw, ps)
                mi += 1

    def linear_tm(self, W, kc, c0, ncols, lhs_buf, evac):
        fw = self.fw
        for g0 in range(0, ncols, 512):
            gn = min(512, ncols - g0)
            wt = self.load_w(W, 0, kc, c0 + g0, gn)
            for tb in range(NT // 128):
                ps, _ = self.psr.next()
                for k in range(kc):
                    fw.op("pe", lambda e, k=k, tb=tb, ps=ps, wt=wt, gn=gn: e.matmul(ps[:, :gn], lhsT=lhs_buf[:, k, tb * 128:(tb + 1) * 128], rhs=wt[:, k, :gn], start=(k == 0), stop=(k == kc - 1)),
                          r=[wt, lhs_buf], w=[ps])
                evac(tb, g0, gn, ps)

    def post_phase(self, i, w_out, kcy, src, dst, t, Y16, A32, B32, U16, tmp, mid_hook=None):
        fw = self.fw
        psS = self.psS

        pend = []

        def evac_out(mi, mw, ps):
            fw.op("act", lambda e: e.activation(out=A32[:, mi, :], in_=ps[:, :], func=AF.Copy), r=[ps], w=[A32])
            sq, _ = tmp["sq"].next()
            fw.op("dve", lambda e: e.tensor_tensor(out=sq[:, :], in0=ps[:, :], in1=A32[:, mi, :], op=ALU.mult), r=[ps, A32], w=[sq])

            def ones_mm():
                fw.op("pe", lambda e: e.matmul(psS[:, :], lhsT=self.ones_bf[:, :], rhs=sq[:, :], start=(mi == 0), stop=(mi == 15)), r=[self.ones_bf, sq], w=[psS])

            if pend:
                pend.pop()()
            pend.append(ones_mm)

        P16 = tmp["P16"]
        fw.op("pool", lambda e: e.dma_start(out=P16[:, :, :], in_=self.pT[i, :, t * NT:(t + 1) * NT].rearrange("(k p) n -> p k n", p=128)), w=[P16], dsem="P16")
        WP = tmp["WP"]
        fw.op("pool", lambda e: e.dma_start(out=WP[:, :, :], in_=self.ple_w_proj[i].rearrange("(k p) n -> p k n", p=128)), w=[WP], dsem="WP")
        self.linear_fm(w_out, kcy, 0, D, Y16, evac_out)
        while pend:
            pend.pop()()
        self.rstd_of(A32, tmp, squares_done=True)
        rstd = tmp["rstd"]
        ap, bufs = self.hbufs[src]
        fw.op("sp", lambda e: e.dma_start(out=B32[:, :, :], in_=ap[:, t * NT:(t + 1) * NT].rearrange("(k p) n -> p k n", p=128)),
              r=[bufs[t]], w=[B32], dsem="B32")
        for k in range(16):
            g = self.vcol(f"post{i}", k)
            fw.op("dve", lambda e, k=k, g=g: e.scalar_tensor_tensor(out=A32[:, k, :], in0=A32[:, k, :], scalar=g, in1=rstd[:, :], op0=ALU.mult, op1=ALU.mult),
                  r=[A32, rstd, self.vecs], w=[A32])
            fw.op("dve" if k % 3 else "pool", lambda e, k=k: e.tensor_tensor(out=B32[:, k, :], in0=B32[:, k, :], in1=A32[:, k, :], op=ALU.add), r=[A32, B32], w=[B32])
            fw.op("act", lambda e, k=k: e.activation(out=U16[:, k, :], in_=B32[:, k, :], func=AF.Copy), r=[B32], w=[U16])
        if mid_hook is not None:
            mid_hook()

        def evac_gate(mi, mw, ps):
            sg, _ = tmp["sg"].next()
            fw.op("act", lambda e: e.activation(out=sg[:, :], in_=ps[:, :], func=AF.Sigmoid), r=[ps], w=[sg])
            ps2 = self.psX
            for kk in range(2):
                fw.op("pe", lambda e, kk=kk: e.matmul(ps2[:, :], lhsT=WP[:, kk, mi * 128:(mi + 1) * 128], rhs=P16[:, kk, :], start=(kk == 0), stop=(kk == 1)), r=[WP, P16], w=[ps2])
            fw.op("dve", lambda e: e.tensor_tensor(out=sg[:, :], in0=sg[:, :], in1=ps2[:, :], op=ALU.mult), r=[sg, ps2], w=[sg])
            fw.op("pool", lambda e: e.tensor_tensor(out=B32[:, mi, :], in0=B32[:, mi, :], in1=sg[:, :], op=ALU.add), r=[B32, sg], w=[B32])

        self.linear_fm(self.ple_w_gate[i], 16, 0, D, U16, evac_gate)
        apd, bufd = self.hbufs[dst]
        fw.op("sp", lambda e: e.dma_start(out=apd[:, t * NT:(t + 1) * NT].rearrange("(k p) n -> p k n", p=128), in_=B32[:, :, :]),
              r=[B32], w=[bufd[t]], dsem="B32")

    def common_tiles(self):
        fw = self.fw
        tmp = {}
        tmp["sq"] = Rot([fw.sbuf([128, 512], BF16, f"sq{i}") for i in range(2)])
        tmp["rstd"] = fw.sbuf([128, 512], F32, "rstd")
        tmp["sg"] = Rot([fw.sbuf([128, 512], F32, f"sg{i}") for i in range(2)])
        tmp["P16"] = fw.sbuf([128, 2, 512], BF16, "P16")
        tmp["WP"] = fw.sbuf([128, 2, D], BF16, "WP")
        return tmp

    def layer_rglru(self, i, j, src, dst):
        fw = self.fw
        self.wq = "sp"
        with ExitStack() as sub:
            fw.push(sub)
            tmp = self.common_tiles()
            XB = fw.sbuf([128, 16, 3 + 512], F32, "XB")
            A32 = View(XB, XB.t[:, :, 3:515])
            B32 = fw.sbuf([128, 16, 512], F32, "B32")
            U16 = fw.sbuf([128, 16, 512], BF16, "U16")
            G16 = fw.sbuf([128, 16, 512], BF16, "G16")
            Y16 = G16
            WR = fw.sbuf([128, 8, 2, 256], BF16, "WR")
            WI = fw.sbuf([128, 8, 2, 256], BF16, "WI")
            c8 = fw.sbuf([128, 32], F32, "c8")
            carry = fw.sbuf([128, 16], F32, "carry")
            XC = Rot([fw.sbuf([128, 2, 512], F32, f"XC{q}") for q in range(2)])
            XCB = Rot([fw.sbuf([128, 2, 512], BF16, f"XCB{q}") for q in range(2)])
            small = {nm: Rot([fw.sbuf([128, 512], F32, f"{nm}{q}") for q in range(2 if nm == "HH" else 4)]) for nm in ("R", "I", "Ss", "HH")}
            fw.op("pool", lambda e: e.dma_start(out=WR[:, :, :, :], in_=self.a_w_r[j].rearrange("n (k p) m -> p n k m", p=128)), w=[WR], dsem="WR")
            fw.op("pool", lambda e: e.dma_start(out=WI[:, :, :, :], in_=self.a_w_i[j].rearrange("n (k p) m -> p n k m", p=128)), w=[WI], dsem="WI")
            lam = self.vcol(f"lam{j}", 0, 16)
            fw.op("act", lambda e: e.activation(out=c8[:, 0:16], in_=lam, func=AF.Exp, scale=-1.0), r=[self.vecs], w=[c8])
            fw.op("act", lambda e: e.activation(out=c8[:, 0:16], in_=c8[:, 0:16], func=AF.Ln, bias=self.cst[:, 1:2]), r=[c8, self.cst], w=[c8])
            fw.op("dve", lambda e: e.tensor_scalar(out=c8[:, 0:16], in0=c8[:, 0:16], scalar1=-4.0, scalar2=None, op0=ALU.mult), r=[c8], w=[c8])
            hb = fw.sbuf([128, 32], F32, "hb")
            fw.op("dve", lambda e: e.tensor_scalar(out=hb[:, 0:16], in0=self.vcol(f"br{j}", 0, 16), scalar1=0.5, scalar2=None, op0=ALU.mult), r=[self.vecs], w=[hb])
            fw.op("dve", lambda e: e.tensor_scalar(out=hb[:, 16:32], in0=self.vcol(f"bi{j}", 0, 16), scalar1=0.5, scalar2=None, op0=ALU.mult), r=[self.vecs], w=[hb])
            fw.op("pool", lambda e: e.memset(carry[:, :], 0.0), w=[carry])
            fw.op("pool", lambda e: e.memset(XB[:, :, 0:3], 0.0), w=[XB])
            W = self.a_w_in[j]
            NTL_ = min(NTI, DBG_TILES)
            self.prenorm_load(src, 0, A32, "XBh")
            for t in range(NTL_):
                self.prenorm(i, src, t, A32, U16, tmp, load=False)

                def evac_x(mi, mw, ps):
                    fw.op("act", lambda e: e.activation(out=XB[:, mi, 3:], in_=ps[:, :], func=AF.Copy), r=[ps], w=[XB])

                def evac_g(mi, mw, ps):
                    fw.op("act", lambda e: e.activation(out=G16[:, mi, :], in_=ps[:, :], func=AF.Silu), r=[ps], w=[G16])

                self.linear_fm(W, 16, 0, D, U16, evac_x)
                pending = []
                for gg in range(4):
                    self.linear_fm(W, 16, D + gg * 512, 512, U16, lambda mi, mw, ps, gg=gg: evac_g(4 * gg + mi, mw, ps))
                    if gg < 3:
                        self.prefetch_w(W, 0, 16, D + (gg + 1) * 512, 512)
                    else:
                        self.prefetch_w(self.a_w_out[j], 0, 16, 0, 512)
                    for n in (2 * gg, 2 * gg + 1):
                        xc, _ = XC.next()
                        xcb, _ = XCB.next()
                        for mm in range(2):
                            ch = 2 * n + mm
                            fw.op("dve", lambda e, ch=ch, mm=mm, xc=xc: e.tensor_scalar(out=xc[:, mm, :], in0=XB[:, ch, 3:515], scalar1=self.vcol(f"cw{j}_3", ch), scalar2=self.vcol(f"cb{j}", ch), op0=ALU.mult, op1=ALU.add),
                                  r=[XB, self.vecs], w=[xc])
                            for tap in range(3):
                                fw.op("dve", lambda e, ch=ch, mm=mm, xc=xc, tap=tap: e.scalar_tensor_tensor(out=xc[:, mm, :], in0=XB[:, ch, tap:tap + 512], scalar=self.vcol(f"cw{j}_{tap}", ch), in1=xc[:, mm, :], op0=ALU.mult, op1=ALU.add),
                                      r=[XB, self.vecs, xc], w=[xc])
                        fw.op("pool", lambda e, xc=xc, xcb=xcb: e.tensor_copy(out=xcb[:, :, :], in_=xc[:, :, :]), r=[xc], w=[xcb])
                        chunks = []
                        for mm in range(2):
                            ch = 2 * n + mm
                            psr_, _ = self.psr.next()
                            psi_, _ = self.psr.next()
                            for kk in range(2):
                                fw.op("pe", lambda e, kk=kk, mm=mm, n=n, xcb=xcb, p=psr_: e.matmul(p[:, :], lhsT=WR[:, n, kk, mm * 128:(mm + 1) * 128], rhs=xcb[:, kk, :], start=(kk == 0), stop=(kk == 1)), r=[WR, xcb], w=[psr_])
                            for kk in range(2):
                                fw.op("pe", lambda e, kk=kk, mm=mm, n=n, xcb=xcb, p=psi_: e.matmul(p[:, :], lhsT=WI[:, n, kk, mm * 128:(mm + 1) * 128], rhs=xcb[:, kk, :], start=(kk == 0), stop=(kk == 1)), r=[WI, xcb], w=[psi_])
                            cR = small["R"].next()[0]
                            cI = small["I"].next()[0]
                            chunks.append(dict(ch=ch, mm=mm, psr=psr_, psi=psi_, R=cR, I=cI, Aa=cR, Ss=small["Ss"].next()[0], BT=cI, HH=small["HH"].next()[0]))
                        for c in chunks:
                            fw.op("act", lambda e, c=c: e.activation(out=c["R"][:, :], in_=c["psr"][:, :], func=AF.Tanh, scale=0.5, bias=hb[:, c["ch"]:c["ch"] + 1]), r=[c["psr"], hb], w=[c["R"]])
                            fw.op("act", lambda e, c=c: e.activation(out=c["I"][:, :], in_=c["psi"][:, :], func=AF.Tanh, scale=0.5, bias=hb[:, 16 + c["ch"]:17 + c["ch"]]), r=[c["psi"], hb], w=[c["I"]])
                        for c in chunks:
                            fw.op("act", lambda e, c=c: e.activation(out=c["Aa"][:, :], in_=c["R"][:, :], func=AF.Exp, scale=c8[:, c["ch"]:c["ch"] + 1], bias=c8[:, c["ch"]:c["ch"] + 1]), r=[c["R"], c8], w=[c["Aa"]])
                        for c in chunks:
                            fw.op("dve", lambda e, c=c: e.tensor_tensor(out=c["Ss"][:, :], in0=c["Aa"][:, :], in1=c["Aa"][:, :], op=ALU.mult), r=[c["Aa"]], w=[c["Ss"]])
                            fw.op("dve", lambda e, c=c, xc=xc: e.scalar_tensor_tensor(out=c["BT"][:, :], in0=c["I"][:, :], scalar=1.0, in1=xc[:, c["mm"], :], op0=ALU.add, op1=ALU.mult), r=[c["I"], xc], w=[c["BT"]])

                        def stage_b(chunks=chunks):
                            for c in chunks:
                                fw.op("act", lambda e, c=c: e.activation(out=c["Ss"][:, :], in_=c["Ss"][:, :], func=AF.Sqrt, scale=-0.25, bias=self.cst[:, 2:3]), r=[c["Ss"], self.cst], w=[c["Ss"]])
                            for c in chunks:
                                fw.op("pool", lambda e, c=c: e.tensor_tensor(out=c["BT"][:, :], in0=c["BT"][:, :], in1=c["Ss"][:, :], op=ALU.mult), r=[c["Ss"], c["BT"]], w=[c["BT"]])
                                fw.op("dve", lambda e, c=c: e.tensor_tensor_scan(out=c["HH"][:, :], data0=c["Aa"][:, :], data1=c["BT"][:, :], initial=carry[:, c["ch"]:c["ch"] + 1], op0=ALU.mult, op1=ALU.add), r=[c["Aa"], c["BT"], carry], w=[c["HH"]])
                                fw.op("dve", lambda e, c=c: e.tensor_copy(out=carry[:, c["ch"]:c["ch"] + 1], in_=c["HH"][:, 511:512]), r=[c["HH"]], w=[carry])
                                fw.op("pool", lambda e, c=c: e.tensor_tensor(out=Y16[:, c["ch"], :], in0=c["HH"][:, :], in1=G16[:, c["ch"], :], op=ALU.mult), r=[c["HH"], G16], w=[Y16])

                        if pending:
                            pending.pop()()
                        pending.append(stage_b)
                while pending:
                    pending.pop()()
                fw.op("dve", lambda e: e.tensor_copy(out=XB[:, :, 0:3], in_=XB[:, :, 512:515]), r=[XB], w=[XB])
                self.post_phase(i, self.a_w_out[j], 16, src, dst, t, Y16, A32, B32, U16, tmp,
                                mid_hook=(lambda t=t: self.prenorm_load(src, t + 1, A32, "XBh")) if t + 1 < NTL_ else None)
            fw.pop()
        self.wq = "pool"

    def inproj_phase(self, i, src, W, segs):
        fw = self.fw
        with ExitStack() as sub:
            fw.push(sub)
            tmp = self.common_tiles()
            B32s = [fw.sbuf([128, 16, 512], F32, f"B32{q}") for q in range(2)]
            U16 = fw.sbuf([128, 16, 512], BF16, "U16")
            stage = Rot([fw.sbuf([128, 512], BF16, f"stg{q}") for q in range(3)])
            stage32 = Rot([fw.sbuf([128, 512], F32, f"stgf{q}") for q in range(2)])
            flip = [0]
            NTL_ = min(NTI, DBG_TILES)
            self.prenorm_load(src, 0, B32s[0], "B32i0")
            for t in range(NTL_):
                if t + 1 < NTL_:
                    self.prenorm_load(src, t + 1, B32s[(t + 1) % 2], f"B32i{(t + 1) % 2}")
                self.prenorm(i, src, t, B32s[t % 2], U16, tmp, load=False)
                for mode, c0, ncols, func, dap, d0 in segs:
                    if mode == "fm":
                        def ev(mi, mw, ps, func=func, dap=dap, d0=d0, t=t):
                            if dap.dtype == F32:
                                st, si = stage32.next()
                                nm = f"stgf{si}"
                            else:
                                st, si = stage.next()
                                nm = f"stg{si}"
                            flip[0] ^= 1
                            if func == AF.Copy and flip[0]:
                                fw.op("dve", lambda e: e.tensor_copy(out=st[:mw, :], in_=ps[:mw, :]), r=[ps], w=[st])
                            else:
                                fw.op("act", lambda e: e.activation(out=st[:mw, :], in_=ps[:mw, :], func=func), r=[ps], w=[st])
                            fw.op("sp", lambda e: e.dma_start(out=dap[d0 + mi * 128:d0 + mi * 128 + mw, t * NT:(t + 1) * NT], in_=st[:mw, :]), r=[st], dsem=nm)
                        self.linear_fm(W, 16, c0, ncols, U16, ev)
                    else:
                        def ev(tb, g0, gn, ps, dap=dap, d0=d0, t=t):
                            st, si = stage.next()
                            flip[0] ^= 1
                            if flip[0]:
                                fw.op("dve", lambda e: e.tensor_copy(out=st[:, :gn], in_=ps[:, :gn]), r=[ps], w=[st])
                            else:
                                fw.op("act", lambda e: e.activation(out=st[:, :gn], in_=ps[:, :gn], func=AF.Copy), r=[ps], w=[st])
                            fw.op("sp", lambda e: e.dma_start(out=dap[t * NT + tb * 128:t * NT + (tb + 1) * 128, d0 + g0:d0 + g0 + gn], in_=st[:, :gn]), r=[st], dsem=f"stg{si}")
                        self.linear_tm(W, 16, c0, ncols, U16, ev)
            fw.pop()

    def post_all(self, i, w_out, kcy, yT, src, dst):
        fw = self.fw
        with ExitStack() as sub:
            fw.push(sub)
            tmp = self.common_tiles()
            A32 = fw.sbuf([128, 16, 512], F32, "A32")
            B32 = fw.sbuf([128, 16, 512], F32, "B32")
            U16 = fw.sbuf([128, 16, 512], BF16, "U16")
            Y16s = [fw.sbuf([128, kcy, 512], BF16, f"Y16{q}") for q in range(2)]
            NTL_ = min(NTI, DBG_TILES)

            def ld(t):
                Y16 = Y16s[t % 2]
                fw.op("sp", lambda e: e.dma_start(out=Y16[:, :, :], in_=yT[:, t * NT:(t + 1) * NT].rearrange("(k p) n -> p k n", p=128)), w=[Y16], dsem=f"Y16{t % 2}")

            ld(0)
            for t in range(NTL_):
                if t + 1 < NTL_:
                    ld(t + 1)
                self.post_phase(i, w_out, kcy, src, dst, t, Y16s[t % 2], A32, B32, U16, tmp)
            fw.pop()

    def layer_dil(self, i, src, dst):
        fw = self.fw
        W = self.b_w_in[0]
        qkT = fw.dram([6144, S], BF16, "qkT")
        Vd = fw.dram([S, 3072], BF16, "Vd")
        sgT = fw.dram([1024, S], BF16, "sgTd")
        yT = fw.dram([1024, S], BF16, "yTd")
        segs = []
        for g in range(3):
            segs.append(("fm", g * 3072, 1024, AF.Copy, qkT, g * 2048))
            segs.append(("fm", g * 3072 + 1024, 1024, AF.Copy, qkT, g * 2048 + 1024))
            segs.append(("tm", g * 3072 + 2048, 1024, None, Vd, g * 1024))
        segs.append(("fm", 9216, 1024, AF.Silu, sgT, 0))
        self.inproj_phase(i, src, W, segs)
        with ExitStack() as sub:
            fw.push(sub)
            QT2 = [fw.sbuf([64, S], BF16, f"QT{q}") for q in range(2)]
            KT2 = [fw.sbuf([64, S], BF16, f"KT{q}") for q in range(2)]
            VT2 = [fw.sbuf([128, 32, 64], BF16, f"VT{q}") for q in range(2)]
            SG = fw.sbuf([64, S], BF16, "SG")
            YO = fw.sbuf([64, S], BF16, "YO")
            UA = fw.sbuf([64, S], F32, "UA")
            DA = fw.sbuf([64, S], F32, "DA")
            BTz2 = [fw.sbuf([128, 1024], F32, f"BTz{g}") for g in range(2)]
            HS = fw.sbuf([128, 1024], F32, "HSd")
            TMP = Rot([fw.sbuf([128, 512], F32, f"TMP{q}") for q in range(2)])
            PT = Rot([fw.sbuf([128, 512], BF16, f"PT{q}") for q in range(6)])
            rotS = Rot(self.ps[0:4])
            rotU = Rot(self.ps[4:6])
            rotD = Rot(self.ps[6:8])
            nheads = DBG_HEADS
            LA = 3
            dils = (1, 4, 16)

            def load_g(h, g, slot):
                d = dils[g]
                L = S // d
                nblk = L // 128
                self.load_toeplitz(BTz2[slot], f"dil{g}", h, -384, 1024, HS, "HSd", rotS)
                fw.op("sp", lambda e: e.dma_start(out=QT2[slot][:, :], in_=qkT[g * 2048 + h * 64:g * 2048 + (h + 1) * 64, :]), w=[QT2[slot]], dsem=f"QT{slot}")
                fw.op("sp", lambda e: e.dma_start(out=KT2[slot][:, :], in_=qkT[g * 2048 + 1024 + h * 64:g * 2048 + 1024 + (h + 1) * 64, :]), w=[KT2[slot]], dsem=f"KT{slot}")
                vsrc = Vd[:, g * 1024 + h * 64:g * 1024 + (h + 1) * 64].rearrange("(jb p dd) c -> dd p jb c", p=128, dd=d)
                for r in range(d):
                    fw.op("sp", lambda e, r=r: e.dma_start(out=VT2[slot][:, r * nblk:(r + 1) * nblk, :], in_=vsrc[r]), w=[VT2[slot]], dsem=f"VT{slot}")

            hg = [(h, g) for h in range(nheads) for g in range(3)]
            load_g(0, 0, 0)
            for hgi, (h, g) in enumerate(hg):
                slot = hgi % 2
                if g == 0:
                    fw.op("sp", lambda e, h=h: e.dma_start(out=SG[:, :], in_=sgT[h * 64:(h + 1) * 64, :]), w=[SG], dsem="SG")
                if hgi + 1 < len(hg):
                    load_g(hg[hgi + 1][0], hg[hgi + 1][1], 1 - slot)
                d = dils[g]
                L = S // d
                nblk = L // 128
                Nq = min(256, L)
                QT, KT, VT, BT = QT2[slot], KT2[slot], VT2[slot], BTz2[slot]
                items = []
                for r in range(d):
                    for q0 in range(0, L, Nq):
                        kbs = [kb for kb in range(q0 // 128 - 1, (q0 + Nq) // 128) if kb >= 0]
                        grp = {"qcols": slice(r + d * q0, r + d * (q0 + Nq - 1) + 1, d)}
                        for ix, kb in enumerate(kbs):
                            items.append((grp, r, q0, kb, ix == 0, ix == len(kbs) - 1))

                def s_stage(it, d=d, Nq=Nq, KT=KT, QT=QT, BT=BT):
                    grp, r, q0, kb, first, last = it
                    qcols = grp["qcols"]
                    psS, _ = rotS.next()
                    kcols = slice(r + d * kb * 128, r + d * (kb * 128 + 127) + 1, d)
                    boff = q0 - kb * 128 + 384
                    tm, _ = TMP.next()
                    pt, _ = PT.next()
                    fw.op("pe", lambda e: e.matmul(psS[:, :Nq], lhsT=KT[:, kcols], rhs=QT[:, qcols], start=True, stop=True), r=[KT, QT], w=[psS])
                    fw.op("dve", lambda e: e.scalar_tensor_tensor(out=tm[:, :Nq], in0=psS[:, :Nq], scalar=0.125, in1=BT[:, boff:boff + Nq], op0=ALU.mult, op1=ALU.add), r=[psS, BT], w=[tm])
                    fw.op("act", lambda e: e.activation(out=pt[:, :Nq], in_=tm[:, :Nq], func=AF.Exp), r=[tm], w=[pt])
                    return pt

                def pv_stage(it, pt, Nq=Nq, VT=VT, g=g, nblk=nblk):
                    grp, r, q0, kb, first, last = it
                    qcols = grp["qcols"]
                    if first:
                        grp["psU"], _ = rotU.next()
                        grp["psD"], _ = rotD.next()
                    psU, psD = grp["psU"], grp["psD"]
                    blk = r * nblk + kb
                    fw.op("pe", lambda e: e.matmul(psU[:64, :Nq], lhsT=VT[:, blk, :], rhs=pt[:, :Nq], start=first, stop=last), r=[VT, pt], w=[psU])
                    fw.op("pe", lambda e: e.matmul(psD[:64, :Nq], lhsT=self.ones_bf[:, :64], rhs=pt[:, :Nq], start=first, stop=last), r=[self.ones_bf, pt], w=[psD])
                    if last:
                        if g == 0:
                            fw.op("act", lambda e: e.activation(out=UA[:, qcols], in_=psU[:64, :Nq], func=AF.Copy), r=[psU], w=[UA])
                            fw.op("dve", lambda e: e.tensor_copy(out=DA[:, qcols], in_=psD[:64, :Nq]), r=[psD], w=[DA])
                        else:
                            fw.op("dve", lambda e: e.tensor_tensor(out=UA[:, qcols], in0=UA[:, qcols], in1=psU[:64, :Nq], op=ALU.add), r=[psU, UA], w=[UA])
                            fw.op("dve", lambda e: e.tensor_tensor(out=DA[:, qcols], in0=DA[:, qcols], in1=psD[:64, :Nq], op=ALU.add), r=[psD, DA], w=[DA])

                pts = {}
                n = len(items)
                for ii in range(n + LA):
                    if ii < n:
                        pts[ii] = s_stage(items[ii])
                    if ii >= LA:
                        pv_stage(items[ii - LA], pts.pop(ii - LA))
                if g < 2:
                    continue
                fw.op("dve", lambda e: e.reciprocal(out=DA[:, :], in_=DA[:, :]), r=[DA], w=[DA])
                fw.op("pool", lambda e: e.tensor_tensor(out=UA[:, :], in0=UA[:, :], in1=DA[:, :], op=ALU.mult), r=[UA, DA], w=[UA])
                fw.op("pool", lambda e: e.tensor_tensor(out=YO[:, :], in0=UA[:, :], in1=SG[:, :], op=ALU.mult), r=[UA, SG], w=[YO])
                fw.op("sp", lambda e, h=h: e.dma_start(out=yT[h * 64:(h + 1) * 64, :], in_=YO[:, :]), r=[YO], dsem="YO")
            fw.pop()
        self.post_all(i, self.b_w_out[0], 8, yT, src, dst)

    def layer_nsa(self, i, src, dst):
        fw = self.fw
        W = self.c_w_in[0]
        SC = 128.0 ** -0.5
        BIGS = 30000.0 / SC
        qT = fw.dram([2048, S], BF16, "qTn")
        kvcT = fw.dram([1024, S], BF16, "kvcT")
        ksT = fw.dram([512, S], BF16, "ksT")
        kwT = fw.dram([512, S], BF16, "kwT")
        vsD = fw.dram([S, 512], BF16, "vsD")
        vwD = fw.dram([S, 512], BF16, "vwD")
        bgT = fw.dram([48, S], F32, "bgT")
        sgT = fw.dram([2048, S], BF16, "sgTn")
        yT = fw.dram([2048, S], BF16, "yTn")
        ocT = fw.dram([2048, S], F32, "ocT")
        negT = fw.dram([4, 64, S], BF16, "negT")
        segs = [("fm", 0, 2048, AF.Copy, qT, 0), ("fm", 2048, 1024, AF.Copy, kvcT, 0), ("fm", 3072, 512, AF.Copy, ksT, 0),
                ("tm", 3584, 512, None, vsD, 0), ("fm", 4096, 512, AF.Copy, kwT, 0), ("tm", 4608, 512, None, vwD, 0),
                ("fm", 5120, 48, AF.Sigmoid, bgT, 0), ("fm", 5168, 2048, AF.Silu, sgT, 0)]
        self.inproj_phase(i, src, W, segs)
        NTL = min(NTI, DBG_TILES)
        with ExitStack() as outer:
            fw.push(outer)
            KCMP = fw.sbuf([128, 4, 256], BF16, "KCMP")
            VCMP = fw.sbuf([128, 4, 2, 128], BF16, "VCMP")
            with ExitStack() as sub:
                fw.push(sub)
                KC = fw.sbuf([128, 4, S], BF16, "KC")
                W1 = fw.sbuf([128, 32, 512], BF16, "W1")
                W2 = fw.sbuf([128, 4, 128], BF16, "W2")
                KCP = Rot([fw.sbuf([128, 1020], BF16, f"KCP{q}") for q in range(3)])
                HG = fw.sbuf([128, 4, 1020], BF16, "HG")
                tA = fw.sbuf([128, 1020], F32, "tA")
                tB = fw.sbuf([128, 1020], F32, "tB")
                for which in range(2):
                    w1 = (self.c_w1_k, self.c_w1_v)[which][0]
                    w2 = (self.c_w2_k, self.c_w2_v)[which][0]
                    posn = ("posk", "posv")[which]
                    fw.op("sp", lambda e, which=which: e.dma_start(out=KC[:, :, :], in_=kvcT[which * 512:(which + 1) * 512, :].rearrange("(g p) n -> p g n", p=128)), w=[KC], dsem="KC")
                    fw.op("pool", lambda e, w1=w1: e.dma_start(out=W1[:, :, :], in_=w1.rearrange("(l p) n -> p l n", p=128)), w=[W1], dsem="W1")
                    fw.op("pool", lambda e, w2=w2: e.dma_start(out=W2[:, :, :], in_=w2.rearrange("(m p) n -> p m n", p=128)), w=[W2], dsem="W2")
                    for m in range(4):
                        psA, psB = self.ps[0], self.ps[1]
                        for l in range(32):
                            kcp, _ = KCP.next()
                            fw.op("dve", lambda e, kcp=kcp, l=l, posn=posn: e.tensor_scalar(out=kcp.t[:, :].rearrange("p (g c) -> p g c", g=4), in0=KC[:, :, l:l + 16 * 254 + 1:16], scalar1=self.vcol(posn, l), scalar2=None, op0=ALU.add),
                                  r=[KC, self.vecs], w=[kcp])
                            fw.op("pe", lambda e, kcp=kcp, l=l, m=m: e.matmul(psA[:, :510], lhsT=W1[:, l, m * 128:(m + 1) * 128], rhs=kcp[:, 0:510], start=(l == 0), stop=(l == 31)), r=[W1, kcp], w=[psA])
                            fw.op("pe", lambda e, kcp=kcp, l=l, m=m: e.matmul(psB[:, :510], lhsT=W1[:, l, m * 128:(m + 1) * 128], rhs=kcp[:, 510:1020], start=(l == 0), stop=(l == 31)), r=[W1, kcp], w=[psB])
                        for half, ps in enumerate((psA, psB)):
                            hs = slice(half * 510, (half + 1) * 510)
                            fw.op("act", lambda e, ps=ps, hs=hs: e.activation(out=tA[:, hs], in_=ps[:, :510], func=AF.Square), r=[ps], w=[tA])
                            fw.op("dve", lambda e, hs=hs: e.tensor_scalar(out=tA[:, hs], in0=tA[:, hs], scalar1=0.044715, scalar2=1.0, op0=ALU.mult, op1=ALU.add), r=[tA], w=[tA])
                            fw.op("dve", lambda e, ps=ps, hs=hs: e.tensor_tensor(out=tA[:, hs], in0=tA[:, hs], in1=ps[:, :510], op=ALU.mult), r=[tA, ps], w=[tA])
                            fw.op("act", lambda e, hs=hs: e.activation(out=tB[:, hs], in_=tA[:, hs], func=AF.Sigmoid, scale=1.5957691216057308), r=[tA], w=[tB])
                            fw.op("dve", lambda e, ps=ps, hs=hs, m=m: e.tensor_tensor(out=HG[:, m, hs], in0=tB[:, hs], in1=ps[:, :510], op=ALU.mult), r=[tB, ps], w=[HG])
                    if which == 0:
                        for half in range(2):
                            ps = self.ps[2 + half]
                            for m in range(4):
                                fw.op("pe", lambda e, ps=ps, m=m, half=half: e.matmul(ps[:, :510], lhsT=W2[:, m, :], rhs=HG[:, m, half * 510:(half + 1) * 510], start=(m == 0), stop=(m == 3)), r=[W2, HG], w=[ps])
                            for gg in range(2):
                                fw.op("act", lambda e, ps=ps, half=half, gg=gg: e.activation(out=KCMP[:, 2 * half + gg, 0:255], in_=ps[:, gg * 255:(gg + 1) * 255], func=AF.Copy), r=[ps], w=[KCMP])
                    else:
                        for g in range(4):
                            for cb in range(2):
                                csz = 128 if cb == 0 else 127
                                ps, _ = self.psr.next()
                                for m in range(4):
                                    fw.op("pe", lambda e, ps=ps, m=m, g=g, cb=cb, csz=csz: e.matmul(ps[:csz, :128], lhsT=HG[:, m, g * 255 + cb * 128:g * 255 + cb * 128 + csz], rhs=W2[:, m, :], start=(m == 0), stop=(m == 3)), r=[W2, HG], w=[ps])
                                fw.op("act", lambda e, ps=ps, g=g, cb=cb, csz=csz: e.activation(out=VCMP[:csz, g, cb, :], in_=ps[:csz, :128], func=AF.Copy), r=[ps], w=[VCMP])
                fw.pop()
            with ExitStack() as sub:
                fw.push(sub)
                QG = fw.sbuf([128, 4, S], BF16, "QG")
                CM = fw.sbuf([128, 8, 2, 512], F32, "CM")
                COV = fw.sbuf([128, 2, 64], BF16, "COV")
                ADDT = fw.sbuf([128, 32, 64], F32, "ADDT")
                IDENT = fw.sbuf([128, 128], BF16, "IDENT")
                PN = fw.sbuf([128, 4, 2, 512], BF16, "PN")
                TMP = Rot([fw.sbuf([128, 512], F32, f"TMPc{q}") for q in range(2)])
                PT = Rot([fw.sbuf([128, 512], BF16, f"PTc{q}") for q in range(6)])
                RD = Rot([fw.sbuf([128, 512], F32, f"RDc{q}") for q in range(2)])
                GT = Rot([fw.sbuf([128, 512], F32, f"GTc{q}") for q in range(2)])
                OST = Rot([fw.sbuf([128, 512], F32, f"OST{q}") for q in range(2)])
                NEGS = Rot([fw.sbuf([64, 512], BF16, f"NEGS{q}") for q in range(2)])
                IMP = fw.sbuf([128, 64], F32, "IMP")
                WK = fw.sbuf([128, 64], F32, "WK")
                M8 = fw.sbuf([128, 16], F32, "M8")
                SEL = fw.sbuf([128, 64], F32, "SEL")
                NS = Rot([fw.sbuf([128, 64], BF16, f"NS{q}") for q in range(2)])
                fw.op("sp", lambda e: e.dma_start(out=CM[:, :, :, :], in_=self.cm), w=[CM], dsem="CM")
                fw.op("sp", lambda e: e.dma_start(out=ADDT[:, :, :], in_=self.addt), w=[ADDT], dsem="ADDT")
                fw.op("pool", lambda e: e.dma_start(out=COV[:, :, :], in_=self.cov), w=[COV], dsem="COV")
                fw.op("pool", lambda e: e.dma_start(out=IDENT[:, :], in_=self.ident), w=[IDENT], dsem="IDENT")
                rotS = Rot(self.ps[0:2])
                rotU = Rot(self.ps[2:4])
                rotD = Rot(self.ps[4:6])
                psI, psT = self.ps[6], self.ps[7]
                for g in range(4):
                    fw.op("sp", lambda e, g=g: e.dma_start(out=QG[:, :, :], in_=qT[g * 512:(g + 1) * 512, :].rearrange("(r p) n -> p r n", p=128)), w=[QG], dsem="QG")
                    for j in range(NTL):
                        tc = slice(j * NT, (j + 1) * NT)
                        cbs = [0] if j < 4 else [0, 1]
                        items = []
                        for r in range(4):
                            grp = {"r": r, "pts": []}
                            for ix, cb in enumerate(cbs):
                                items.append((grp, cb, ix == 0, ix == len(cbs) - 1))

                        def s_stage(it, g=g, j=j, tc=tc):
                            grp, cb, first, last = it
                            r = grp["r"]
                            csz = 128 if cb == 0 else 127
                            psS, _ = rotS.next()
                            tm, _ = TMP.next()
                            pt, _ = PT.next()
                            grp["pts"].append(pt)
                            fw.op("pe", lambda e: e.matmul(psS[:csz, :], lhsT=KCMP[:, g, cb * 128:cb * 128 + csz], rhs=QG[:, r, tc], start=True, stop=True), r=[KCMP, QG], w=[psS])
                            fw.op("dve", lambda e: e.scalar_tensor_tensor(out=tm[:csz, :], in0=psS[:csz, :], scalar=SC, in1=CM[:csz, j, cb, :], op0=ALU.mult, op1=ALU.add), r=[psS, CM], w=[tm])
                            fw.op("act", lambda e: e.activation(out=pt[:csz, :], in_=tm[:csz, :], func=AF.Exp), r=[tm], w=[pt])
                            return pt

                        def pv_stage(it, pt, g=g, j=j, tc=tc, cbs=cbs):
                            grp, cb, first, last = it
                            r = grp["r"]
                            h = 4 * g + r
                            csz = 128 if cb == 0 else 127
                            if first:
                                grp["psU"], _ = rotU.next()
                                grp["psD"], _ = rotD.next()
                            psU_, psD_ = grp["psU"], grp["psD"]
                            fw.op("pe", lambda e: e.matmul(psU_[:, :], lhsT=VCMP[:csz, g, cb, :], rhs=pt[:csz, :], start=first, stop=last), r=[VCMP, pt], w=[psU_])
                            fw.op("pe", lambda e: e.matmul(psD_[:, :], lhsT=self.ones_bf[:csz, :], rhs=pt[:csz, :], start=first, stop=last), r=[self.ones_bf, pt], w=[psD_])
                            if not last:
                                return
                            rd, _ = RD.next()
                            fw.op("dve", lambda e: e.tensor_scalar(out=rd[:, :], in0=psD_[:, :], scalar1=1e-30, scalar2=None, op0=ALU.max), r=[psD_], w=[rd])
                            fw.op("dve", lambda e: e.reciprocal(out=rd[:, :], in_=rd[:, :]), r=[rd], w=[rd])
                            for ix, cb2 in enumerate(cbs):
                                csz2 = 128 if cb2 == 0 else 127
                                p2 = grp["pts"][ix]
                                fw.op("pool", lambda e, p2=p2, cb2=cb2, csz2=csz2: e.tensor_tensor(out=PN[:csz2, r, cb2, :], in0=p2[:csz2, :], in1=rd[:csz2, :], op=ALU.mult), r=[p2, rd], w=[PN])
                            gt, gi = GT.next()
                            fw.op("sp", lambda e: e.dma_start(out=gt[:, :], in_=bass.AP(bgT.tensor, (h * 3 + 0) * S + j * NT, [[0, 128], [1, NT]])), w=[gt], dsem=f"GTc{gi}")
                            ost, oi = OST.next()
                            fw.op("pool", lambda e: e.tensor_tensor(out=gt[:, :], in0=gt[:, :], in1=rd[:, :], op=ALU.mult), r=[gt, rd], w=[gt])
                            fw.op("dve", lambda e: e.tensor_tensor(out=ost[:, :], in0=gt[:, :], in1=psU_[:, :], op=ALU.mult), r=[gt, psU_], w=[ost])
                            fw.op("sp", lambda e: e.dma_start(out=ocT[h * 128:(h + 1) * 128, tc], in_=ost[:, :]), r=[ost], dsem=f"OST{oi}")

                        LAc = 1
                        ptd = {}
                        for ii in range(len(items) + LAc):
                            if ii < len(items):
                                ptd[ii] = s_stage(items[ii])
                            if ii >= LAc:
                                pv_stage(items[ii - LAc], ptd.pop(ii - LAc))
                        for qb in range(4):
                            n_mm = 4 * len(cbs)
                            ix = 0
                            for r in range(4):
                                for cb in cbs:
                                    csz = 128 if cb == 0 else 127
                                    fw.op("pe", lambda e, r=r, cb=cb, csz=csz, qb=qb, ix=ix, n_mm=n_mm: e.matmul(psI[:, :64], lhsT=PN[:csz, r, cb, qb * 128:(qb + 1) * 128], rhs=COV[:csz, cb, :], start=(ix == 0), stop=(ix == n_mm - 1)), r=[PN, COV], w=[psI])
                                    ix += 1
                            fw.op("dve", lambda e, j=j, qb=qb: e.tensor_tensor(out=IMP[:, :], in0=psI[:, :64], in1=ADDT[:, j * 4 + qb, :], op=ALU.add), r=[psI, ADDT], w=[IMP])
                            fw.op("dve", lambda e: e.max(out=M8[:, 0:8], in_=IMP[:, :]), r=[IMP], w=[M8])
                            fw.op("dve", lambda e: e.match_replace(out=WK[:, :], in_to_replace=M8[:, 0:8], in_values=IMP[:, :], imm_value=-3.0e38), r=[IMP, M8], w=[WK])
                            fw.op("dve", lambda e: e.max(out=M8[:, 8:16], in_=WK[:, :]), r=[WK], w=[M8])
                            fw.op("dve", lambda e: e.tensor_scalar(out=SEL[:, :], in0=IMP[:, :], scalar1=M8[:, 15:16], scalar2=None, op0=ALU.is_ge), r=[IMP, M8], w=[SEL])
                            ns, _ = NS.next()
                            fw.op("dve", lambda e, ns=ns: e.tensor_scalar(out=ns[:, :], in0=SEL[:, :], scalar1=-1.0, scalar2=BIGS, op0=ALU.add, op1=ALU.mult), r=[SEL], w=[ns])
                            fw.op("pe", lambda e, ns=ns, qb=qb: e.matmul(psT[:64, qb * 128:(qb + 1) * 128], lhsT=ns[:, :], rhs=IDENT[:, :], start=True, stop=True), r=[ns, IDENT], w=[psT])
                        negs, ni = NEGS.next()
                        fw.op("act", lambda e, negs=negs: e.activation(out=negs[:, :], in_=psT[:64, :], func=AF.Copy), r=[psT], w=[negs])
                        fw.op("sp", lambda e, negs=negs, g=g, tc=tc: e.dma_start(out=negT[g, :, tc], in_=negs[:, :]), r=[negs], dsem=f"NEGS{ni}")
                fw.pop()
            with ExitStack() as sub:
                fw.push(sub)
                Q1 = fw.sbuf([128, S], BF16, "Q1")
                KS = fw.sbuf([128, S], BF16, "KS")
                KW = fw.sbuf([128, S], BF16, "KW")
                VS = fw.sbuf([128, 32, 128], BF16, "VS")
                VW = fw.sbuf([128, 32, 128], BF16, "VW")
                NEGT = fw.sbuf([64, S], BF16, "NEGT")
                EX = fw.sbuf([64, 32, 128], BF16, "EX")
                WSEL = fw.sbuf([128, 2432], F32, "WSEL")
                WWIN = fw.sbuf([128, 1408], F32, "WWIN")
                HS = fw.sbuf([128, 2432], F32, "HSn")
                OACC = fw.sbuf([128, S], F32, "OACC")
                SGP = fw.sbuf([128, S], BF16, "SGP")
                YO = fw.sbuf([128, S], BF16, "YOn")
                RB31 = fw.sbuf([128, 16], F32, "RB31")
                GT = Rot([fw.sbuf([128, 512], F32, f"GTs{q}") for q in range(3)])
                TMP = Rot([fw.sbuf([128, 512], F32, f"TMPs{q}") for q in range(2)])
                PT = Rot([fw.sbuf([128, 512], BF16, f"PTs{q}") for q in range(6)])
                RD = Rot([fw.sbuf([128, 512], F32, f"RDs{q}") for q in range(2)])
                fw.op("pool", lambda e: e.dma_start(out=EX[:, :, :], in_=self.ex), w=[EX], dsem="EX")
                fw.op("sp", lambda e: e.dma_start(out=RB31[:, :], in_=bass.AP(self.relb.tensor, 31 * 16, [[0, 128], [1, 16]])), w=[RB31], dsem="RB31")
                rotS = Rot(self.ps[0:4])
                rotU = Rot(self.ps[4:6])
                rotD = Rot(self.ps[6:8])
                ngr = (DBG_HEADS + 3) // 4
                for g in range(ngr):
                    fw.op("sp", lambda e, g=g: e.dma_start(out=KS[:, :], in_=ksT[g * 128:(g + 1) * 128, :]), w=[KS], dsem="KS")
                    fw.op("sp", lambda e, g=g: e.dma_start(out=KW[:, :], in_=kwT[g * 128:(g + 1) * 128, :]), w=[KW], dsem="KW")
                    fw.op("sp", lambda e, g=g: e.dma_start(out=VS[:, :, :], in_=vsD[:, g * 128:(g + 1) * 128].rearrange("(jb p) c -> p jb c", p=128)), w=[VS], dsem="VS")
                    fw.op("sp", lambda e, g=g: e.dma_start(out=VW[:, :, :], in_=vwD[:, g * 128:(g + 1) * 128].rearrange("(jb p) c -> p jb c", p=128)), w=[VW], dsem="VW")
                    fw.op("sp", lambda e, g=g: e.dma_start(out=NEGT[:, :], in_=negT[g]), w=[NEGT], dsem="NEGT")
                    for r in range(min(4, DBG_HEADS)):
                        h = 4 * g + r
                        fw.op("sp", lambda e, h=h: e.dma_start(out=Q1[:, :], in_=qT[h * 128:(h + 1) * 128, :]), w=[Q1], dsem="Q1")
                        fw.op("sp", lambda e, h=h: e.dma_start(out=OACC[:, :], in_=ocT[h * 128:(h + 1) * 128, :]), w=[OACC], dsem="OACC")
                        fw.op("sp", lambda e, h=h: e.dma_start(out=SGP[:, :], in_=sgT[h * 128:(h + 1) * 128, :]), w=[SGP], dsem="SGP")
                        self.load_toeplitz(WSEL, "sel", h, -384, 2432, HS, "HSn", rotS)
                        self.load_toeplitz(WWIN, "win", h, -384, 1408, HS, "HSn", rotS)
                        items = []
                        for j in range(NTL):
                            for br in (1, 2):
                                if br == 1:
                                    kbs = list(range(0, 4 * j + 4))
                                else:
                                    kbs = list(range(max(0, 4 * j - 4), 4 * j + 4))
                                grp = {"j": j, "br": br}
                                for ix, kb in enumerate(kbs):
                                    items.append((grp, kb, ix == 0, ix == len(kbs) - 1))

                        def s_stage(it, h=h):
                            grp, kb, first, last = it
                            j, br = grp["j"], grp["br"]
                            t0 = j * NT
                            tc = slice(t0, t0 + NT)
                            KK, WW = (KS, WSEL) if br == 1 else (KW, WWIN)
                            if first:
                                gt, gi = GT.next()
                                grp["gt"] = gt
                                fw.op("sp", lambda e: e.dma_start(out=gt[:, :], in_=bass.AP(bgT.tensor, (h * 3 + br) * S + t0, [[0, 128], [1, NT]])), w=[gt], dsem=f"GTs{gi}")
                            d0 = t0 - kb * 128
                            psS, _ = rotS.next()
                            pt, _ = PT.next()
                            kc_ = slice(kb * 128, (kb + 1) * 128)
                            if br == 1:
                                fw.op("pe", lambda e: e.matmul(psS[:, :], lhsT=KK[:, kc_], rhs=Q1[:, tc], start=True, stop=False), r=[KK, Q1], w=[psS])
                                fw.op("pe", lambda e: e.matmul(psS[:, :], lhsT=EX[:, kb, :], rhs=NEGT[:, tc], start=False, stop=True), r=[EX, NEGT], w=[psS])
                            else:
                                fw.op("pe", lambda e: e.matmul(psS[:, :], lhsT=KK[:, kc_], rhs=Q1[:, tc], start=True, stop=True), r=[KK, Q1], w=[psS])
                            if br == 1 and d0 >= 1664:
                                fw.op("act", lambda e: e.activation(out=pt[:, :], in_=psS[:, :], func=AF.Exp, scale=SC, bias=RB31[:, h:h + 1]), r=[psS, RB31], w=[pt])
                            else:
                                tm, _ = TMP.next()
                                fw.op("dve", lambda e: e.scalar_tensor_tensor(out=tm[:, :], in0=psS[:, :], scalar=SC, in1=WW[:, d0 + 384:d0 + 384 + NT], op0=ALU.mult, op1=ALU.add), r=[psS, WW], w=[tm])
                                fw.op("act", lambda e: e.activation(out=pt[:, :], in_=tm[:, :], func=AF.Exp), r=[tm], w=[pt])
                            return pt

                        def pv_stage(it, pt):
                            grp, kb, first, last = it
                            j, br = grp["j"], grp["br"]
                            tc = slice(j * NT, (j + 1) * NT)
                            VV = VS if br == 1 else VW
                            if first:
                                grp["psU"], _ = rotU.next()
                                grp["psD"], _ = rotD.next()
                            psU, psD, gt = grp["psU"], grp["psD"], grp["gt"]
                            fw.op("pe", lambda e: e.matmul(psU[:, :], lhsT=VV[:, kb, :], rhs=pt[:, :], start=first, stop=last), r=[VV, pt], w=[psU])
                            fw.op("pe", lambda e: e.matmul(psD[:, :], lhsT=self.ones_bf[:, :], rhs=pt[:, :], start=first, stop=last), r=[self.ones_bf, pt], w=[psD])
                            if last:
                                rd, _ = RD.next()
                                fw.op("dve", lambda e: e.reciprocal(out=rd[:, :], in_=psD[:, :]), r=[psD], w=[rd])
                                fw.op("pool", lambda e: e.tensor_tensor(out=rd[:, :], in0=rd[:, :], in1=gt[:, :], op=ALU.mult), r=[rd, gt], w=[rd])
                                fw.op("dve", lambda e: e.tensor_tensor(out=rd[:, :], in0=rd[:, :], in1=psU[:, :], op=ALU.mult), r=[rd, psU], w=[rd])
                                fw.op("pool", lambda e: e.tensor_tensor(out=OACC[:, tc], in0=OACC[:, tc], in1=rd[:, :], op=ALU.add), r=[rd, OACC], w=[OACC])

                        LA = 3
                        pts = {}
                        n = len(items)
                        for ii in range(n + LA):
                            if ii < n:
                                pts[ii] = s_stage(items[ii])
                            if ii >= LA:
                                pv_stage(items[ii - LA], pts.pop(ii - LA))
                        fw.op("pool", lambda e: e.tensor_tensor(out=YO[:, :], in0=OACC[:, :], in1=SGP[:, :], op=ALU.mult), r=[OACC, SGP], w=[YO])
                        fw.op("sp", lambda e, h=h: e.dma_start(out=yT[h * 128:(h + 1) * 128, :], in_=YO[:, :]), r=[YO], dsem="YOn")
                fw.pop()
            fw.pop()
        self.post_all(i, self.c_w_out[0], 16, yT, src, dst)


_CACHE = {}


def _prep(inputs, layers=(0, 1, 2, 3), ncores=8):
    vecs, voff = _pack_vecs(inputs)
    key = (tuple(layers), vecs.shape[1])
    if key not in _CACHE:
        _CACHE[key] = Builder(voff, vecs.shape[1], layers)
    b = _CACHE[key]
    oh, cov, addt, cm, ex, ident = _host_tables()
    relb = np.concatenate([np.asarray(inputs["rel_bias"], np.float32), np.full((1, 16), NEG, np.float32)], axis=0)
    shared = {"vecs": vecs, "relb": relb, "oh": oh, "cov": cov, "addt": addt, "cm": cm, "ex": ex, "ident": ident,
              "jrev": np.ascontiguousarray(ident[::-1])}
    for k in ("ple_w_proj", "ple_w_gate", "a_w_in", "a_w_r", "a_w_i", "a_w_out", "b_w_in", "b_w_out", "c_w_in",
              "c_cmp_w1_k", "c_cmp_w2_k", "c_cmp_w1_v", "c_cmp_w2_v", "c_w_out"):
        shared[k] = np.ascontiguousarray(inputs[k], dtype=np.float32)
    x = np.asarray(inputs["x"], np.float32)
    p = np.asarray(inputs["p"], np.float32)
    in_maps = []
    for c in range(ncores):
        m = dict(shared)
        m["xT"] = np.ascontiguousarray(x[c].T)
        m["pT"] = np.ascontiguousarray(p[:, c].transpose(0, 2, 1))
        in_maps.append({k: v for k, v in m.items() if k in b.input_names})
    return b, in_maps


def kernel(**inputs):
    b, in_maps = _prep(inputs)
    res = run_bass_kernel_spmd(b.nc, in_maps, core_ids=list(range(8)))
    out = np.stack([np.ascontiguousarray(r["outT"].T) for r in res.results], axis=0)
    return out.astype(np.float32)
```

```python
import math
from contextlib import ExitStack
import numpy as np
import concourse.bass as bass
import concourse.mybir as mybir
from concourse.bass_utils import run_bass_kernel_spmd

F32 = mybir.dt.float32
BF16 = mybir.dt.bfloat16
AF = mybir.ActivationFunctionType
ALU = mybir.AluOpType

S = 4096
D = 2048
NT = 512
NTI = S // NT
import os
DBG_TILES = int(os.environ.get('DBG_TILES', '8'))
DBG_HEADS = int(os.environ.get('DBG_HEADS', '16'))
DBG_DUMP = os.environ.get('DBG_DUMP', '') != ''
DEPTH = 4
NEG = -30000.0
EPS = 1e-6


class Buf:
    def __init__(self, t, name):
        self.t = t
        self.name = name
        self.writers = {}
        self.readers = {}

    def __getitem__(self, k):
        return self.t[k]


class View:
    def __init__(self, base, t):
        self.base = base
        self.t = t
        self.name = base.name + "_v"

    def __getitem__(self, k):
        return self.t[k]

    @property
    def writers(self):
        return self.base.writers

    @writers.setter
    def writers(self, v):
        self.base.writers = v

    @property
    def readers(self):
        return self.base.readers

    @readers.setter
    def readers(self, v):
        self.base.readers = v


class FW:
    ENGS = ("pe", "act", "dve", "pool", "sp")

    def __init__(self, nc, ctx):
        self.nc = nc
        self.ctx = ctx
        self.stack = [ctx]
        self.lists = {e: [] for e in self.ENGS}
        self.waited = {e: {} for e in self.ENGS}
        self.sems = {}
        self.count = {}
        for e in ("pe", "act", "dve", "pool"):
            self.getsem("E_" + e)
        self.nbuf = 0
        self.ninst = 0

    def getsem(self, key):
        if key not in self.sems:
            self.sems[key] = self.ctx.enter_context(self.nc.semaphore("s_" + key))
            self.count[key] = 0
        return key

    def push(self, sub):
        self.stack.append(sub)

    def pop(self):
        self.barrier()
        self.stack.pop()

    def sbuf(self, shape, dtype, name=None):
        self.nbuf += 1
        name = (name or "sb") + f"_{self.nbuf}"
        t = self.stack[-1].enter_context(self.nc.sbuf_tensor(name, list(shape), dtype))
        return Buf(t, name)

    def psum(self, shape, dtype, name=None):
        self.nbuf += 1
        name = (name or "ps") + f"_{self.nbuf}"
        t = self.stack[-1].enter_context(self.nc.psum_tensor(name, list(shape), dtype))
        return Buf(t, name)

    def dram(self, shape, dtype, name=None):
        self.nbuf += 1
        name = (name or "dr") + f"_{self.nbuf}"
        if DBG_DUMP:
            t = self.nc.dram_tensor(name.rsplit("_", 1)[0], list(shape), dtype, kind="ExternalOutput")
        else:
            t = self.nc.dram_tensor(name, list(shape), dtype, kind="Internal")
        return t.ap()

    def op(self, eng, fn, r=(), w=(), dsem=None):
        E = self.lists[eng]
        needs = {}

        def upd(d):
            for s, v in d.items():
                if needs.get(s, 0) < v:
                    needs[s] = v

        for b in r:
            upd(b.writers)
        for b in w:
            if eng == "pe":
                upd({s: v for s, v in b.writers.items() if s != "E_pe"})
            else:
                upd(b.writers)
            upd(b.readers)
        if dsem is not None:
            sem = self.getsem("D_" + dsem)
            inc = 16
            if self.count[sem] > 0:
                upd({sem: self.count[sem]})
        else:
            sem = "E_" + eng
            inc = 1
        wd = self.waited[eng]
        waits = []
        for s, v in needs.items():
            if wd.get(s, 0) < v:
                wd[s] = v
                waits.append((s, v))
        self.count[sem] += inc
        val = self.count[sem]
        E.append((waits, fn, sem, val, inc))
        self.ninst += 1 + len(waits)
        for b in r:
            if b.readers.get(sem, 0) < val:
                b.readers[sem] = val
        for b in w:
            b.writers = {sem: val}
            b.readers = {}

    def barrier(self):
        for e in self.ENGS:
            wd = self.waited[e]
            waits = []
            for s, v in self.count.items():
                if v > 0 and wd.get(s, 0) < v:
                    wd[s] = v
                    waits.append((s, v))
            if waits:
                self.lists[e].append((waits, None, None, 0, 0))

    def emit(self):
        nc = self.nc
        sems = self.sems
        lists = self.lists
        targets = {}
        for e in self.ENGS:
            for waits, fn, sem, val, inc in lists[e]:
                for s, v in waits:
                    targets.setdefault(s, set()).add(v)
        remap = {}
        for s, vals in targets.items():
            if s.startswith("E_"):
                remap[s] = {v: i + 1 for i, v in enumerate(sorted(vals))}
        self.nsignal = sum(len(m) for m in remap.values())

        def run(e, lst):
            for waits, fn, sem, val, inc in lst:
                for s, v in waits:
                    e.wait_ge(sems[s], remap[s][v] if s in remap else v)
                if fn is not None:
                    ins = fn(e)
                    if sem.startswith("E_"):
                        if val in remap.get(sem, ()):
                            ins.then_inc(sems[sem], 1)
                    else:
                        ins.then_inc(sems[sem], inc)

        with nc.Block() as block:
            @block.tensor
            def _(e):
                run(e, lists["pe"])

            @block.scalar
            def _(e):
                run(e, lists["act"])

            @block.vector
            def _(e):
                run(e, lists["dve"])

            @block.gpsimd
            def _(e):
                run(e, lists["pool"])

            @block.sync
            def _(e):
                run(e, lists["sp"])


class Rot:
    def __init__(self, items):
        self.items = items
        self.i = 0

    def next(self):
        b = self.items[self.i % len(self.items)]
        idx = self.i % len(self.items)
        self.i += 1
        return b, idx


def _bucket_np(n):
    n = np.maximum(n, 0)
    nf = np.maximum(n, 16).astype(np.float32)
    large = 16 + (np.log(nf / np.float32(16)) / np.float32(math.log(2048 / 16)) * np.float32(16)).astype(np.int32)
    return np.where(n < 16, n, np.minimum(large, 31))


TV_KINDS = [("dil0", 1, 128, 1152), ("dil1", 4, 128, 1152), ("dil2", 16, 128, 1152),
            ("sel", 1, 1 << 30, 2688), ("win", 1, 511, 1536)]
TV_OFF = {}
_o = 0
for _n, _d, _m, _x in TV_KINDS:
    TV_OFF[_n] = (_o, _x)
    _o += _x
TV_TOT = _o


def _host_tables():
    oh = np.zeros((33, TV_TOT), np.float32)
    for name, dil, maxd, X in TV_KINDS:
        o, _ = TV_OFF[name]
        x = np.arange(X)
        delta = x - 511
        valid = (delta >= 0) & (delta <= maxd)
        b = _bucket_np(delta * dil)
        for xi in range(X):
            if valid[xi]:
                oh[b[xi], o + xi] = 1.0
            else:
                oh[32, o + xi] = 1.0
    ncmp = 255
    cidx0 = np.arange(ncmp) * 16
    cend = cidx0 + 31
    sel_start = np.arange(64) * 64
    cover = ((cidx0[:, None] < sel_start[None, :] + 64) & (cend[:, None] >= sel_start[None, :])).astype(np.float32)
    cov = np.zeros((256, 64), np.float32)
    cov[:255] = cover
    cov = cov.reshape(2, 128, 64).transpose(1, 0, 2).copy()
    t = np.arange(S)
    cur = t // 64
    n = np.arange(64)
    forced = (n[None, :] == 0) | (n[None, :] == cur[:, None]) | (n[None, :] == cur[:, None] - 1)
    validb = n[None, :] * 64 <= t[:, None]
    addt = np.where(validb, 1e4 * forced.astype(np.float32), -1e30).astype(np.float32)
    addt = addt.reshape(32, 128, 64).transpose(1, 0, 2).copy()
    cm = np.zeros((128, 8, 2, 512), np.float32)
    for j in range(8):
        for cb in range(2):
            c = cb * 128 + np.arange(128)
            tt = j * 512 + np.arange(512)
            ok = (16 * c[:, None] + 31) <= tt[None, :]
            cm[:, j, cb, :] = np.where(ok, 0.0, NEG)
    ex = np.zeros((64, 32, 128), np.float32)
    for kb in range(32):
        for k in range(128):
            ex[2 * kb + k // 64, kb, k] = 1.0
    ident = np.eye(128, dtype=np.float32)
    return oh, cov, addt, cm, ex, ident


def _pack_vecs(inp):
    cols = []
    off = {}

    def add(name, arr):
        off[name] = sum(c.shape[1] for c in cols)
        cols.append(np.ascontiguousarray(arr, dtype=np.float32))

    def fm(v):
        return np.asarray(v, np.float32).reshape(16, 128).T

    for i in range(DEPTH):
        add(f"pre{i}", fm(inp["norm_pre"][i]))
        add(f"post{i}", fm(inp["norm_post"][i]))
    for j in range(inp["a_w_in"].shape[0]):
        for t in range(4):
            add(f"cw{j}_{t}", fm(inp["a_conv_w"][j, t]))
        add(f"cb{j}", fm(inp["a_conv_b"][j]))
        add(f"br{j}", fm(inp["a_b_r"][j].reshape(-1)))
        add(f"bi{j}", fm(inp["a_b_i"][j].reshape(-1)))
        add(f"lam{j}", fm(inp["a_lam"][j]))
    add("posk", np.asarray(inp["c_cmp_pos_k"][0], np.float32).T)
    add("posv", np.asarray(inp["c_cmp_pos_v"][0], np.float32).T)
    return np.concatenate(cols, axis=1), off


class Builder:
    def __init__(self, voff, nvec, layers=(0, 1, 2, 3)):
        self.voff = voff
        self.nvec = nvec
        self.layers = layers
        nc = bass.Bass("TRN2", target_bir_lowering=False)
        self.nc = nc

        self.input_names = []
        kinds = {l % 3 for l in layers}

        def inp(name, shape, dt=F32, need=True):
            if not need:
                return None
            self.input_names.append(name)
            return nc.dram_tensor(name, list(shape), dt, kind="ExternalInput").ap()

        att = (1 in kinds) or (2 in kinds)
        self.xT = inp("xT", [D, S])
        self.pT = inp("pT", [DEPTH, 256, S])
        self.vecs_d = inp("vecs", [128, nvec])
        self.relb = inp("relb", [33, 16], need=att)
        self.oh = inp("oh", [33, TV_TOT], need=att)
        self.cov = inp("cov", [128, 2, 64], need=2 in kinds)
        self.addt = inp("addt", [128, 32, 64], need=2 in kinds)
        self.cm = inp("cm", [128, 8, 2, 512], need=2 in kinds)
        self.ex = inp("ex", [64, 32, 128], need=2 in kinds)
        self.ident = inp("ident", [128, 128], need=2 in kinds)
        self.jrev = inp("jrev", [128, 128], need=att)
        self.ple_w_proj = inp("ple_w_proj", [DEPTH, 256, D])
        self.ple_w_gate = inp("ple_w_gate", [DEPTH, D, D])
        self.a_w_in = inp("a_w_in", [2, D, 2 * D], need=0 in kinds)
        self.a_w_r = inp("a_w_r", [2, 8, 256, 256], need=0 in kinds)
        self.a_w_i = inp("a_w_i", [2, 8, 256, 256], need=0 in kinds)
        self.a_w_out = inp("a_w_out", [2, D, D], need=0 in kinds)
        self.b_w_in = inp("b_w_in", [1, D, 10240], need=1 in kinds)
        self.b_w_out = inp("b_w_out", [1, 1024, D], need=1 in kinds)
        self.c_w_in = inp("c_w_in", [1, D, 7216], need=2 in kinds)
        self.c_w1_k = inp("c_cmp_w1_k", [1, 4096, 512], need=2 in kinds)
        self.c_w2_k = inp("c_cmp_w2_k", [1, 512, 128], need=2 in kinds)
        self.c_w1_v = inp("c_cmp_w1_v", [1, 4096, 512], need=2 in kinds)
        self.c_w2_v = inp("c_cmp_w2_v", [1, 512, 128], need=2 in kinds)
        self.c_w_out = inp("c_w_out", [1, D, D], need=2 in kinds)
        self.outT = nc.dram_tensor("outT", [D, S], F32, kind="ExternalOutput").ap()

        with ExitStack() as ctx:
            fw = FW(nc, ctx)
            self.fw = fw
            self.build()
            fw.barrier()
            fw.emit()
            print("kernel build: instr+waits", fw.ninst, "sems", len(fw.sems), "signals", fw.nsignal)

    def vcol(self, name, k=0, n=1):
        o = self.voff[name] + k
        return self.vecs[:, o:o + n]

    def build(self):
        fw = self.fw
        self.vecs = fw.sbuf([128, self.nvec], F32, "vecs")
        fw.op("sp", lambda e: e.dma_start(out=self.vecs[:, :], in_=self.vecs_d), w=[self.vecs], dsem="vecs")
        self.ones_bf = fw.sbuf([128, 128], BF16, "ones")
        fw.op("pool", lambda e: e.memset(self.ones_bf[:, :], 1.0), w=[self.ones_bf])
        self.cst = fw.sbuf([128, 4], F32, "cst")
        fw.op("pool", lambda e: e.memset(self.cst[:, 0:1], EPS), w=[self.cst])
        fw.op("pool", lambda e: e.memset(self.cst[:, 1:2], 1.0), w=[self.cst])
        fw.op("pool", lambda e: e.memset(self.cst[:, 2:3], 0.25), w=[self.cst])
        if self.jrev is not None:
            self.JREV = fw.sbuf([128, 128], F32, "JREV")
            fw.op("sp", lambda e: e.dma_start(out=self.JREV[:, :], in_=self.jrev), w=[self.JREV], dsem="JREV")
        self.ps = [fw.psum([128, 512], F32, f"psb{i}") for i in range(8)]
        self.psr = Rot(self.ps[:6])
        self.psS = self.ps[6]
        self.psX = self.ps[7]
        self.wcache = {}
        self.wq = "pool"
        self.pref = {}
        self.nwc = 0
        self.wt = Rot([fw.sbuf([128, 16, 512], BF16, f"wt{i}") for i in range(2)])
        hA = fw.dram([D, S], F32, "hA")
        hB = fw.dram([D, S], F32, "hB")
        self.hbufs = {}
        for nm, ap in (("x", self.xT), ("A", hA), ("B", hB), ("out", self.outT)):
            self.hbufs[nm] = (ap, [Buf(None, f"h{nm}{t}") for t in range(NTI)])
        self.tv = fw.dram([16, TV_TOT], F32, "tv")
        self.tvbuf = Buf(None, "tv")
        if any(l in (1, 2) for l in self.layers):
            self.build_tv()
        cur = "x"
        nl = len(self.layers)
        for idx, i in enumerate(self.layers):
            dst = "out" if idx == nl - 1 else ("A" if cur != "A" else "B")
            kind = i % 3
            j = i // 3
            if kind == 0:
                self.layer_rglru(i, j, cur, dst)
            elif kind == 1:
                self.layer_dil(i, cur, dst)
            else:
                self.layer_nsa(i, cur, dst)
            cur = dst

    def build_tv(self):
        fw = self.fw
        with ExitStack() as sub:
            fw.push(sub)
            rb = fw.sbuf([33, 16], F32, "rb")
            fw.op("sp", lambda e: e.dma_start(out=rb[:, :], in_=self.relb), w=[rb], dsem="rb")
            nchunk = (TV_TOT + 511) // 512
            ohs = Rot([fw.sbuf([33, 512], F32, f"ohs{i}") for i in range(2)])
            tvs = Rot([fw.sbuf([16, 512], F32, f"tvs{i}") for i in range(2)])
            for c in range(nchunk):
                c0 = c * 512
                n = min(512, TV_TOT - c0)
                o, oi = ohs.next()
                fw.op("sp", lambda e, o=o, c0=c0, n=n: e.dma_start(out=o[:, :n], in_=self.oh[:, c0:c0 + n]), w=[o], dsem=f"ohs{oi}")
                ps, _ = self.psr.next()
                fw.op("pe", lambda e, o=o, ps=ps, n=n: e.matmul(ps[:16, :n], lhsT=rb[:, :], rhs=o[:, :n], start=True, stop=True), r=[rb, o], w=[ps])
                t, ti = tvs.next()
                fw.op("act", lambda e, t=t, ps=ps, n=n: e.activation(out=t[:, :n], in_=ps[:16, :n], func=AF.Copy), r=[ps], w=[t])
                fw.op("sp", lambda e, t=t, c0=c0, n=n: e.dma_start(out=self.tv[:, c0:c0 + n], in_=t[:, :n]), r=[t], w=[self.tvbuf], dsem=f"tvs{ti}")
            fw.pop()

    def load_toeplitz(self, Wt, kind, h, base_delta, width, Hs, nm, rot):
        fw = self.fw
        o, X = TV_OFF[kind]
        assert base_delta + 384 >= 0 and base_delta + 384 + 127 + width - 1 < X, (kind, base_delta, width)
        start = h * TV_TOT + o + base_delta + 511 - 127
        src = bass.AP(self.tv.tensor, start, [[1, 128], [1, width]])
        fw.op("sp", lambda e: e.dma_start(out=Hs[:, :width], in_=src), w=[Hs], dsem=nm)
        for c0 in range(0, width, 512):
            n = min(512, width - c0)
            ps, _ = rot.next()
            fw.op("pe", lambda e, ps=ps, c0=c0, n=n: e.matmul(ps[:, :n], lhsT=self.JREV[:, :], rhs=Hs[:, c0:c0 + n], start=True, stop=True), r=[self.JREV, Hs], w=[ps])
            fw.op("act", lambda e, ps=ps, c0=c0, n=n: e.activation(out=Wt[:, c0:c0 + n], in_=ps[:, :n], func=AF.Copy), r=[ps], w=[Wt])

    def load_w(self, W, r0, kc, c0, ncols):
        fw = self.fw
        key = (W.tensor.name, int(W.offset), r0, kc, c0, ncols)
        if key in self.pref:
            return self.pref.pop(key)
        wt, wi = self.wt.next()
        if key in self.wcache:
            scr, sb = self.wcache[key]
            fw.op(self.wq, lambda e: e.dma_start(out=wt[:, :kc, :ncols], in_=scr), r=[sb], w=[wt], dsem=f"wt{wi}_{self.wq}")
        else:
            src = W[r0:r0 + kc * 128, c0:c0 + ncols].rearrange("(k p) n -> p k n", p=128)
            fw.op("pool", lambda e: e.dma_start(out=wt[:, :kc, :ncols], in_=src), w=[wt], dsem=f"wt{wi}_pool")
            self.nwc += 1
            t = self.nc.dram_tensor(f"wc{self.nwc}", [128, kc, ncols], BF16, kind="Internal")
            scr = t.ap()
            sb = Buf(None, f"wc{self.nwc}")
            fw.op("sp", lambda e: e.dma_start(out=scr, in_=wt[:, :kc, :ncols]), r=[wt], w=[sb], dsem=f"wts{wi}")
            self.wcache[key] = (scr, sb)
        return wt

    def prefetch_w(self, W, r0, kc, c0, ncols):
        key = (W.tensor.name, int(W.offset), r0, kc, c0, ncols)
        assert key not in self.pref
        wt = self.load_w(W, r0, kc, c0, ncols)
        self.pref[key] = wt

    def prenorm_load(self, src, t, B32, nm="B32"):
        fw = self.fw
        ap, bufs = self.hbufs[src]
        fw.op("sp", lambda e: e.dma_start(out=B32[:, :, :], in_=ap[:, t * NT:(t + 1) * NT].rearrange("(k p) n -> p k n", p=128)),
              r=[bufs[t]], w=[B32], dsem=nm)

    def prenorm(self, i, src, t, B32, U16, tmp, load=True, nm="B32"):
        fw = self.fw
        if load:
            self.prenorm_load(src, t, B32, nm)
        self.rstd_of(B32, tmp)
        rstd = tmp["rstd"]
        for k in range(16):
            g = self.vcol(f"pre{i}", k)
            fw.op("dve", lambda e, k=k, g=g: e.scalar_tensor_tensor(out=U16[:, k, :], in0=B32[:, k, :], scalar=g, in1=rstd[:, :], op0=ALU.mult, op1=ALU.mult),
                  r=[B32, rstd, self.vecs], w=[U16])

    def rstd_of(self, X32, tmp, squares_done=False):
        fw = self.fw
        psS = self.psS
        if not squares_done:
            for k in range(16):
                sq, _ = tmp["sq"].next()
                fw.op("act", lambda e, k=k, sq=sq: e.activation(out=sq[:, :], in_=X32[:, k, :], func=AF.Square), r=[X32], w=[sq])
                fw.op("pe", lambda e, k=k, sq=sq: e.matmul(psS[:, :], lhsT=self.ones_bf[:, :], rhs=sq[:, :], start=(k == 0), stop=(k == 15)),
                      r=[self.ones_bf, sq], w=[psS])
        rstd = tmp["rstd"]
        fw.op("act", lambda e: e.activation(out=rstd[:, :], in_=psS[:, :], func=AF.Sqrt, scale=1.0 / D, bias=self.cst[:, 0:1]), r=[psS, self.cst], w=[rstd])
        fw.op("dve", lambda e: e.reciprocal(out=rstd[:, :], in_=rstd[:, :]), r=[rstd], w=[rstd])

    def linear_fm(self, W, kc, c0, ncols, rhs_buf, evac):
        fw = self.fw
        mi = 0
        for g0 in range(0, ncols, 512):
            gn = min(512, ncols - g0)
            wt = self.load_w(W, 0, kc, c0 + g0, gn)
            for m0 in range(0, gn, 128):
                mw = min(128, gn - m0)
                ps, _ = self.psr.next()
                for k in range(kc):
                    fw.op("pe", lambda e, k=k, m0=m0, mw=mw, ps=ps, wt=wt: e.matmul(ps[:mw, :], lhsT=wt[:, k, m0:m0 + mw], rhs=rhs_buf[:, k, :], start=(k == 0), stop=(k == kc - 1)),
                          r=[wt, rhs_buf], w=[ps])
                evac(mi, mw, ps)
                mi += 1

    def linear_tm(self, W, kc, c0, ncols, lhs_buf, evac):
        fw = self.fw
        for g0 in range(0, ncols, 512):
            gn = min(512, ncols - g0)
            wt = self.load_w(W, 0, kc, c0 + g0, gn)
            for tb in range(NT // 128):
                ps, _ = self.psr.next()
                for k in range(kc):
                    fw.op("pe", lambda e, k=k, tb=tb, ps=ps, wt=wt, gn=gn: e.matmul(ps[:, :gn], lhsT=lhs_buf[:, k, tb * 128:(tb + 1) * 128], rhs=wt[:, k, :gn], start=(k == 0), stop=(k == kc - 1)),
                          r=[wt, lhs_buf], w=[ps])
                evac(tb, g0, gn, ps)

    def post_phase(self, i, w_out, kcy, src, dst, t, Y16, A32, B32, U16, tmp, mid_hook=None):
        fw = self.fw
        psS = self.psS

        pend = []

        def evac_out(mi, mw, ps):
            fw.op("act", lambda e: e.activation(out=A32[:, mi, :], in_=ps[:, :], func=AF.Copy), r=[ps], w=[A32])
            sq, _ = tmp["sq"].next()
            fw.op("dve", lambda e: e.tensor_tensor(out=sq[:, :], in0=ps[:, :], in1=A32[:, mi, :], op=ALU.mult), r=[ps, A32], w=[sq])

            def ones_mm():
                fw.op("pe", lambda e: e.matmul(psS[:, :], lhsT=self.ones_bf[:, :], rhs=sq[:, :], start=(mi == 0), stop=(mi == 15)), r=[self.ones_bf, sq], w=[psS])

            if pend:
                pend.pop()()
            pend.append(ones_mm)

        P16 = tmp["P16"]
        fw.op("pool", lambda e: e.dma_start(out=P16[:, :, :], in_=self.pT[i, :, t * NT:(t + 1) * NT].rearrange("(k p) n -> p k n", p=128)), w=[P16], dsem="P16")
        WP = tmp["WP"]
        fw.op("pool", lambda e: e.dma_start(out=WP[:, :, :], in_=self.ple_w_proj[i].rearrange("(k p) n -> p k n", p=128)), w=[WP], dsem="WP")
        self.linear_fm(w_out, kcy, 0, D, Y16, evac_out)
        while pend:
            pend.pop()()
        self.rstd_of(A32, tmp, squares_done=True)
        rstd = tmp["rstd"]
        ap, bufs = self.hbufs[src]
        fw.op("sp", lambda e: e.dma_start(out=B32[:, :, :], in_=ap[:, t * NT:(t + 1) * NT].rearrange("(k p) n -> p k n", p=128)),
              r=[bufs[t]], w=[B32], dsem="B32")
        for k in range(16):
            g = self.vcol(f"post{i}", k)
            fw.op("dve", lambda e, k=k, g=g: e.scalar_tensor_tensor(out=A32[:, k, :], in0=A32[:, k, :], scalar=g, in1=rstd[:, :], op0=ALU.mult, op1=ALU.mult),
                  r=[A32, rstd, self.vecs], w=[A32])
            fw.op("dve" if k % 3 else "pool", lambda e, k=k: e.tensor_tensor(out=B32[:, k, :], in0=B32[:, k, :], in1=A32[:, k, :], op=ALU.add), r=[A32, B32], w=[B32])
            fw.op("act", lambda e, k=k: e.activation(out=U16[:, k, :], in_=B32[:, k, :], func=AF.Copy), r=[B32], w=[U16])
        if mid_hook is not None:
            mid_hook()

        def evac_gate(mi, mw, ps):
            sg, _ = tmp["sg"].next()
            fw.op("act", lambda e: e.activation(out=sg[:, :], in_=ps[:, :], func=AF.Sigmoid), r=[ps], w=[sg])
            ps2 = self.psX
            for kk in range(2):
                fw.op("pe", lambda e, kk=kk: e.matmul(ps2[:, :], lhsT=WP[:, kk, mi * 128:(mi + 1) * 128], rhs=P16[:, kk, :], start=(kk == 0), stop=(kk == 1)), r=[WP, P16], w=[ps2])
            fw.op("dve", lambda e: e.tensor_tensor(out=sg[:, :], in0=sg[:, :], in1=ps2[:, :], op=ALU.mult), r=[sg, ps2], w=[sg])
            fw.op("pool", lambda e: e.tensor_tensor(out=B32[:, mi, :], in0=B32[:, mi, :], in1=sg[:, :], op=ALU.add), r=[B32, sg], w=[B32])

        self.linear_fm(self.ple_w_gate[i], 16, 0, D, U16, evac_gate)
        apd, bufd = self.hbufs[dst]
        fw.op("sp", lambda e: e.dma_start(out=apd[:, t * NT:(t + 1) * NT].rearrange("(k p) n -> p k n", p=128), in_=B32[:, :, :]),
              r=[B32], w=[bufd[t]], dsem="B32")

    def common_tiles(self):
        fw = self.fw
        tmp = {}
        tmp["sq"] = Rot([fw.sbuf([128, 512], BF16, f"sq{i}") for i in range(2)])
        tmp["rstd"] = fw.sbuf([128, 512], F32, "rstd")
        tmp["sg"] = Rot([fw.sbuf([128, 512], F32, f"sg{i}") for i in range(2)])
        tmp["P16"] = fw.sbuf([128, 2, 512], BF16, "P16")
        tmp["WP"] = fw.sbuf([128, 2, D], BF16, "WP")
        return tmp

    def layer_rglru(self, i, j, src, dst):
        fw = self.fw
        self.wq = "sp"
        with ExitStack() as sub:
            fw.push(sub)
            tmp = self.common_tiles()
            XB = fw.sbuf([128, 16, 3 + 512], F32, "XB")
            A32 = View(XB, XB.t[:, :, 3:515])
            B32 = fw.sbuf([128, 16, 512], F32, "B32")
            U16 = fw.sbuf([128, 16, 512], BF16, "U16")
            G16 = fw.sbuf([128, 16, 512], BF16, "G16")
            Y16 = G16
            WR = fw.sbuf([128, 8, 2, 256], BF16, "WR")
            WI = fw.sbuf([128, 8, 2, 256], BF16, "WI")
            c8 = fw.sbuf([128, 32], F32, "c8")
            carry = fw.sbuf([128, 16], F32, "carry")
            XC = Rot([fw.sbuf([128, 2, 512], F32, f"XC{q}") for q in range(2)])
            XCB = Rot([fw.sbuf([128, 2, 512], BF16, f"XCB{q}") for q in range(2)])
            small = {nm: Rot([fw.sbuf([128, 512], F32, f"{nm}{q}") for q in range(2 if nm == "HH" else 4)]) for nm in ("R", "I", "Ss", "HH")}
            fw.op("pool", lambda e: e.dma_start(out=WR[:, :, :, :], in_=self.a_w_r[j].rearrange("n (k p) m -> p n k m", p=128)), w=[WR], dsem="WR")
            fw.op("pool", lambda e: e.dma_start(out=WI[:, :, :, :], in_=self.a_w_i[j].rearrange("n (k p) m -> p n k m", p=128)), w=[WI], dsem="WI")
            lam = self.vcol(f"lam{j}", 0, 16)
            fw.op("act", lambda e: e.activation(out=c8[:, 0:16], in_=lam, func=AF.Exp, scale=-1.0), r=[self.vecs], w=[c8])
            fw.op("act", lambda e: e.activation(out=c8[:, 0:16], in_=c8[:, 0:16], func=AF.Ln, bias=self.cst[:, 1:2]), r=[c8, self.cst], w=[c8])
            fw.op("dve", lambda e: e.tensor_scalar(out=c8[:, 0:16], in0=c8[:, 0:16], scalar1=-4.0, scalar2=None, op0=ALU.mult), r=[c8], w=[c8])
            hb = fw.sbuf([128, 32], F32, "hb")
            fw.op("dve", lambda e: e.tensor_scalar(out=hb[:, 0:16], in0=self.vcol(f"br{j}", 0, 16), scalar1=0.5, scalar2=None, op0=ALU.mult), r=[self.vecs], w=[hb])
            fw.op("dve", lambda e: e.tensor_scalar(out=hb[:, 16:32], in0=self.vcol(f"bi{j}", 0, 16), scalar1=0.5, scalar2=None, op0=ALU.mult), r=[self.vecs], w=[hb])
            fw.op("pool", lambda e: e.memset(carry[:, :], 0.0), w=[carry])
            fw.op("pool", lambda e: e.memset(XB[:, :, 0:3], 0.0), w=[XB])
            W = self.a_w_in[j]
            NTL_ = min(NTI, DBG_TILES)
            self.prenorm_load(src, 0, A32, "XBh")
            for t in range(NTL_):
                self.prenorm(i, src, t, A32, U16, tmp, load=False)

                def evac_x(mi, mw, ps):
                    fw.op("act", lambda e: e.activation(out=XB[:, mi, 3:], in_=ps[:, :], func=AF.Copy), r=[ps], w=[XB])

                def evac_g(mi, mw, ps):
                    fw.op("act", lambda e: e.activation(out=G16[:, mi, :], in_=ps[:, :], func=AF.Silu), r=[ps], w=[G16])

                self.linear_fm(W, 16, 0, D, U16, evac_x)
                pending = []
                for gg in range(4):
                    self.linear_fm(W, 16, D + gg * 512, 512, U16, lambda mi, mw, ps, gg=gg: evac_g(4 * gg + mi, mw, ps))
                    if gg < 3:
                        self.prefetch_w(W, 0, 16, D + (gg + 1) * 512, 512)
                    else:
                        self.prefetch_w(self.a_w_out[j], 0, 16, 0, 512)
                    for n in (2 * gg, 2 * gg + 1):
                        xc, _ = XC.next()
                        xcb, _ = XCB.next()
                        for mm in range(2):
                            ch = 2 * n + mm
                            fw.op("dve", lambda e, ch=ch, mm=mm, xc=xc: e.tensor_scalar(out=xc[:, mm, :], in0=XB[:, ch, 3:515], scalar1=self.vcol(f"cw{j}_3", ch), scalar2=self.vcol(f"cb{j}", ch), op0=ALU.mult, op1=ALU.add),
                                  r=[XB, self.vecs], w=[xc])
                            for tap in range(3):
                                fw.op("dve", lambda e, ch=ch, mm=mm, xc=xc, tap=tap: e.scalar_tensor_tensor(out=xc[:, mm, :], in0=XB[:, ch, tap:tap + 512], scalar=self.vcol(f"cw{j}_{tap}", ch), in1=xc[:, mm, :], op0=ALU.mult, op1=ALU.add),
                                      r=[XB, self.vecs, xc], w=[xc])
                        fw.op("pool", lambda e, xc=xc, xcb=xcb: e.tensor_copy(out=xcb[:, :, :], in_=xc[:, :, :]), r=[xc], w=[xcb])
                        chunks = []
                        for mm in range(2):
                            ch = 2 * n + mm
                            psr_, _ = self.psr.next()
                            psi_, _ = self.psr.next()
                            for kk in range(2):
                                fw.op("pe", lambda e, kk=kk, mm=mm, n=n, xcb=xcb, p=psr_: e.matmul(p[:, :], lhsT=WR[:, n, kk, mm * 128:(mm + 1) * 128], rhs=xcb[:, kk, :], start=(kk == 0), stop=(kk == 1)), r=[WR, xcb], w=[psr_])
                            for kk in range(2):
                                fw.op("pe", lambda e, kk=kk, mm=mm, n=n, xcb=xcb, p=psi_: e.matmul(p[:, :], lhsT=WI[:, n, kk, mm * 128:(mm + 1) * 128], rhs=xcb[:, kk, :], start=(kk == 0), stop=(kk == 1)), r=[WI, xcb], w=[psi_])
                            cR = small["R"].next()[0]
                            cI = small["I"].next()[0]
                            chunks.append(dict(ch=ch, mm=mm, psr=psr_, psi=psi_, R=cR, I=cI, Aa=cR, Ss=small["Ss"].next()[0], BT=cI, HH=small["HH"].next()[0]))
                        for c in chunks:
                            fw.op("act", lambda e, c=c: e.activation(out=c["R"][:, :], in_=c["psr"][:, :], func=AF.Tanh, scale=0.5, bias=hb[:, c["ch"]:c["ch"] + 1]), r=[c["psr"], hb], w=[c["R"]])
                            fw.op("act", lambda e, c=c: e.activation(out=c["I"][:, :], in_=c["psi"][:, :], func=AF.Tanh, scale=0.5, bias=hb[:, 16 + c["ch"]:17 + c["ch"]]), r=[c["psi"], hb], w=[c["I"]])
                        for c in chunks:
                            fw.op("act", lambda e, c=c: e.activation(out=c["Aa"][:, :], in_=c["R"][:, :], func=AF.Exp, scale=c8[:, c["ch"]:c["ch"] + 1], bias=c8[:, c["ch"]:c["ch"] + 1]), r=[c["R"], c8], w=[c["Aa"]])
                        for c in chunks:
                            fw.op("dve", lambda e, c=c: e.tensor_tensor(out=c["Ss"][:, :], in0=c["Aa"][:, :], in1=c["Aa"][:, :], op=ALU.mult), r=[c["Aa"]], w=[c["Ss"]])
                            fw.op("dve", lambda e, c=c, xc=xc: e.scalar_tensor_tensor(out=c["BT"][:, :], in0=c["I"][:, :], scalar=1.0, in1=xc[:, c["mm"], :], op0=ALU.add, op1=ALU.mult), r=[c["I"], xc], w=[c["BT"]])

                        def stage_b(chunks=chunks):
                            for c in chunks:
                                fw.op("act", lambda e, c=c: e.activation(out=c["Ss"][:, :], in_=c["Ss"][:, :], func=AF.Sqrt, scale=-0.25, bias=self.cst[:, 2:3]), r=[c["Ss"], self.cst], w=[c["Ss"]])
                            for c in chunks:
                                fw.op("pool", lambda e, c=c: e.tensor_tensor(out=c["BT"][:, :], in0=c["BT"][:, :], in1=c["Ss"][:, :], op=ALU.mult), r=[c["Ss"], c["BT"]], w=[c["BT"]])
                                fw.op("dve", lambda e, c=c: e.tensor_tensor_scan(out=c["HH"][:, :], data0=c["Aa"][:, :], data1=c["BT"][:, :], initial=carry[:, c["ch"]:c["ch"] + 1], op0=ALU.mult, op1=ALU.add), r=[c["Aa"], c["BT"], carry], w=[c["HH"]])
                                fw.op("dve", lambda e, c=c: e.tensor_copy(out=carry[:, c["ch"]:c["ch"] + 1], in_=c["HH"][:, 511:512]), r=[c["HH"]], w=[carry])
                                fw.op("pool", lambda e, c=c: e.tensor_tensor(out=Y16[:, c["ch"], :], in0=c["HH"][:, :], in1=G16[:, c["ch"], :], op=ALU.mult), r=[c["HH"], G16], w=[Y16])

                        if pending:
                            pending.pop()()
                        pending.append(stage_b)
                while pending:
                    pending.pop()()
                fw.op("dve", lambda e: e.tensor_copy(out=XB[:, :, 0:3], in_=XB[:, :, 512:515]), r=[XB], w=[XB])
                self.post_phase(i, self.a_w_out[j], 16, src, dst, t, Y16, A32, B32, U16, tmp,
                                mid_hook=(lambda t=t: self.prenorm_load(src, t + 1, A32, "XBh")) if t + 1 < NTL_ else None)
            fw.pop()
        self.wq = "pool"

    def inproj_phase(self, i, src, W, segs):
        fw = self.fw
        with ExitStack() as sub:
            fw.push(sub)
            tmp = self.common_tiles()
            B32s = [fw.sbuf([128, 16, 512], F32, f"B32{q}") for q in range(2)]
            U16 = fw.sbuf([128, 16, 512], BF16, "U16")
            stage = Rot([fw.sbuf([128, 512], BF16, f"stg{q}") for q in range(3)])
            stage32 = Rot([fw.sbuf([128, 512], F32, f"stgf{q}") for q in range(2)])
            flip = [0]
            NTL_ = min(NTI, DBG_TILES)
            self.prenorm_load(src, 0, B32s[0], "B32i0")
            for t in range(NTL_):
                if t + 1 < NTL_:
                    self.prenorm_load(src, t + 1, B32s[(t + 1) % 2], f"B32i{(t + 1) % 2}")
                self.prenorm(i, src, t, B32s[t % 2], U16, tmp, load=False)
                for mode, c0, ncols, func, dap, d0 in segs:
                    if mode == "fm":
                        def ev(mi, mw, ps, func=func, dap=dap, d0=d0, t=t):
                            if dap.dtype == F32:
                                st, si = stage32.next()
                                nm = f"stgf{si}"
                            else:
                                st, si = stage.next()
                                nm = f"stg{si}"
                            flip[0] ^= 1
                            if func == AF.Copy and flip[0]:
                                fw.op("dve", lambda e: e.tensor_copy(out=st[:mw, :], in_=ps[:mw, :]), r=[ps], w=[st])
                            else:
                                fw.op("act", lambda e: e.activation(out=st[:mw, :], in_=ps[:mw, :], func=func), r=[ps], w=[st])
                            fw.op("sp", lambda e: e.dma_start(out=dap[d0 + mi * 128:d0 + mi * 128 + mw, t * NT:(t + 1) * NT], in_=st[:mw, :]), r=[st], dsem=nm)
                        self.linear_fm(W, 16, c0, ncols, U16, ev)
                    else:
                        def ev(tb, g0, gn, ps, dap=dap, d0=d0, t=t):
                            st, si = stage.next()
                            flip[0] ^= 1
                            if flip[0]:
                                fw.op("dve", lambda e: e.tensor_copy(out=st[:, :gn], in_=ps[:, :gn]), r=[ps], w=[st])
                            else:
                                fw.op("act", lambda e: e.activation(out=st[:, :gn], in_=ps[:, :gn], func=AF.Copy), r=[ps], w=[st])
                            fw.op("sp", lambda e: e.dma_start(out=dap[t * NT + tb * 128:t * NT + (tb + 1) * 128, d0 + g0:d0 + g0 + gn], in_=st[:, :gn]), r=[st], dsem=f"stg{si}")
                        self.linear_tm(W, 16, c0, ncols, U16, ev)
            fw.pop()

    def post_all(self, i, w_out, kcy, yT, src, dst):
        fw = self.fw
        with ExitStack() as sub:
            fw.push(sub)
            tmp = self.common_tiles()
            A32 = fw.sbuf([128, 16, 512], F32, "A32")
            B32 = fw.sbuf([128, 16, 512], F32, "B32")
            U16 = fw.sbuf([128, 16, 512], BF16, "U16")
            Y16s = [fw.sbuf([128, kcy, 512], BF16, f"Y16{q}") for q in range(2)]
            NTL_ = min(NTI, DBG_TILES)

            def ld(t):
                Y16 = Y16s[t % 2]
                fw.op("sp", lambda e: e.dma_start(out=Y16[:, :, :], in_=yT[:, t * NT:(t + 1) * NT].rearrange("(k p) n -> p k n", p=128)), w=[Y16], dsem=f"Y16{t % 2}")

            ld(0)
            for t in range(NTL_):
                if t + 1 < NTL_:
                    ld(t + 1)
                self.post_phase(i, w_out, kcy, src, dst, t, Y16s[t % 2], A32, B32, U16, tmp)
            fw.pop()

    def layer_dil(self, i, src, dst):
        fw = self.fw
        W = self.b_w_in[0]
        qkT = fw.dram([6144, S], BF16, "qkT")
        Vd = fw.dram([S, 3072], BF16, "Vd")
        sgT = fw.dram([1024, S], BF16, "sgTd")
        yT = fw.dram([1024, S], BF16, "yTd")
        segs = []
        for g in range(3):
            segs.append(("fm", g * 3072, 1024, AF.Copy, qkT, g * 2048))
            segs.append(("fm", g * 3072 + 1024, 1024, AF.Copy, qkT, g * 2048 + 1024))
            segs.append(("tm", g * 3072 + 2048, 1024, None, Vd, g * 1024))
        segs.append(("fm", 9216, 1024, AF.Silu, sgT, 0))
        self.inproj_phase(i, src, W, segs)
        with ExitStack() as sub:
            fw.push(sub)
            QT2 = [fw.sbuf([64, S], BF16, f"QT{q}") for q in range(2)]
            KT2 = [fw.sbuf([64, S], BF16, f"KT{q}") for q in range(2)]
            VT2 = [fw.sbuf([128, 32, 64], BF16, f"VT{q}") for q in range(2)]
            SG = fw.sbuf([64, S], BF16, "SG")
            YO = fw.sbuf([64, S], BF16, "YO")
            UA = fw.sbuf([64, S], F32, "UA")
            DA = fw.sbuf([64, S], F32, "DA")
            BTz2 = [fw.sbuf([128, 1024], F32, f"BTz{g}") for g in range(2)]
            HS = fw.sbuf([128, 1024], F32, "HSd")
            TMP = Rot([fw.sbuf([128, 512], F32, f"TMP{q}") for q in range(2)])
            PT = Rot([fw.sbuf([128, 512], BF16, f"PT{q}") for q in range(6)])
            rotS = Rot(self.ps[0:4])
            rotU = Rot(self.ps[4:6])
            rotD = Rot(self.ps[6:8])
            nheads = DBG_HEADS
            LA = 3
            dils = (1, 4, 16)

            def load_g(h, g, slot):
                d = dils[g]
                L = S // d
                nblk = L // 128
                self.load_toeplitz(BTz2[slot], f"dil{g}", h, -384, 1024, HS, "HSd", rotS)
                fw.op("sp", lambda e: e.dma_start(out=QT2[slot][:, :], in_=qkT[g * 2048 + h * 64:g * 2048 + (h + 1) * 64, :]), w=[QT2[slot]], dsem=f"QT{slot}")
                fw.op("sp", lambda e: e.dma_start(out=KT2[slot][:, :], in_=qkT[g * 2048 + 1024 + h * 64:g * 2048 + 1024 + (h + 1) * 64, :]), w=[KT2[slot]], dsem=f"KT{slot}")
                vsrc = Vd[:, g * 1024 + h * 64:g * 1024 + (h + 1) * 64].rearrange("(jb p dd) c -> dd p jb c", p=128, dd=d)
                for r in range(d):
                    fw.op("sp", lambda e, r=r: e.dma_start(out=VT2[slot][:, r * nblk:(r + 1) * nblk, :], in_=vsrc[r]), w=[VT2[slot]], dsem=f"VT{slot}")

            hg = [(h, g) for h in range(nheads) for g in range(3)]
            load_g(0, 0, 0)
            for hgi, (h, g) in enumerate(hg):
                slot = hgi % 2
                if g == 0:
                    fw.op("sp", lambda e, h=h: e.dma_start(out=SG[:, :], in_=sgT[h * 64:(h + 1) * 64, :]), w=[SG], dsem="SG")
                if hgi + 1 < len(hg):
                    load_g(hg[hgi + 1][0], hg[hgi + 1][1], 1 - slot)
                d = dils[g]
                L = S // d
                nblk = L // 128
                Nq = min(256, L)
                QT, KT, VT, BT = QT2[slot], KT2[slot], VT2[slot], BTz2[slot]
                items = []
                for r in range(d):
                    for q0 in range(0, L, Nq):
                        kbs = [kb for kb in range(q0 // 128 - 1, (q0 + Nq) // 128) if kb >= 0]
                        grp = {"qcols": slice(r + d * q0, r + d * (q0 + Nq - 1) + 1, d)}
                        for ix, kb in enumerate(kbs):
                            items.append((grp, r, q0, kb, ix == 0, ix == len(kbs) - 1))

                def s_stage(it, d=d, Nq=Nq, KT=KT, QT=QT, BT=BT):
                    grp, r, q0, kb, first, last = it
                    qcols = grp["qcols"]
                    psS, _ = rotS.next()
                    kcols = slice(r + d * kb * 128, r + d * (kb * 128 + 127) + 1, d)
                    boff = q0 - kb * 128 + 384
                    tm, _ = TMP.next()
                    pt, _ = PT.next()
                    fw.op("pe", lambda e: e.matmul(psS[:, :Nq], lhsT=KT[:, kcols], rhs=QT[:, qcols], start=True, stop=True), r=[KT, QT], w=[psS])
                    fw.op("dve", lambda e: e.scalar_tensor_tensor(out=tm[:, :Nq], in0=psS[:, :Nq], scalar=0.125, in1=BT[:, boff:boff + Nq], op0=ALU.mult, op1=ALU.add), r=[psS, BT], w=[tm])
                    fw.op("act", lambda e: e.activation(out=pt[:, :Nq], in_=tm[:, :Nq], func=AF.Exp), r=[tm], w=[pt])
                    return pt

                def pv_stage(it, pt, Nq=Nq, VT=VT, g=g, nblk=nblk):
                    grp, r, q0, kb, first, last = it
                    qcols = grp["qcols"]
                    if first:
                        grp["psU"], _ = rotU.next()
                        grp["psD"], _ = rotD.next()
                    psU, psD = grp["psU"], grp["psD"]
                    blk = r * nblk + kb
                    fw.op("pe", lambda e: e.matmul(psU[:64, :Nq], lhsT=VT[:, blk, :], rhs=pt[:, :Nq], start=first, stop=last), r=[VT, pt], w=[psU])
                    fw.op("pe", lambda e: e.matmul(psD[:64, :Nq], lhsT=self.ones_bf[:, :64], rhs=pt[:, :Nq], start=first, stop=last), r=[self.ones_bf, pt], w=[psD])
                    if last:
                        if g == 0:
                            fw.op("act", lambda e: e.activation(out=UA[:, qcols], in_=psU[:64, :Nq], func=AF.Copy), r=[psU], w=[UA])
                            fw.op("dve", lambda e: e.tensor_copy(out=DA[:, qcols], in_=psD[:64, :Nq]), r=[psD], w=[DA])
                        else:
                            fw.op("dve", lambda e: e.tensor_tensor(out=UA[:, qcols], in0=UA[:, qcols], in1=psU[:64, :Nq], op=ALU.add), r=[psU, UA], w=[UA])
                            fw.op("dve", lambda e: e.tensor_tensor(out=DA[:, qcols], in0=DA[:, qcols], in1=psD[:64, :Nq], op=ALU.add), r=[psD, DA], w=[DA])

                pts = {}
                n = len(items)
                for ii in range(n + LA):
                    if ii < n:
                        pts[ii] = s_stage(items[ii])
                    if ii >= LA:
                        pv_stage(items[ii - LA], pts.pop(ii - LA))
                if g < 2:
                    continue
                fw.op("dve", lambda e: e.reciprocal(out=DA[:, :], in_=DA[:, :]), r=[DA], w=[DA])
                fw.op("pool", lambda e: e.tensor_tensor(out=UA[:, :], in0=UA[:, :], in1=DA[:, :], op=ALU.mult), r=[UA, DA], w=[UA])
                fw.op("pool", lambda e: e.tensor_tensor(out=YO[:, :], in0=UA[:, :], in1=SG[:, :], op=ALU.mult), r=[UA, SG], w=[YO])
                fw.op("sp", lambda e, h=h: e.dma_start(out=yT[h * 64:(h + 1) * 64, :], in_=YO[:, :]), r=[YO], dsem="YO")
            fw.pop()
        self.post_all(i, self.b_w_out[0], 8, yT, src, dst)

    def layer_nsa(self, i, src, dst):
        fw = self.fw
        W = self.c_w_in[0]
        SC = 128.0 ** -0.5
        BIGS = 30000.0 / SC
        qT = fw.dram([2048, S], BF16, "qTn")
        kvcT = fw.dram([1024, S], BF16, "kvcT")
        ksT = fw.dram([512, S], BF16, "ksT")
        kwT = fw.dram([512, S], BF16, "kwT")
        vsD = fw.dram([S, 512], BF16, "vsD")
        vwD = fw.dram([S, 512], BF16, "vwD")
        bgT = fw.dram([48, S], F32, "bgT")
        sgT = fw.dram([2048, S], BF16, "sgTn")
        yT = fw.dram([2048, S], BF16, "yTn")
        ocT = fw.dram([2048, S], F32, "ocT")
        negT = fw.dram([4, 64, S], BF16, "negT")
        segs = [("fm", 0, 2048, AF.Copy, qT, 0), ("fm", 2048, 1024, AF.Copy, kvcT, 0), ("fm", 3072, 512, AF.Copy, ksT, 0),
                ("tm", 3584, 512, None, vsD, 0), ("fm", 4096, 512, AF.Copy, kwT, 0), ("tm", 4608, 512, None, vwD, 0),
                ("fm", 5120, 48, AF.Sigmoid, bgT, 0), ("fm", 5168, 2048, AF.Silu, sgT, 0)]
        self.inproj_phase(i, src, W, segs)
        NTL = min(NTI, DBG_TILES)
        with ExitStack() as outer:
            fw.push(outer)
            KCMP = fw.sbuf([128, 4, 256], BF16, "KCMP")
            VCMP = fw.sbuf([128, 4, 2, 128], BF16, "VCMP")
            with ExitStack() as sub:
                fw.push(sub)
                KC = fw.sbuf([128, 4, S], BF16, "KC")
                W1 = fw.sbuf([128, 32, 512], BF16, "W1")
                W2 = fw.sbuf([128, 4, 128], BF16, "W2")
                KCPA = fw.sbuf([128, 32, 1020], BF16, "KCPA")
                HG = fw.sbuf([128, 4, 1020], BF16, "HG")
                tA = fw.sbuf([128, 1020], F32, "tA")
                tB = fw.sbuf([128, 1020], F32, "tB")
                for which in range(2):
                    w1 = (self.c_w1_k, self.c_w1_v)[which][0]
                    w2 = (self.c_w2_k, self.c_w2_v)[which][0]
                    posn = ("posk", "posv")[which]
                    fw.op("sp", lambda e, which=which: e.dma_start(out=KC[:, :, :], in_=kvcT[which * 512:(which + 1) * 512, :].rearrange("(g p) n -> p g n", p=128)), w=[KC], dsem="KC")
                    fw.op("pool", lambda e, w1=w1: e.dma_start(out=W1[:, :, :], in_=w1.rearrange("(l p) n -> p l n", p=128)), w=[W1], dsem="W1")
                    fw.op("pool", lambda e, w2=w2: e.dma_start(out=W2[:, :, :], in_=w2.rearrange("(m p) n -> p m n", p=128)), w=[W2], dsem="W2")
                    for l in range(32):
                        fw.op("dve", lambda e, l=l, posn=posn: e.tensor_scalar(out=KCPA.t[:, l, :].rearrange("p (g c) -> p g c", g=4), in0=KC[:, :, l:l + 16 * 254 + 1:16], scalar1=self.vcol(posn, l), scalar2=None, op0=ALU.add),
                              r=[KC, self.vecs], w=[KCPA])
                    for m in range(4):
                        psA, psB = self.ps[0], self.ps[1]
                        for l in range(32):
                            fw.op("pe", lambda e, l=l, m=m: e.matmul(psA[:, :510], lhsT=W1[:, l, m * 128:(m + 1) * 128], rhs=KCPA[:, l, 0:510], start=(l == 0), stop=(l == 31)), r=[W1, KCPA], w=[psA])
                            fw.op("pe", lambda e, l=l, m=m: e.matmul(psB[:, :510], lhsT=W1[:, l, m * 128:(m + 1) * 128], rhs=KCPA[:, l, 510:1020], start=(l == 0), stop=(l == 31)), r=[W1, KCPA], w=[psB])
                        for half, ps in enumerate((psA, psB)):
                            hs = slice(half * 510, (half + 1) * 510)
                            fw.op("act", lambda e, ps=ps, hs=hs: e.activation(out=tA[:, hs], in_=ps[:, :510], func=AF.Square), r=[ps], w=[tA])
                            fw.op("dve", lambda e, hs=hs: e.tensor_scalar(out=tA[:, hs], in0=tA[:, hs], scalar1=0.044715, scalar2=1.0, op0=ALU.mult, op1=ALU.add), r=[tA], w=[tA])
                            fw.op("dve", lambda e, ps=ps, hs=hs: e.tensor_tensor(out=tA[:, hs], in0=tA[:, hs], in1=ps[:, :510], op=ALU.mult), r=[tA, ps], w=[tA])
                            fw.op("act", lambda e, hs=hs: e.activation(out=tB[:, hs], in_=tA[:, hs], func=AF.Sigmoid, scale=1.5957691216057308), r=[tA], w=[tB])
                            fw.op("dve", lambda e, ps=ps, hs=hs, m=m: e.tensor_tensor(out=HG[:, m, hs], in0=tB[:, hs], in1=ps[:, :510], op=ALU.mult), r=[tB, ps], w=[HG])
                    if which == 0:
                        for half in range(2):
                            ps = self.ps[2 + half]
                            for m in range(4):
                                fw.op("pe", lambda e, ps=ps, m=m, half=half: e.matmul(ps[:, :510], lhsT=W2[:, m, :], rhs=HG[:, m, half * 510:(half + 1) * 510], start=(m == 0), stop=(m == 3)), r=[W2, HG], w=[ps])
                            for gg in range(2):
                                fw.op("act", lambda e, ps=ps, half=half, gg=gg: e.activation(out=KCMP[:, 2 * half + gg, 0:255], in_=ps[:, gg * 255:(gg + 1) * 255], func=AF.Copy), r=[ps], w=[KCMP])
                    else:
                        for g in range(4):
                            for cb in range(2):
                                csz = 128 if cb == 0 else 127
                                ps, _ = self.psr.next()
                                for m in range(4):
                                    fw.op("pe", lambda e, ps=ps, m=m, g=g, cb=cb, csz=csz: e.matmul(ps[:csz, :128], lhsT=HG[:, m, g * 255 + cb * 128:g * 255 + cb * 128 + csz], rhs=W2[:, m, :], start=(m == 0), stop=(m == 3)), r=[W2, HG], w=[ps])
                                fw.op("act", lambda e, ps=ps, g=g, cb=cb, csz=csz: e.activation(out=VCMP[:csz, g, cb, :], in_=ps[:csz, :128], func=AF.Copy), r=[ps], w=[VCMP])
                fw.pop()
            with ExitStack() as sub:
                fw.push(sub)
                QG = fw.sbuf([128, 4, S], BF16, "QG")
                CM = fw.sbuf([128, 8, 2, 512], F32, "CM")
                COV = fw.sbuf([128, 2, 64], BF16, "COV")
                ADDT = fw.sbuf([128, 32, 64], F32, "ADDT")
                IDENT = fw.sbuf([128, 128], BF16, "IDENT")
                PN = fw.sbuf([128, 4, 2, 512], BF16, "PN")
                TMP = Rot([fw.sbuf([128, 512], F32, f"TMPc{q}") for q in range(2)])
                PT = Rot([fw.sbuf([128, 512], BF16, f"PTc{q}") for q in range(6)])
                RD = Rot([fw.sbuf([128, 512], F32, f"RDc{q}") for q in range(2)])
                GT = Rot([fw.sbuf([128, 512], F32, f"GTc{q}") for q in range(2)])
                OST = Rot([fw.sbuf([128, 512], F32, f"OST{q}") for q in range(2)])
                NEGS = Rot([fw.sbuf([64, 512], BF16, f"NEGS{q}") for q in range(2)])
                IMP = fw.sbuf([128, 64], F32, "IMP")
                WK = fw.sbuf([128, 64], F32, "WK")
                M8 = fw.sbuf([128, 16], F32, "M8")
                SEL = fw.sbuf([128, 64], F32, "SEL")
                NS = Rot([fw.sbuf([128, 64], BF16, f"NS{q}") for q in range(2)])
                fw.op("sp", lambda e: e.dma_start(out=CM[:, :, :, :], in_=self.cm), w=[CM], dsem="CM")
                fw.op("sp", lambda e: e.dma_start(out=ADDT[:, :, :], in_=self.addt), w=[ADDT], dsem="ADDT")
                fw.op("pool", lambda e: e.dma_start(out=COV[:, :, :], in_=self.cov), w=[COV], dsem="COV")
                fw.op("pool", lambda e: e.dma_start(out=IDENT[:, :], in_=self.ident), w=[IDENT], dsem="IDENT")
                rotS = Rot(self.ps[0:2])
                rotU = Rot(self.ps[2:4])
                rotD = Rot(self.ps[4:6])
                psI, psT = self.ps[6], self.ps[7]
                for g in range(4):
                    fw.op("sp", lambda e, g=g: e.dma_start(out=QG[:, :, :], in_=qT[g * 512:(g + 1) * 512, :].rearrange("(r p) n -> p r n", p=128)), w=[QG], dsem="QG")
                    for j in range(NTL):
                        tc = slice(j * NT, (j + 1) * NT)
                        cbs = [0] if j < 4 else [0, 1]
                        items = []
                        for r in range(4):
                            grp = {"r": r, "pts": []}
                            for ix, cb in enumerate(cbs):
                                items.append((grp, cb, ix == 0, ix == len(cbs) - 1))

                        def s_stage(it, g=g, j=j, tc=tc):
                            grp, cb, first, last = it
                            r = grp["r"]
                            csz = 128 if cb == 0 else 127
                            psS, _ = rotS.next()
                            tm, _ = TMP.next()
                            pt, _ = PT.next()
                            grp["pts"].append(pt)
                            fw.op("pe", lambda e: e.matmul(psS[:csz, :], lhsT=KCMP[:, g, cb * 128:cb * 128 + csz], rhs=QG[:, r, tc], start=True, stop=True), r=[KCMP, QG], w=[psS])
                            fw.op("dve", lambda e: e.scalar_tensor_tensor(out=tm[:csz, :], in0=psS[:csz, :], scalar=SC, in1=CM[:csz, j, cb, :], op0=ALU.mult, op1=ALU.add), r=[psS, CM], w=[tm])
                            fw.op("act", lambda e: e.activation(out=pt[:csz, :], in_=tm[:csz, :], func=AF.Exp), r=[tm], w=[pt])
                            return pt

                        def pv_stage(it, pt, g=g, j=j, tc=tc, cbs=cbs):
                            grp, cb, first, last = it
                            r = grp["r"]
                            h = 4 * g + r
                            csz = 128 if cb == 0 else 127
                            if first:
                                grp["psU"], _ = rotU.next()
                                grp["psD"], _ = rotD.next()
                            psU_, psD_ = grp["psU"], grp["psD"]
                            fw.op("pe", lambda e: e.matmul(psU_[:, :], lhsT=VCMP[:csz, g, cb, :], rhs=pt[:csz, :], start=first, stop=last), r=[VCMP, pt], w=[psU_])
                            fw.op("pe", lambda e: e.matmul(psD_[:, :], lhsT=self.ones_bf[:csz, :], rhs=pt[:csz, :], start=first, stop=last), r=[self.ones_bf, pt], w=[psD_])
                            if not last:
                                return
                            rd, _ = RD.next()
                            fw.op("dve", lambda e: e.tensor_scalar(out=rd[:, :], in0=psD_[:, :], scalar1=1e-30, scalar2=None, op0=ALU.max), r=[psD_], w=[rd])
                            fw.op("dve", lambda e: e.reciprocal(out=rd[:, :], in_=rd[:, :]), r=[rd], w=[rd])
                            for ix, cb2 in enumerate(cbs):
                                csz2 = 128 if cb2 == 0 else 127
                                p2 = grp["pts"][ix]
                                fw.op("pool", lambda e, p2=p2, cb2=cb2, csz2=csz2: e.tensor_tensor(out=PN[:csz2, r, cb2, :], in0=p2[:csz2, :], in1=rd[:csz2, :], op=ALU.mult), r=[p2, rd], w=[PN])
                            gt, gi = GT.next()
                            fw.op("sp", lambda e: e.dma_start(out=gt[:, :], in_=bass.AP(bgT.tensor, (h * 3 + 0) * S + j * NT, [[0, 128], [1, NT]])), w=[gt], dsem=f"GTc{gi}")
                            ost, oi = OST.next()
                            fw.op("pool", lambda e: e.tensor_tensor(out=gt[:, :], in0=gt[:, :], in1=rd[:, :], op=ALU.mult), r=[gt, rd], w=[gt])
                            fw.op("dve", lambda e: e.tensor_tensor(out=ost[:, :], in0=gt[:, :], in1=psU_[:, :], op=ALU.mult), r=[gt, psU_], w=[ost])
                            fw.op("sp", lambda e: e.dma_start(out=ocT[h * 128:(h + 1) * 128, tc], in_=ost[:, :]), r=[ost], dsem=f"OST{oi}")

                        LAc = 1
                        ptd = {}
                        for ii in range(len(items) + LAc):
                            if ii < len(items):
                                ptd[ii] = s_stage(items[ii])
                            if ii >= LAc:
                                pv_stage(items[ii - LAc], ptd.pop(ii - LAc))
                        for qb in range(4):
                            n_mm = 4 * len(cbs)
                            ix = 0
                            for r in range(4):
                                for cb in cbs:
                                    csz = 128 if cb == 0 else 127
                                    fw.op("pe", lambda e, r=r, cb=cb, csz=csz, qb=qb, ix=ix, n_mm=n_mm: e.matmul(psI[:, :64], lhsT=PN[:csz, r, cb, qb * 128:(qb + 1) * 128], rhs=COV[:csz, cb, :], start=(ix == 0), stop=(ix == n_mm - 1)), r=[PN, COV], w=[psI])
                                    ix += 1
                            fw.op("dve", lambda e, j=j, qb=qb: e.tensor_tensor(out=IMP[:, :], in0=psI[:, :64], in1=ADDT[:, j * 4 + qb, :], op=ALU.add), r=[psI, ADDT], w=[IMP])
                            fw.op("dve", lambda e: e.max(out=M8[:, 0:8], in_=IMP[:, :]), r=[IMP], w=[M8])
                            fw.op("dve", lambda e: e.match_replace(out=WK[:, :], in_to_replace=M8[:, 0:8], in_values=IMP[:, :], imm_value=-3.0e38), r=[IMP, M8], w=[WK])
                            fw.op("dve", lambda e: e.max(out=M8[:, 8:16], in_=WK[:, :]), r=[WK], w=[M8])
                            fw.op("dve", lambda e: e.tensor_scalar(out=SEL[:, :], in0=IMP[:, :], scalar1=M8[:, 15:16], scalar2=None, op0=ALU.is_ge), r=[IMP, M8], w=[SEL])
                            ns, _ = NS.next()
                            fw.op("dve", lambda e, ns=ns: e.tensor_scalar(out=ns[:, :], in0=SEL[:, :], scalar1=-1.0, scalar2=BIGS, op0=ALU.add, op1=ALU.mult), r=[SEL], w=[ns])
                            fw.op("pe", lambda e, ns=ns, qb=qb: e.matmul(psT[:64, qb * 128:(qb + 1) * 128], lhsT=ns[:, :], rhs=IDENT[:, :], start=True, stop=True), r=[ns, IDENT], w=[psT])
                        negs, ni = NEGS.next()
                        fw.op("act", lambda e, negs=negs: e.activation(out=negs[:, :], in_=psT[:64, :], func=AF.Copy), r=[psT], w=[negs])
                        fw.op("sp", lambda e, negs=negs, g=g, tc=tc: e.dma_start(out=negT[g, :, tc], in_=negs[:, :]), r=[negs], dsem=f"NEGS{ni}")
                fw.pop()
            with ExitStack() as sub:
                fw.push(sub)
                Q1 = fw.sbuf([128, S], BF16, "Q1")
                KS = fw.sbuf([128, S], BF16, "KS")
                KW = fw.sbuf([128, S], BF16, "KW")
                VS = fw.sbuf([128, 32, 128], BF16, "VS")
                VW = fw.sbuf([128, 32, 128], BF16, "VW")
                NEGT = fw.sbuf([64, S], BF16, "NEGT")
                EX = fw.sbuf([64, 32, 128], BF16, "EX")
                WSEL = fw.sbuf([128, 2432], F32, "WSEL")
                WWIN = fw.sbuf([128, 1408], F32, "WWIN")
                HS = fw.sbuf([128, 2432], F32, "HSn")
                OACC = fw.sbuf([128, S], F32, "OACC")
                SGP = fw.sbuf([128, S], BF16, "SGP")
                YO = fw.sbuf([128, S], BF16, "YOn")
                RB31 = fw.sbuf([128, 16], F32, "RB31")
                GT = Rot([fw.sbuf([128, 512], F32, f"GTs{q}") for q in range(3)])
                TMP = Rot([fw.sbuf([128, 512], F32, f"TMPs{q}") for q in range(2)])
                PT = Rot([fw.sbuf([128, 512], BF16, f"PTs{q}") for q in range(6)])
                RD = Rot([fw.sbuf([128, 512], F32, f"RDs{q}") for q in range(2)])
                fw.op("pool", lambda e: e.dma_start(out=EX[:, :, :], in_=self.ex), w=[EX], dsem="EX")
                fw.op("sp", lambda e: e.dma_start(out=RB31[:, :], in_=bass.AP(self.relb.tensor, 31 * 16, [[0, 128], [1, 16]])), w=[RB31], dsem="RB31")
                rotS = Rot(self.ps[0:4])
                rotU = Rot(self.ps[4:6])
                rotD = Rot(self.ps[6:8])
                ngr = (DBG_HEADS + 3) // 4
                for g in range(ngr):
                    fw.op("sp", lambda e, g=g: e.dma_start(out=KS[:, :], in_=ksT[g * 128:(g + 1) * 128, :]), w=[KS], dsem="KS")
                    fw.op("sp", lambda e, g=g: e.dma_start(out=KW[:, :], in_=kwT[g * 128:(g + 1) * 128, :]), w=[KW], dsem="KW")
                    fw.op("sp", lambda e, g=g: e.dma_start(out=VS[:, :, :], in_=vsD[:, g * 128:(g + 1) * 128].rearrange("(jb p) c -> p jb c", p=128)), w=[VS], dsem="VS")
                    fw.op("sp", lambda e, g=g: e.dma_start(out=VW[:, :, :], in_=vwD[:, g * 128:(g + 1) * 128].rearrange("(jb p) c -> p jb c", p=128)), w=[VW], dsem="VW")
                    fw.op("sp", lambda e, g=g: e.dma_start(out=NEGT[:, :], in_=negT[g]), w=[NEGT], dsem="NEGT")
                    for r in range(min(4, DBG_HEADS)):
                        h = 4 * g + r
                        fw.op("sp", lambda e, h=h: e.dma_start(out=Q1[:, :], in_=qT[h * 128:(h + 1) * 128, :]), w=[Q1], dsem="Q1")
                        fw.op("sp", lambda e, h=h: e.dma_start(out=OACC[:, :], in_=ocT[h * 128:(h + 1) * 128, :]), w=[OACC], dsem="OACC")
                        fw.op("sp", lambda e, h=h: e.dma_start(out=SGP[:, :], in_=sgT[h * 128:(h + 1) * 128, :]), w=[SGP], dsem="SGP")
                        self.load_toeplitz(WSEL, "sel", h, -384, 2432, HS, "HSn", rotS)
                        self.load_toeplitz(WWIN, "win", h, -384, 1408, HS, "HSn", rotS)
                        items = []
                        for j in range(NTL):
                            for br in (1, 2):
                                if br == 1:
                                    kbs = list(range(0, 4 * j + 4))
                                else:
                                    kbs = list(range(max(0, 4 * j - 4), 4 * j + 4))
                                grp = {"j": j, "br": br}
                                for ix, kb in enumerate(kbs):
                                    items.append((grp, kb, ix == 0, ix == len(kbs) - 1))

                        def s_stage(it, h=h):
                            grp, kb, first, last = it
                            j, br = grp["j"], grp["br"]
                            t0 = j * NT
                            tc = slice(t0, t0 + NT)
                            KK, WW = (KS, WSEL) if br == 1 else (KW, WWIN)
                            if first:
                                gt, gi = GT.next()
                                grp["gt"] = gt
                                fw.op("sp", lambda e: e.dma_start(out=gt[:, :], in_=bass.AP(bgT.tensor, (h * 3 + br) * S + t0, [[0, 128], [1, NT]])), w=[gt], dsem=f"GTs{gi}")
                            d0 = t0 - kb * 128
                            psS, _ = rotS.next()
                            pt, _ = PT.next()
                            kc_ = slice(kb * 128, (kb + 1) * 128)
                            if br == 1:
                                fw.op("pe", lambda e: e.matmul(psS[:, :], lhsT=KK[:, kc_], rhs=Q1[:, tc], start=True, stop=False), r=[KK, Q1], w=[psS])
                                fw.op("pe", lambda e: e.matmul(psS[:, :], lhsT=EX[:, kb, :], rhs=NEGT[:, tc], start=False, stop=True), r=[EX, NEGT], w=[psS])
                            else:
                                fw.op("pe", lambda e: e.matmul(psS[:, :], lhsT=KK[:, kc_], rhs=Q1[:, tc], start=True, stop=True), r=[KK, Q1], w=[psS])
                            if br == 1 and d0 >= 1664:
                                fw.op("act", lambda e: e.activation(out=pt[:, :], in_=psS[:, :], func=AF.Exp, scale=SC, bias=RB31[:, h:h + 1]), r=[psS, RB31], w=[pt])
                            else:
                                tm, _ = TMP.next()
                                fw.op("dve", lambda e: e.scalar_tensor_tensor(out=tm[:, :], in0=psS[:, :], scalar=SC, in1=WW[:, d0 + 384:d0 + 384 + NT], op0=ALU.mult, op1=ALU.add), r=[psS, WW], w=[tm])
                                fw.op("act", lambda e: e.activation(out=pt[:, :], in_=tm[:, :], func=AF.Exp), r=[tm], w=[pt])
                            return pt

                        def pv_stage(it, pt):
                            grp, kb, first, last = it
                            j, br = grp["j"], grp["br"]
                            tc = slice(j * NT, (j + 1) * NT)
                            VV = VS if br == 1 else VW
                            if first:
                                grp["psU"], _ = rotU.next()
                                grp["psD"], _ = rotD.next()
                            psU, psD, gt = grp["psU"], grp["psD"], grp["gt"]
                            fw.op("pe", lambda e: e.matmul(psU[:, :], lhsT=VV[:, kb, :], rhs=pt[:, :], start=first, stop=last), r=[VV, pt], w=[psU])
                            fw.op("pe", lambda e: e.matmul(psD[:, :], lhsT=self.ones_bf[:, :], rhs=pt[:, :], start=first, stop=last), r=[self.ones_bf, pt], w=[psD])
                            if last:
                                rd, _ = RD.next()
                                fw.op("dve", lambda e: e.reciprocal(out=rd[:, :], in_=psD[:, :]), r=[psD], w=[rd])
                                fw.op("pool", lambda e: e.tensor_tensor(out=rd[:, :], in0=rd[:, :], in1=gt[:, :], op=ALU.mult), r=[rd, gt], w=[rd])
                                fw.op("dve", lambda e: e.tensor_tensor(out=rd[:, :], in0=rd[:, :], in1=psU[:, :], op=ALU.mult), r=[rd, psU], w=[rd])
                                fw.op("pool", lambda e: e.tensor_tensor(out=OACC[:, tc], in0=OACC[:, tc], in1=rd[:, :], op=ALU.add), r=[rd, OACC], w=[OACC])

                        LA = 3
                        pts = {}
                        n = len(items)
                        for ii in range(n + LA):
                            if ii < n:
                                pts[ii] = s_stage(items[ii])
                            if ii >= LA:
                                pv_stage(items[ii - LA], pts.pop(ii - LA))
                        fw.op("pool", lambda e: e.tensor_tensor(out=YO[:, :], in0=OACC[:, :], in1=SGP[:, :], op=ALU.mult), r=[OACC, SGP], w=[YO])
                        fw.op("sp", lambda e, h=h: e.dma_start(out=yT[h * 128:(h + 1) * 128, :], in_=YO[:, :]), r=[YO], dsem="YOn")
                fw.pop()
            fw.pop()
        self.post_all(i, self.c_w_out[0], 16, yT, src, dst)


_CACHE = {}


def _prep(inputs, layers=(0, 1, 2, 3), ncores=8):
    vecs, voff = _pack_vecs(inputs)
    key = (tuple(layers), vecs.shape[1])
    if key not in _CACHE:
        _CACHE[key] = Builder(voff, vecs.shape[1], layers)
    b = _CACHE[key]
    oh, cov, addt, cm, ex, ident = _host_tables()
    relb = np.concatenate([np.asarray(inputs["rel_bias"], np.float32), np.full((1, 16), NEG, np.float32)], axis=0)
    shared = {"vecs": vecs, "relb": relb, "oh": oh, "cov": cov, "addt": addt, "cm": cm, "ex": ex, "ident": ident,
              "jrev": np.ascontiguousarray(ident[::-1])}
    for k in ("ple_w_proj", "ple_w_gate", "a_w_in", "a_w_r", "a_w_i", "a_w_out", "b_w_in", "b_w_out", "c_w_in",
              "c_cmp_w1_k", "c_cmp_w2_k", "c_cmp_w1_v", "c_cmp_w2_v", "c_w_out"):
        shared[k] = np.ascontiguousarray(inputs[k], dtype=np.float32)
    x = np.asarray(inputs["x"], np.float32)
    p = np.asarray(inputs["p"], np.float32)
    in_maps = []
    for c in range(ncores):
        m = dict(shared)
        m["xT"] = np.ascontiguousarray(x[c].T)
        m["pT"] = np.ascontiguousarray(p[:, c].transpose(0, 2, 1))
        in_maps.append({k: v for k, v in m.items() if k in b.input_names})
    return b, in_maps


def kernel(**inputs):
    b, in_maps = _prep(inputs)
    res = run_bass_kernel_spmd(b.nc, in_maps, core_ids=list(range(8)))
    out = np.stack([np.ascontiguousarray(r["outT"].T) for r in res.results], axis=0)
    return out.astype(np.float32)
```
